# Optimizing a Trainium2 kernel written in Bass

```python
import math
import jax, jax.numpy as jnp
from jax import lax
import numpy as np

D_MODEL = 1024
BATCH = 32
SEQ = 2048
DEPTH = 2

GRID_W = 64
CTX_LEN = 256

NA_HEADS = 4
NA_DIM = 64
NA_WIN_H = 8
NA_WIN_W = 16
NA_QBLK_W = 16
NA_KBAND_W = NA_QBLK_W + NA_WIN_W
RET_HEADS = 4
RET_DK = 64
RET_DV = 64
RET_CHUNK = 128
GQA_HEADS = 4
GQA_KV_HEADS = 2
GQA_DIM = 64
DIFF_HEADS = 4
DIFF_QK_DIM = 32
DIFF_V_DIM = 64
Q_BLOCK = 128
N_BRANCH = 4
BRANCH_W = 256
ROPE_THETA = 10000.0
EPS = 1e-6
NEG_INF = -1e30
N_EXPERTS = 16
N_GROUPS = 4
EXPERTS_PER_GROUP = N_EXPERTS // N_GROUPS
TOP_K = 2
D_EXPERT = 512
MOE_BLOCK = 512

NA_COLS = 3 * NA_HEADS * NA_DIM
RET_COLS = RET_HEADS * (2 * RET_DK + 2 * RET_DV)
GQA_COLS = (GQA_HEADS + 2 * GQA_KV_HEADS) * GQA_DIM
DIFF_COLS = DIFF_HEADS * (4 * DIFF_QK_DIM + DIFF_V_DIM)
MIX_COLS = NA_COLS + RET_COLS + GQA_COLS + DIFF_COLS
MIX_SPLITS = (NA_COLS, NA_COLS + RET_COLS, NA_COLS + RET_COLS + GQA_COLS)
IN_COLS = MIX_COLS + N_BRANCH * D_MODEL

kernel_name = 'hybrid_grid_diffusion_block'


def rms_norm(x, g):
    xf = x.astype(jnp.float32)
    y = xf * lax.rsqrt(jnp.mean(xf * xf, axis=-1, keepdims=True) + EPS)
    return (y * g.astype(jnp.float32)).astype(x.dtype)


def modulate(h, shift, scale):
    return h * (1.0 + scale) + shift


def split_heads(t, n):
    b, l, _ = t.shape
    return t.reshape(b, l, n, -1).transpose(0, 2, 1, 3)


def merge_heads(t):
    b, n, l, d = t.shape
    return t.transpose(0, 2, 1, 3).reshape(b, l, n * d)


def grid_positions(length):
    t = jnp.arange(length, dtype=jnp.int32)
    return t // GRID_W, t % GRID_W


def axial_rope(x, rows, cols):
    d = x.shape[-1]
    half = d // 2
    nf = half // 2
    inv = 1.0 / (ROPE_THETA ** (jnp.arange(nf, dtype=jnp.float32) / nf))

    def rot(xp, pos):
        ang = pos.astype(jnp.float32)[:, None] * inv[None, :]
        cos, sin = jnp.cos(ang), jnp.sin(ang)
        x1 = xp[..., :nf].astype(jnp.float32)
        x2 = xp[..., nf:].astype(jnp.float32)
        return jnp.concatenate([x1 * cos - x2 * sin, x2 * cos + x1 * sin], axis=-1)

    return jnp.concatenate([rot(x[..., :half], rows), rot(x[..., half:], cols)], axis=-1).astype(x.dtype)


def blocked_attention(q, k, v, scale):
    b, hk, g, lq, d = q.shape
    nb = lq // Q_BLOCK
    qb = q.reshape(b, hk, g, nb, Q_BLOCK, d).transpose(3, 0, 1, 2, 4, 5)

    def one_block(qi):
        s = jnp.einsum('bhgqd,bhkd->bhgqk', qi, k).astype(jnp.float32) * scale
        p = jax.nn.softmax(s, axis=-1).astype(v.dtype)
        return jnp.einsum('bhgqk,bhkd->bhgqd', p, v)

    o = lax.map(one_block, qb)
    return o.transpose(1, 2, 3, 0, 4, 5).reshape(b, hk, g, lq, v.shape[-1])


def blocked_diff_attention(q, k, v, lam, scale):
    b, h, _, lq, dq = q.shape
    nb = lq // Q_BLOCK
    qb = q.reshape(b, h, 2, nb, Q_BLOCK, dq).transpose(3, 0, 1, 2, 4, 5)

    def one_block(qi):
        s = jnp.einsum('bhmqd,bhmkd->bhmqk', qi, k).astype(jnp.float32) * scale
        p = jax.nn.softmax(s, axis=-1)
        a = (p[:, :, 0] - lam * p[:, :, 1]).astype(v.dtype)
        return jnp.einsum('bhqk,bhkd->bhqd', a, v)

    o = lax.map(one_block, qb)
    return o.transpose(1, 2, 0, 3, 4).reshape(b, h, lq, v.shape[-1])


def neighbourhood_attention(p_lat, p_ctx, rpb, need_ctx):
    b, l, _ = p_lat.shape
    rows = l // GRID_W
    win_h = min(NA_WIN_H, rows)
    n_cb = GRID_W // NA_QBLK_W
    n_win = win_h * NA_KBAND_W
    scale = NA_DIM ** -0.5
    q, k, v = (split_heads(t, NA_HEADS) for t in jnp.split(p_lat, 3, axis=-1))
    qc, kc, vc = (split_heads(t, NA_HEADS) for t in jnp.split(p_ctx, 3, axis=-1))
    qg = (q * scale).reshape(b, NA_HEADS, rows, GRID_W, NA_DIM)
    kg = k.reshape(b, NA_HEADS, rows, GRID_W, NA_DIM)
    vg = v.reshape(b, NA_HEADS, rows, GRID_W, NA_DIM)
    band_start = np.clip(np.arange(n_cb) * NA_QBLK_W - NA_WIN_W // 2, 0, GRID_W - NA_KBAND_W)
    band_cols = band_start[:, None] + np.arange(NA_KBAND_W)[None, :]
    q_cols = np.arange(GRID_W).reshape(n_cb, NA_QBLK_W)
    win_start = np.clip(q_cols - NA_WIN_W // 2, 0, GRID_W - NA_WIN_W)
    col_ok = ((band_cols[:, None, :] >= win_start[:, :, None])
              & (band_cols[:, None, :] < win_start[:, :, None] + NA_WIN_W))
    col_idx = np.clip(band_cols[:, None, :] - q_cols[:, :, None], -(NA_WIN_W - 1), NA_WIN_W - 1) + NA_WIN_W - 1
    key_ok = np.broadcast_to(col_ok[:, :, None, :], (n_cb, NA_QBLK_W, win_h, NA_KBAND_W)).reshape(n_cb, NA_QBLK_W, n_win)

    def attend_row(r):
        r0 = jnp.clip(r - win_h // 2, 0, rows - win_h)
        k_band = lax.dynamic_slice_in_dim(kg, r0, win_h, axis=2)[:, :, :, band_cols]
        v_band = lax.dynamic_slice_in_dim(vg, r0, win_h, axis=2)[:, :, :, band_cols]
        k_blk = k_band.transpose(0, 1, 3, 2, 4, 5).reshape(b, NA_HEADS, n_cb, n_win, NA_DIM)
        v_blk = v_band.transpose(0, 1, 3, 2, 4, 5).reshape(b, NA_HEADS, n_cb, n_win, NA_DIM)
        q_row = lax.dynamic_index_in_dim(qg, r, axis=2, keepdims=False).reshape(b, NA_HEADS, n_cb, NA_QBLK_W, NA_DIM)
        row_idx = r0 + jnp.arange(win_h) - r + NA_WIN_H - 1
        bias = rpb[:, row_idx][:, :, col_idx]
        bias = bias.transpose(0, 2, 3, 1, 4).reshape(NA_HEADS, n_cb, NA_QBLK_W, n_win)
        s_win = jnp.einsum('bhcqd,bhckd->bhcqk', q_row, k_blk).astype(jnp.float32) + bias.astype(jnp.float32)
        s_win = jnp.where(key_ok, s_win, NEG_INF)
        s_ctx = jnp.einsum('bhcqd,bhkd->bhcqk', q_row, kc).astype(jnp.float32)
        p = jax.nn.softmax(jnp.concatenate([s_win, s_ctx], axis=-1), axis=-1).astype(v.dtype)
        o = (jnp.einsum('bhcqk,bhckd->bhcqd', p[..., :n_win], v_blk)
             + jnp.einsum('bhcqk,bhkd->bhcqd', p[..., n_win:], vc))
        return o.reshape(b, NA_HEADS, GRID_W, NA_DIM)

    o = lax.map(attend_row, jnp.arange(rows, dtype=jnp.int32))
    y_lat = o.transpose(1, 0, 3, 2, 4).reshape(b, l, NA_HEADS * NA_DIM)
    y_ctx = None
    if need_ctx:
        y_ctx = merge_heads(blocked_attention(qc[:, :, None], kc, vc, scale)[:, :, 0])
    return y_lat, y_ctx


def retention_scan(q, k, v, log_gamma, state0):
    b, h, l, _ = q.shape
    dv = v.shape[-1]
    n = l // RET_CHUNK

    def to_chunks(t):
        return t.reshape(b, h, n, RET_CHUNK, t.shape[-1]).transpose(2, 0, 1, 3, 4)

    pos = jnp.arange(RET_CHUNK, dtype=jnp.float32)
    lag = pos[:, None] - pos[None, :]
    lower = lag >= 0
    intra = jnp.where(lower, jnp.exp(jnp.where(lower, lag, 0.0)[None] * log_gamma[:, None, None]), 0.0)
    q_dec = jnp.exp((pos + 1.0)[None, :] * log_gamma[:, None])
    k_dec = jnp.exp((RET_CHUNK - 1.0 - pos)[None, :] * log_gamma[:, None])
    chunk_dec = jnp.exp(RET_CHUNK * log_gamma)

    def step(state, inp):
        qi, ki, vi = inp
        inner = jnp.einsum('bhqd,bhkd->bhqk', qi, ki) * intra
        o = (jnp.einsum('bhqk,bhke->bhqe', inner, vi)
             + jnp.einsum('bhqd,bhde->bhqe', qi * q_dec[..., None], state))
        state = state * chunk_dec[:, None, None] + jnp.einsum('bhkd,bhke->bhde', ki * k_dec[..., None], vi)
        return state, o

    state, o = lax.scan(step, state0, (to_chunks(q), to_chunks(k), to_chunks(v)))
    return o.transpose(1, 2, 0, 3, 4).reshape(b, h, l, dv), state


def retention_mixer(p_lat, p_ctx, log_decay_param, rows_pos, cols_pos, need_ctx):
    splits = [RET_HEADS * RET_DK, 2 * RET_HEADS * RET_DK, 2 * RET_HEADS * RET_DK + RET_HEADS * RET_DV]

    def prep(p, rope):
        q, k, v, g = jnp.split(p, splits, axis=-1)
        q, k, v = (split_heads(t, RET_HEADS).astype(jnp.float32) for t in (q, k, v))
        if rope:
            q = axial_rope(q, rows_pos, cols_pos)
            k = axial_rope(k, rows_pos, cols_pos)
        return q, k * RET_DK ** -0.5, v, g

    ql, kl, vl, gl = prep(p_lat, True)
    qc, kc, vc, gc = prep(p_ctx, False)
    log_gamma = jnp.log1p(-jnp.exp(log_decay_param.astype(jnp.float32)))
    zero = jnp.zeros((p_lat.shape[0], RET_HEADS, RET_DK, RET_DV), jnp.float32)

    def flip(t):
        return jnp.flip(t, axis=2)

    oc_f, s_f = retention_scan(qc, kc, vc, log_gamma[0], zero)
    oc_b, s_b = retention_scan(flip(qc), flip(kc), flip(vc), log_gamma[1], zero)
    ol_f, _ = retention_scan(ql, kl, vl, log_gamma[0], s_f)
    ol_b, _ = retention_scan(flip(ql), flip(kl), flip(vl), log_gamma[1], s_b)

    def finish(o, g):
        mu = jnp.mean(o, axis=-1, keepdims=True)
        var = jnp.mean(jnp.square(o - mu), axis=-1, keepdims=True)
        o = (o - mu) * lax.rsqrt(var + EPS)
        return jax.nn.silu(g) * merge_heads(o).astype(g.dtype)

    y_lat = finish(ol_f + flip(ol_b), gl)
    y_ctx = finish(oc_f + flip(oc_b), gc) if need_ctx else None
    return y_lat, y_ctx


def gqa_mixer(p_lat, p_ctx, q_gain, k_gain, rows_pos, cols_pos, need_ctx):
    nq = GQA_HEADS * GQA_DIM
    nkv = GQA_KV_HEADS * GQA_DIM
    group = GQA_HEADS // GQA_KV_HEADS
    scale = GQA_DIM ** -0.5

    def prep(p):
        q, k, v = jnp.split(p, [nq, nq + nkv], axis=-1)
        return (rms_norm(split_heads(q, GQA_HEADS), q_gain),
                rms_norm(split_heads(k, GQA_KV_HEADS), k_gain),
                split_heads(v, GQA_KV_HEADS))

    def grouped(q):
        b, _, l, d = q.shape
        return q.reshape(b, GQA_KV_HEADS, group, l, d)

    def ungroup(o):
        b, hk, g, l, d = o.shape
        return merge_heads(o.reshape(b, hk * g, l, d))

    ql, kl, vl = prep(p_lat)
    qc, kc, vc = prep(p_ctx)
    ql = axial_rope(ql, rows_pos, cols_pos)
    kl = axial_rope(kl, rows_pos, cols_pos)
    k_all = jnp.concatenate([kl, kc], axis=2)
    v_all = jnp.concatenate([vl, vc], axis=2)
    y_lat = ungroup(blocked_attention(grouped(ql), k_all, v_all, scale))
    y_ctx = ungroup(blocked_attention(grouped(qc), kc, vc, scale)) if need_ctx else None
    return y_lat, y_ctx


def diff_mixer(p_lat, p_ctx, lam_params, subln_gain, layer_idx, rows_pos, cols_pos, need_ctx):
    nqk = DIFF_HEADS * 2 * DIFF_QK_DIM
    scale = DIFF_QK_DIM ** -0.5

    def prep(p):
        b, l, _ = p.shape
        q, k, v = jnp.split(p, [nqk, 2 * nqk], axis=-1)
        q = q.reshape(b, l, DIFF_HEADS, 2, DIFF_QK_DIM).transpose(0, 2, 3, 1, 4)
        k = k.reshape(b, l, DIFF_HEADS, 2, DIFF_QK_DIM).transpose(0, 2, 3, 1, 4)
        return q, k, split_heads(v, DIFF_HEADS)

    ql, kl, vl = prep(p_lat)
    qc, kc, vc = prep(p_ctx)
    ql = axial_rope(ql, rows_pos, cols_pos)
    kl = axial_rope(kl, rows_pos, cols_pos)
    lam_init = 0.8 - 0.6 * math.exp(-0.3 * layer_idx)
    lp = lam_params.astype(jnp.float32)
    lam = jnp.exp(jnp.sum(lp[0] * lp[1])) - jnp.exp(jnp.sum(lp[2] * lp[3])) + lam_init

    def finish(o):
        return merge_heads(rms_norm(o, subln_gain) * (1.0 - lam_init))

    k_all = jnp.concatenate([kl, kc], axis=3)
    v_all = jnp.concatenate([vl, vc], axis=2)
    y_lat = finish(blocked_diff_attention(ql, k_all, v_all, lam, scale))
    y_ctx = finish(blocked_diff_attention(qc, kc, vc, lam, scale)) if need_ctx else None
    return y_lat, y_ctx


def token_mixers(h_lat, h_ctx, w_in, na_rpb, ret_log_decay, gqa_q_gain, gqa_k_gain,
                 diff_lambda, diff_subln, w_branch, w_out, layer_idx, need_ctx):
    d = h_lat.shape[-1]
    rows_pos, cols_pos = grid_positions(h_lat.shape[1])
    w_mix = w_in[:, :MIX_COLS]
    a_l, b_l, c_l, d_l = jnp.split(h_lat @ w_mix, MIX_SPLITS, axis=-1)
    a_c, b_c, c_c, d_c = jnp.split(h_ctx @ w_mix, MIX_SPLITS, axis=-1)
    ya = neighbourhood_attention(a_l, a_c, na_rpb, need_ctx)
    yb = retention_mixer(b_l, b_c, ret_log_decay, rows_pos, cols_pos, need_ctx)
    yc = gqa_mixer(c_l, c_c, gqa_q_gain, gqa_k_gain, rows_pos, cols_pos, need_ctx)
    yd = diff_mixer(d_l, d_c, diff_lambda, diff_subln, layer_idx, rows_pos, cols_pos, need_ctx)

    def merge(h, ys):
        acc = None
        for n, y in enumerate(ys):
            gate = jax.nn.sigmoid(h @ w_in[:, MIX_COLS + n * d: MIX_COLS + (n + 1) * d])
            term = gate * (y @ w_branch[n])
            acc = term if acc is None else acc + term
        return acc @ w_out

    y_lat = merge(h_lat, (ya[0], yb[0], yc[0], yd[0]))
    y_ctx = merge(h_ctx, (ya[1], yb[1], yc[1], yd[1])) if need_ctx else None
    return y_lat, y_ctx


def expert_dispatch(h, e_idx, e_w, w_gate, w_up, w_down):
    n, d = h.shape
    a = n * TOP_K
    flat_e = e_idx.reshape(a)
    flat_tok = jnp.repeat(jnp.arange(n, dtype=jnp.int32), TOP_K)
    flat_w = e_w.reshape(a)
    order = jnp.argsort(flat_e)
    se = flat_e[order]
    counts = jnp.bincount(flat_e, length=N_EXPERTS)
    padded = (counts + MOE_BLOCK - 1) // MOE_BLOCK * MOE_BLOCK
    pad_end = jnp.cumsum(padded)
    pad_start = pad_end - padded
    grp_start = jnp.cumsum(counts) - counts
    dest = pad_start[se] + jnp.arange(a, dtype=jnp.int32) - grp_start[se]
    n_blocks = -(-a // MOE_BLOCK) + N_EXPERTS
    total = n_blocks * MOE_BLOCK
    slot_tok = jnp.full((total,), n, jnp.int32).at[dest].set(flat_tok[order])
    slot_w = jnp.zeros((total,), flat_w.dtype).at[dest].set(flat_w[order])
    blk_e = jnp.minimum(jnp.searchsorted(pad_end, jnp.arange(n_blocks, dtype=jnp.int32) * MOE_BLOCK, side='right'),
                        N_EXPERTS - 1)
    h_pad = jnp.concatenate([h, jnp.zeros((1, d), h.dtype)], axis=0)
    xb = h_pad[slot_tok].reshape(n_blocks, MOE_BLOCK, d)

    def run(args):
        xi, e = args
        return (jax.nn.silu(xi @ w_gate[e]) * (xi @ w_up[e])) @ w_down[e]

    yb = lax.map(run, (xb, blk_e)).reshape(total, d)
    out = jnp.zeros((n + 1, d), h.dtype).at[slot_tok].add(yb * slot_w[:, None].astype(yb.dtype))
    return out[:n]


def routed_moe(h, w_router, router_bias, w_gate, w_up, w_down):
    n = h.shape[0]
    s = jax.nn.sigmoid((h @ w_router).astype(jnp.float32))
    sel = (s + router_bias.astype(jnp.float32)).reshape(n, N_GROUPS, EXPERTS_PER_GROUP)
    group_score = jnp.sum(lax.top_k(sel, TOP_K)[0], axis=-1)
    g_idx = jnp.argmax(group_score, axis=-1).astype(jnp.int32)
    in_group = jnp.take_along_axis(sel, g_idx[:, None, None], axis=1)[:, 0]
    _, local = lax.top_k(in_group, TOP_K)
    e_idx = g_idx[:, None] * EXPERTS_PER_GROUP + local.astype(jnp.int32)
    w = jnp.take_along_axis(s, e_idx, axis=1)
    w = w / jnp.sum(w, axis=-1, keepdims=True)
    return expert_dispatch(h, e_idx, w, w_gate, w_up, w_down)


def setup_inputs(seed: int = 0) -> dict:
    key = jax.random.key(seed)
    ks = jax.random.split(key, 23)
    d = D_MODEL

    def normal(k, shape, std):
        return jax.random.normal(k, shape, jnp.float32) * std

    ret_base = -(5.0 + jnp.arange(RET_HEADS, dtype=jnp.float32)) * math.log(2.0)
    return {
        'x': normal(ks[0], (BATCH, SEQ, d), 1.0),
        'c': normal(ks[1], (BATCH, d), 1.0),
        'ctx': normal(ks[2], (BATCH, CTX_LEN, d), 1.0),
        'c_ctx': normal(ks[3], (d,), 1.0),
        'w_mod': normal(ks[4], (DEPTH, d, 6 * d), 0.5 * d ** -0.5),
        'b_mod': normal(ks[5], (DEPTH, 6 * d), 0.02),
        'g_norm1': 1.0 + normal(ks[6], (DEPTH, d), 0.02),
        'g_norm2': 1.0 + normal(ks[7], (DEPTH, d), 0.02),
        'w_in': normal(ks[8], (DEPTH, d, IN_COLS), d ** -0.5),
        'na_rpb': normal(ks[9], (DEPTH, NA_HEADS, 2 * NA_WIN_H - 1, 2 * NA_WIN_W - 1), 0.02),
        'ret_log_decay': ret_base + normal(ks[10], (DEPTH, 2, RET_HEADS), 0.05),
        'gqa_q_gain': 1.0 + normal(ks[11], (DEPTH, GQA_DIM), 0.02),
        'gqa_k_gain': 1.0 + normal(ks[12], (DEPTH, GQA_DIM), 0.02),
        'diff_lambda': normal(ks[13], (DEPTH, 4, DIFF_QK_DIM), 0.1),
        'diff_subln': 1.0 + normal(ks[14], (DEPTH, DIFF_V_DIM), 0.02),
        'w_branch': normal(ks[15], (DEPTH, N_BRANCH, BRANCH_W, d), BRANCH_W ** -0.5),
        'w_out': normal(ks[16], (DEPTH, d, d), d ** -0.5),
        'w_router': normal(ks[17], (d, N_EXPERTS), d ** -0.5),
        'router_bias': normal(ks[18], (N_EXPERTS,), 0.01),
        'w_gate_e': normal(ks[19], (DEPTH, N_EXPERTS, d, D_EXPERT), d ** -0.5),
        'w_up_e': normal(ks[20], (DEPTH, N_EXPERTS, d, D_EXPERT), d ** -0.5),
        'w_down_e': normal(ks[21], (DEPTH, N_EXPERTS, D_EXPERT, d), D_EXPERT ** -0.5),
        'g_final': 1.0 + normal(ks[22], (d,), 0.02),
    }


def reference(x, c, ctx, c_ctx, w_mod, b_mod, g_norm1, g_norm2, w_in, na_rpb, ret_log_decay,
              gqa_q_gain, gqa_k_gain, diff_lambda, diff_subln, w_branch, w_out, w_router,
              router_bias, w_gate_e, w_up_e, w_down_e, g_final):
    b, l, d = x.shape
    n_ctx = ctx.shape[1]
    hc = ctx
    for layer in range(DEPTH):
        need_ctx = layer < DEPTH - 1
        mod_lat = (jax.nn.silu(c) @ w_mod[layer] + b_mod[layer])[:, None, :]
        mod_ctx = (jax.nn.silu(c_ctx) @ w_mod[layer] + b_mod[layer])[None, None, :]
        sh1, sc1, gt1, sh2, sc2, gt2 = jnp.split(mod_lat, 6, axis=-1)
        sh1c, sc1c, gt1c, sh2c, sc2c, gt2c = jnp.split(mod_ctx, 6, axis=-1)
        h_lat = modulate(rms_norm(x, g_norm1[layer]), sh1, sc1)
        h_ctx = modulate(rms_norm(hc, g_norm1[layer]), sh1c, sc1c)
        y_lat, y_ctx = token_mixers(h_lat, h_ctx, w_in[layer], na_rpb[layer], ret_log_decay[layer],
                                    gqa_q_gain[layer], gqa_k_gain[layer], diff_lambda[layer],
                                    diff_subln[layer], w_branch[layer], w_out[layer], layer, need_ctx)
        x = x + gt1 * y_lat
        m_lat = modulate(rms_norm(x, g_norm2[layer]), sh2, sc2).reshape(b * l, d)
        if need_ctx:
            hc = hc + gt1c * y_ctx
            m_ctx = modulate(rms_norm(hc, g_norm2[layer]), sh2c, sc2c).reshape(b * n_ctx, d)
            f = routed_moe(jnp.concatenate([m_lat, m_ctx], axis=0), w_router, router_bias,
                           w_gate_e[layer], w_up_e[layer], w_down_e[layer])
            x = x + gt2 * f[:b * l].reshape(b, l, d)
            hc = hc + gt2c * f[b * l:].reshape(b, n_ctx, d)
        else:
            f = routed_moe(m_lat, w_router, router_bias, w_gate_e[layer], w_up_e[layer], w_down_e[layer])
            x = x + gt2 * f.reshape(b, l, d)
    return rms_norm(x, g_final)
```

```python
import math
import numpy as np
import concourse.bass as bass
import concourse.mybir as mybir
from concourse.bass_utils import run_bass_kernel_spmd

F32 = mybir.dt.float32
AF = mybir.ActivationFunctionType
ALU = mybir.AluOpType
AX = mybir.AxisListType

D = 1024
T = 2304
NCTX = 256
L = 2048
EPS = 1e-6
TILES = [(0, 256), (256, 512), (768, 512), (1280, 512), (1792, 512)]
NEGM = -1.0e4
DEPTH = 2
import os
RET_STOP = int(os.environ.get('RET_STOP', '99'))
RET_STEP = int(os.environ.get('RET_STEP', '99'))


class Ctx:
    pass


def build(nsamp=4, nlayers=2, dbg=None, stop_after=None):
    nc = bass.Bass("TRN2", target_bir_lowering=False)
    K = Ctx()

    def din(name, shape):
        return nc.dram_tensor(name, list(shape), F32, kind="ExternalInput").ap()

    xin = din("xin", [nsamp, D, T])
    cT_in = din("cT", [128, 8, 5])
    w_mod = din("w_mod", [2, D, 6 * D])
    bmod_in = din("bmod", [128, 2, 48])
    gn_in = din("gn", [128, 5, 8])
    w_in = din("w_in", [2, D, 7168])
    nab_in = din("nab", [2, 128, 4, 14, 64])
    namask_in = din("namask", [128, 64])
    retp_in = din("retp", [2, 128, 8])
    gqag_in = din("gqag", [2, 64, 4])
    dlam_in = din("dlam", [2, 128, 128])
    dsub_in = din("dsub", [64, 2])
    lamc_in = din("lamc", [128, 2, 2])
    w_branch = din("w_branch", [2, 4, 256, D])
    w_out = din("w_out", [2, D, D])
    w_router = din("w_router", [D, 16])
    rb_in = din("rb", [128, 16])
    w_gate = din("w_gate", [2, 16, D, 512])
    w_up = din("w_up", [2, 16, D, 512])
    w_down = din("w_down", [2, 16, 512, D])
    rope64_in = din("rope64", [2, 64, T])
    rope32_in = din("rope32", [2, 32, T])
    ident_in = din("ident", [128, 128])
    seld_in = din("seld", [65, 64])
    sel16_in = din("sel16", [16, 16, 128])
    retc_in = din("retc", [128, 4, 128])
    retpos_in = din("retpos", [128, 4, 128])

    out = nc.dram_tensor("out", [nsamp + 1, D, L], F32, kind="ExternalOutput").ap()
    OUTS = nc.dram_tensor("OUTS", [D, L], F32, kind="Internal").ap()
    dbg_outs = {}
    dbg_t = dbg_outs

    def dscratch(name, shape):
        kind = "ExternalOutput" if (dbg and name in dbg) else "Internal"
        t = nc.dram_tensor(name, list(shape), F32, kind=kind).ap()
        if kind == "ExternalOutput":
            dbg_outs[name] = t
        return t

    XS = dscratch("XS", [D, T])
    PT = dscratch("PT", [7168, T])
    VT = dscratch("VT", [T, 896])
    YT = dscratch("YT", [1024, T])
    AT = dscratch("AT", [D, T])
    STG = dscratch("STG", [128, T])
    ACCD = dscratch("ACCD", [D, T])
    MTD = dscratch("MTD", [D, T])
    if dbg and "X1" in dbg:
        dscratch("X1", [D, T])
    if dbg and "HT" in dbg:
        dscratch("HT", [D, T])

    from contextlib import ExitStack
    es = ExitStack()
    es.__enter__()

    _cnt = [0]

    _peak = {}

    class _SBTW:
        def __init__(self, name, shape):
            self.name = name
            self.cm = nc.sbuf_tensor(f"sb{_cnt[0]}_{name}", list(shape), F32)

        def __enter__(self):
            r = self.cm.__enter__()
            used = 229376 - nc.sbuf_bytes_remaining
            key = self.name.split("_")[0]
            _peak[key] = max(_peak.get(key, 0), used)
            return r

        def __exit__(self, *a):
            return self.cm.__exit__(*a)

    def SBT(name, shape):
        _cnt[0] += 1
        return _SBTW(name, shape)

    def sb(name, shape):
        return es.enter_context(SBT(name, list(shape)))

    PS = [es.enter_context(nc.psum_tensor(f"ps{i}", [128, 512], F32)) for i in range(8)]
    dsem = es.enter_context(nc.semaphore("dsem"))

    class DQ:
        n = 0

        def start(self, out, in_):
            nc.sync.dma_start(out=out, in_=in_).then_inc(dsem, 16)
            self.n += 1

        def wait(self):
            if self.n:
                nc.sync.wait_ge(dsem, 16 * self.n)
                nc.sync.sem_clear(dsem)
                self.n = 0

    dq = DQ()

    def B():
        nc.all_engine_barrier()

    def LW():
        dq.wait()
        B()

    V = nc.vector
    A = nc.scalar
    PE = nc.tensor

    import re as _re
    from contextlib import contextmanager as _cm
    _RH = bass.RegisterHandle
    _ENGS = {"Pool": mybir.EngineType.Pool, "Activation": mybir.EngineType.Activation, "PE": mybir.EngineType.PE,
             "DVE": mybir.EngineType.DVE, "SP": mybir.EngineType.SP}
    _ALLE = mybir.ALL_ENGINES
    _lc = [0]

    def _cur_id():
        h = nc.vector.alloc_register()
        m = _re.search(r"(\d+)$", h.name)
        nc.vector.free_register(h)
        return int(m.group(1))

    @_cm
    def MyFori(start, end):
        id0 = _cur_id()
        _lc[0] += 1
        name = f"mf{_lc[0]}"
        ls, le = name + "_loop", name + "_end"
        regs = nc.alloc_registers(name + "_i", engines=_ALLE)
        nc.regs_mov(regs, start)
        nc.br(ls, engines=_ALLE)
        with nc.body(ls, valid_engines=_ALLE):
            i = nc.snap(regs, min_val=start, max_val=end - 1)
            yield i
            nc.regs_alu(regs, regs, 1, op=mybir.AluOpType.add)
            nc.br_lt(regs, end, on_true=ls, on_false=le, engines=_ALLE)
        nc.switch_bb(le)
        for h in regs.handles:
            nc.free_register(h)
        if not isinstance(i, int):
            for e in nc.engines.values():
                al = e.get_value_cache().lookup(i)
                if al is not None:
                    nc.free_register(al.val)
        id1 = _cur_id()
        for en, et in _ENGS.items():
            for k in range(id0, id1 + 1):
                nm = f"{en}_tmp_{k}"
                try:
                    r = nc.lookup_reg(nm)
                except Exception:
                    r = None
                if r is not None and getattr(r, "allocated", False):
                    nc.free_register(_RH(name=nm, engine=et))


    ident = sb("ident", [128, 128])
    ones = sb("ones", [128, 128])
    epsc = sb("epsc", [128, 1])
    MOD = sb("MOD", [128, 2, 48, 5])
    MODS = sb("MODS", [128, 2, 2, 48])
    GG = sb("GG", [128, 2, 2, 2, 8])
    GN = sb("GN", [128, 5, 8])
    BMOD = sb("BMOD", [128, 2, 48])
    SELD = sb("SELD", [65, 64])
    ZERO8 = sb("ZERO8", [128, 8])
    LAMC = sb("LAMC", [128, 2, 2])
    DSUB = sb("DSUB", [64, 2])

    dq.start(ident[:], ident_in)
    dq.start(GN[:], gn_in)
    dq.start(BMOD[:], bmod_in)
    dq.start(SELD[:], seld_in)
    dq.start(LAMC[:], lamc_in)
    dq.start(DSUB[:], dsub_in)
    V.memset(ones[:], 1.0)
    V.memset(epsc[:], EPS)
    V.memset(ZERO8[:], 0.0)
    LW()

    def stage_mods():
        with SBT("scT", [128, 8, 5]) as scT, SBT("wm", [128, 8, 128]) as wm:
            dq.start(scT[:], cT_in)
            LW()
            A.activation(out=scT[:], in_=scT[:], func=AF.Silu)
            B()
            for l in range(2):
                wv = w_mod[l].rearrange("(k p) (m j) -> p k m j", p=128, j=128)
                with MyFori(0, 48) as m:
                    dq.start(wm[:], wv[:, :, m, :])
                    LW()
                    for k in range(8):
                        PE.matmul(PS[0][:, 0:5], wm[:, k, :], scT[:, k, :], start=(k == 0), stop=(k == 7))
                    B()
                    V.tensor_scalar(out=MOD[:, l, m, :], in0=PS[0][:, 0:5], scalar1=BMOD[:, l, bass.ts(m, 1)],
                                    scalar2=None, op0=ALU.add)
                    B()

    stage_mods()

    def stage_norm(BIG, gfun, shfun):
        XSv = XS.rearrange("(k p) t -> p k t", p=128)
        with SBT("n_xt", [128, 8, 512]) as xt, SBT("n_sq", [128, 8, 512]) as sq, \
                SBT("n_rs", [128, 512]) as rs:
            for ti, (t0, n) in enumerate(TILES):
                v = 0 if ti == 0 else 1
                dq.start(xt[:, :, 0:n], XSv[:, :, t0:t0 + n])
                LW()
                A.activation(out=sq[:, :, 0:n], in_=xt[:, :, 0:n], func=AF.Square)
                B()
                for k in range(8):
                    PE.matmul(PS[0][:, 0:n], ones[:, :], sq[:, k, 0:n], start=(k == 0), stop=(k == 7))
                B()
                A.activation(out=rs[:, 0:n], in_=PS[0][:, 0:n], func=AF.Sqrt, bias=epsc[:, :], scale=1.0 / D)
                B()
                V.reciprocal(out=rs[:, 0:n], in_=rs[:, 0:n])
                B()
                for k in range(8):
                    V.scalar_tensor_tensor(out=sq[:, k, 0:n], in0=xt[:, k, 0:n], scalar=gfun(v, k), in1=rs[:, 0:n],
                                           op0=ALU.mult, op1=ALU.mult)
                B()
                for k in range(8):
                    A.activation(out=BIG[:, k, t0:t0 + n], in_=sq[:, k, 0:n], func=AF.Identity, bias=shfun(v, k),
                                 scale=1.0)
                B()

    def stage_gemm(BIG, wview, m_lo, m_hi, orow0, dst, epi):
        dstv = dst.rearrange("(m p) t -> m p t", p=128)
        with SBT("g_w", [128, 8, 128]) as wt, SBT("g_o", [128, T]) as ot, \
                SBT("g_x", [128, T]) as xr:
            with MyFori(m_lo, m_hi) as m:
                dq.start(wt[:], wview[:, :, m, :])
                if epi[0] == "resid":
                    dq.start(xr[:], dstv[m + orow0])
                LW()
                for ti, (t0, n) in enumerate(TILES):
                    for k in range(8):
                        PE.matmul(PS[ti][:, 0:n], wt[:, k, :], BIG[:, k, t0:t0 + n], start=(k == 0), stop=(k == 7))
                B()
                for ti, (t0, n) in enumerate(TILES):
                    if epi[0] == "copy":
                        if ti % 2 == 0:
                            A.activation(out=ot[:, t0:t0 + n], in_=PS[ti][:, 0:n], func=AF.Copy)
                        else:
                            V.tensor_copy(out=ot[:, t0:t0 + n], in_=PS[ti][:, 0:n])
                    elif epi[0] == "sigmoid":
                        A.activation(out=ot[:, t0:t0 + n], in_=PS[ti][:, 0:n], func=AF.Sigmoid)
                    elif epi[0] == "resid":
                        v = 0 if ti == 0 else 1
                        V.scalar_tensor_tensor(out=ot[:, t0:t0 + n], in0=PS[ti][:, 0:n], scalar=epi[1](v, m),
                                               in1=xr[:, t0:t0 + n], op0=ALU.mult, op1=ALU.add)
                B()
                dq.start(dstv[m + orow0], ot[:])
                LW()

    VCOLS = [(512, 256), (1280, 256), (2176, 128), (2816, 256)]

    def stage_vtok(BIG, l):
        VTv = VT.rearrange("(c p) f -> c p f", p=128)
        wl = w_in[l].rearrange("(k p) f -> p k f", p=128)
        with SBT("v_w", [128, 8, 896]) as wv, SBT("v_o", [128, 896]) as vo, SBT("v_stg", [128, 8, 128]) as stg:
            c0 = 0
            for (col0, wd) in VCOLS:
                dq.start(wv[:, :, c0:c0 + wd], wl[:, :, col0:col0 + wd])
                c0 += wd
            LW()
            with MyFori(0, 18) as tc:
                V.tensor_copy(out=stg[:], in_=BIG[:, :, bass.ts(tc, 128)])
                B()
                for k in range(8):
                    PE.matmul(PS[0][:, 0:512], stg[:, k, :], wv[:, k, 0:512], start=(k == 0), stop=(k == 7))
                for k in range(8):
                    PE.matmul(PS[1][:, 0:384], stg[:, k, :], wv[:, k, 512:896], start=(k == 0), stop=(k == 7))
                B()
                A.activation(out=vo[:, 0:512], in_=PS[0][:, 0:512], func=AF.Copy)
                V.tensor_copy(out=vo[:, 512:896], in_=PS[1][:, 0:384])
                B()
                dq.start(VTv[tc], vo[:])
                LW()

    def prep(dst, blk, d, tabs, bufs, gains=None, norm=False):
        q = d // 4
        Ab, Bb, t1, t2 = bufs
        if callable(blk):
            rb = blk
        else:
            PTq = PT.rearrange("(a p) t -> a p t", p=q)
            rb = lambda i: PTq[blk + i]
        for i in range(4):
            dq.start(Ab[i * q:(i + 1) * q, :], rb(i))
        for i, j in enumerate((1, 0, 3, 2)):
            dq.start(Bb[i * q:(i + 1) * q, :], rb(j))
        LW()
        C, S = tabs
        if norm:
            A.activation(out=t2[0:d, :], in_=Ab[0:d, :], func=AF.Square)
            B()
            for ti, (t0, n) in enumerate(TILES):
                PE.matmul(PS[ti][0:d, 0:n], ones[0:d, 0:d], t2[0:d, t0:t0 + n], start=True, stop=True)
            B()
            for ti, (t0, n) in enumerate(TILES):
                A.activation(out=dst[0:d, t0:t0 + n], in_=PS[ti][0:d, 0:n], func=AF.Sqrt, bias=epsc[0:d, :], scale=1.0 / d)
            B()
            V.reciprocal(out=dst[0:d, :], in_=dst[0:d, :])
        if gains is not None:
            V.tensor_scalar(out=Ab[0:d, :], in0=Ab[0:d, :], scalar1=gains[0], scalar2=None, op0=ALU.mult)
            V.tensor_scalar(out=Bb[0:d, :], in0=Bb[0:d, :], scalar1=gains[1], scalar2=None, op0=ALU.mult)
        B()
        if t1 is None:
            t1, t2 = Ab, Bb
        V.tensor_tensor(out=t1[0:d, :], in0=Ab[0:d, :], in1=C, op=ALU.mult)
        V.tensor_tensor(out=t2[0:d, :], in0=Bb[0:d, :], in1=S, op=ALU.mult)
        B()
        if norm:
            V.tensor_tensor(out=t1[0:d, :], in0=t1[0:d, :], in1=t2[0:d, :], op=ALU.add)
            B()
            V.tensor_tensor(out=dst[0:d, :], in0=t1[0:d, :], in1=dst[0:d, :], op=ALU.mult)
        else:
            V.tensor_tensor(out=dst[0:d, :], in0=t1[0:d, :], in1=t2[0:d, :], op=ALU.add)
        B()

    def load_vaug(Vb, col0, nch=18, tok0=0):
        src = VT[tok0:tok0 + nch * 128, :].rearrange("(c p) f -> p c f", p=128)
        dq.start(Vb[:, 0:nch, 0:64], src[:, :, col0:col0 + 64] if isinstance(col0, int) else None)

    def attn_core(QRq, KR, dqk, n, kchunks, vfun, scale, oa_bank, E):
        groups = [kchunks[i:i + 4] for i in range(0, len(kchunks), 4)]
        npv = len(kchunks)
        pv_i = 0
        prev = None
        for g in groups + [None]:
            if prev is not None:
                for j, c in enumerate(prev):
                    PE.matmul(PS[oa_bank][0:65, 0:n], vfun(c), E[:, j, 0:n], start=(pv_i == 0), stop=(pv_i == npv - 1))
                    pv_i += 1
            if g is not None:
                for j, c in enumerate(g):
                    PE.matmul(PS[j][:, 0:n], KR[0:dqk, c * 128:(c + 1) * 128], QRq, start=True, stop=True)
            B()
            if g is not None:
                for j, c in enumerate(g):
                    A.activation(out=E[:, j, 0:n], in_=PS[j][:, 0:n], func=AF.Exp, scale=scale)
                B()
            prev = g

    def attn_finish(oa_bank, n, OAS, RZ, YO, zbank=6):
        A.activation(out=OAS[0:65, 0:n], in_=PS[oa_bank][0:65, 0:n], func=AF.Copy)
        B()
        PE.matmul(PS[zbank][0:64, 0:n], SELD[0:65, 0:64], OAS[0:65, 0:n], start=True, stop=True)
        B()
        V.reciprocal(out=RZ[0:64, 0:n], in_=PS[zbank][0:64, 0:n])
        B()
        V.tensor_tensor(out=YO[0:64, 0:n], in0=OAS[0:64, 0:n], in1=RZ[0:64, 0:n], op=ALU.mult)
        B()

    ALLK = list(range(18))
    YT64 = YT.rearrange("(a p) t -> a p t", p=64)
    PT64 = PT.rearrange("(a p) t -> a p t", p=64)
    VTc = VT.rearrange("(c p) f -> p c f", p=128)

    def stage_gqa(l):
        with ExitStack() as s2:
            def t(name, shape):
                return s2.enter_context(SBT(name, list(shape)))
            RC = t("q_rc", [64, T]); RS = t("q_rs", [64, T])
            Ab = t("q_a", [64, T]); Bb = t("q_b", [64, T]); t1 = t("q_t1", [64, T]); t2 = t("q_t2", [64, T])
            KR = t("q_kr", [64, T]); QR = t("q_qr", [64, T]); Vb = t("q_v", [128, 18, 65])
            E = t("q_e", [128, 4, 512]); OAS = t("q_oas", [65, 512]); RZ = t("q_rz", [64, 512]); YO = t("q_yo", [64, 512])
            GQ = t("q_g", [64, 4]); YH = t("q_yh", [64, T])
            dq.start(RC[:], rope64_in[0]); dq.start(RS[:], rope64_in[1]); dq.start(GQ[:], gqag_in[l])
            LW()
            bufs = (Ab, Bb, t1, t2)
            for kv in range(2):
                prep(KR, (2048 + kv * 64) // 16, 64, (RC[:], RS[:]), bufs, gains=(GQ[:, 2:3], GQ[:, 3:4]), norm=True)
                dq.start(Vb[:, :, 0:64], VTc[:, :, 512 + kv * 64:512 + kv * 64 + 64])
                V.memset(Vb[:, :, 64:65], 1.0)
                LW()
                for hh in range(2):
                    dq.start(STG[0:64, :], PT64[hh + (1792 + kv * 128) // 64])
                    LW()
                    prep(QR, lambda i: STG.rearrange("(a p) t -> a p t", p=16)[i], 64, (RC[:], RS[:]), bufs, gains=(GQ[:, 0:1], GQ[:, 1:2]), norm=True)
                    QRl = QR[:, NCTX:T]
                    with MyFori(0, 4) as qt:
                        attn_core(QRl[:, bass.ts(qt, 512)], KR, 64, 512, ALLK, lambda c: Vb[:, c, :], 0.125, 4, E)
                        attn_finish(4, 512, OAS, RZ, YO)
                        V.tensor_copy(out=YH[:, NCTX:T][:, bass.ts(qt, 512)], in_=YO[:, :])
                        B()
                    attn_core(QR[:, 0:NCTX], KR, 64, NCTX, [0, 1], lambda c: Vb[:, c, :], 0.125, 4, E)
                    attn_finish(4, NCTX, OAS, RZ, YO)
                    V.tensor_copy(out=YH[:, 0:NCTX], in_=YO[:, 0:NCTX])
                    B()
                    dq.start(YT64[8 + kv * 2 + hh], YH[:])
                    LW()

    def stage_diff(l):
        lam_init = 0.8 - 0.6 * math.exp(-0.3 * l)
        with ExitStack() as s2:
            def t(name, shape):
                return s2.enter_context(SBT(name, list(shape)))
            RC = t("d_rc", [32, T]); RS = t("d_rs", [32, T])
            Ab = t("d_a", [32, T]); Bb = t("d_b", [32, T]); t1 = None; t2 = None
            QK = [t(f"d_qk{i}", [32, T]) for i in range(4)]
            Vb = t("d_v", [128, 18, 65])
            E = t("d_e", [128, 4, 512]); OAS = t("d_oas", [65, 512]); RZ = t("d_rz", [64, 512])
            Y1 = t("d_y1", [64, 512]); Y2 = t("d_y2", [64, 512]); SQ = t("d_sq", [64, 512])
            LM = t("d_lm", [128, 128]); LV = t("d_lv", [128, 4]); YH = t("d_yh", [64, T])
            dq.start(RC[:], rope32_in[0]); dq.start(RS[:], rope32_in[1]); dq.start(LM[:], dlam_in[l])
            LW()
            V.tensor_tensor(out=LM[:, 0:32], in0=LM[:, 0:32], in1=LM[:, 32:64], op=ALU.mult)
            V.tensor_tensor(out=LM[:, 64:96], in0=LM[:, 64:96], in1=LM[:, 96:128], op=ALU.mult)
            B()
            V.reduce_sum(out=LV[:, 0:1], in_=LM[:, 0:32], axis=AX.X)
            V.reduce_sum(out=LV[:, 1:2], in_=LM[:, 64:96], axis=AX.X)
            B()
            A.activation(out=LV[:, 0:2], in_=LV[:, 0:2], func=AF.Exp)
            B()
            V.tensor_tensor(out=LV[:, 2:3], in0=LV[:, 1:2], in1=LV[:, 0:1], op=ALU.subtract)
            B()
            V.tensor_scalar(out=LV[:, 2:3], in0=LV[:, 2:3], scalar1=-lam_init, scalar2=None, op0=ALU.add)
            B()
            bufs = (Ab, Bb, t1, t2)
            sc = 32 ** -0.5
            with MyFori(0, 4) as h:
                dq.start(YT64[h + 11], YH[:])
                dq.start(STG[0:64, :], PT64[h + 36]); dq.start(STG[64:128, :], PT64[h + 40])
                LW()
                prep(QK[0], lambda i: STG.rearrange("(a p) t -> a p t", p=8)[0 + i], 32, (RC[:], RS[:]), bufs)
                prep(QK[1], lambda i: STG.rearrange("(a p) t -> a p t", p=8)[4 + i], 32, (RC[:], RS[:]), bufs)
                prep(QK[2], lambda i: STG.rearrange("(a p) t -> a p t", p=8)[8 + i], 32, (RC[:], RS[:]), bufs)
                prep(QK[3], lambda i: STG.rearrange("(a p) t -> a p t", p=8)[12 + i], 32, (RC[:], RS[:]), bufs)
                VTh = VT[:, 640:896].rearrange("(c p) (h f) -> h p c f", p=128, f=64)
                dq.start(Vb[:, :, 0:64], VTh[h])
                V.memset(Vb[:, :, 64:65], 1.0)
                LW()

                def one_tile(q0sl, n, kch, dst):
                    attn_core(QK[0][:, q0sl] if not callable(q0sl) else q0sl(QK[0]), QK[2], 32, n, kch, lambda c: Vb[:, c, :], sc, 4, E)
                    attn_finish(4, n, OAS, RZ, Y1)
                    attn_core(QK[1][:, q0sl] if not callable(q0sl) else q0sl(QK[1]), QK[3], 32, n, kch, lambda c: Vb[:, c, :], sc, 5, E)
                    attn_finish(5, n, OAS, RZ, Y2)
                    V.scalar_tensor_tensor(out=Y1[:, 0:n], in0=Y2[:, 0:n], scalar=LV[0:64, 2:3], in1=Y1[:, 0:n],
                                           op0=ALU.mult, op1=ALU.add)
                    B()
                    A.activation(out=SQ[:, 0:n], in_=Y1[:, 0:n], func=AF.Square)
                    B()
                    PE.matmul(PS[6][0:64, 0:n], ones[0:64, 0:64], SQ[:, 0:n], start=True, stop=True)
                    B()
                    A.activation(out=SQ[:, 0:n], in_=PS[6][0:64, 0:n], func=AF.Sqrt, bias=epsc[0:64, :], scale=1.0 / 64)
                    B()
                    V.reciprocal(out=SQ[:, 0:n], in_=SQ[:, 0:n])
                    B()
                    V.scalar_tensor_tensor(out=Y2[:, 0:n], in0=Y1[:, 0:n], scalar=DSUB[:, l:l + 1], in1=SQ[:, 0:n],
                                           op0=ALU.mult, op1=ALU.mult)
                    B()
                    A.activation(out=Y2[:, 0:n], in_=Y2[:, 0:n], func=AF.Copy, scale=(1.0 - lam_init))
                    B()
                    V.tensor_copy(out=dst, in_=Y2[:, 0:n])
                    B()

                with MyFori(0, 4) as qt:
                    one_tile(lambda Q: Q[:, NCTX:T][:, bass.ts(qt, 512)], 512, ALLK,
                             YH[:, NCTX:T][:, bass.ts(qt, 512)])
                one_tile(slice(0, NCTX), NCTX, [0, 1], YH[:, 0:NCTX])
            dq.start(YT64[15], YH[:])
            LW()

    def stage_na(l):
        with ExitStack() as s2:
            def t(name, shape):
                return s2.enter_context(SBT(name, list(shape)))
            NAB = t("a_nab", [128, 4, 14, 64]); NABh = t("a_nabh", [128, 14, 64]); MSK = t("a_msk", [128, 64])
            QT = t("a_q", [64, T]); KT = t("a_k", [64, T])
            Ve = t("a_ve", [128, 18, 65]); Vo = t("a_vo", [128, 17, 65])
            E = t("a_e", [128, 6, 64]); OAll = t("a_oall", [65, L])
            E4 = t("a_e4", [128, 4, 512])
            OAS = t("a_oas", [65, 512]); RZ = t("a_rz", [64, 512]); YO = t("a_yo", [64, 512])
            dq.start(NAB[:], nab_in[l]); dq.start(MSK[:], namask_in)
            LW()
            for hh in range(4):
                for dd in range(14):
                    V.tensor_tensor(out=NAB[:, hh, dd, :], in0=NAB[:, hh, dd, :], in1=MSK[:, :], op=ALU.add)
            B()
            VT_e = VT[:, 0:256].rearrange("(c p) (h f) -> h p c f", p=128, f=64)
            VT_o = VT[64:64 + 17 * 128, 0:256].rearrange("(c p) (h f) -> h p c f", p=128, f=64)
            with MyFori(0, 4) as h:
                dq.start(QT[:], PT64[h]); dq.start(KT[:], PT64[4 + h])
                dq.start(Ve[:, :, 0:64], VT_e[h]); dq.start(Vo[:, :, 0:64], VT_o[h])
                V.memset(Ve[:, :, 64:65], 1.0)
                V.memset(Vo[:, :, 64:65], 1.0)
                V.tensor_copy(out=NABh[:], in_=NAB[:, h, :, :])
                LW()

                def row(qcols, r0tok, off, vsel, slot):
                    for j in range(4):
                        PE.matmul(PS[0][:, j * 64:(j + 1) * 64], r0tok(j), qcols, start=True, stop=True)
                    for j in range(2):
                        PE.matmul(PS[0][:, (4 + j) * 64:(5 + j) * 64], KT[:, j * 128:(j + 1) * 128], qcols, start=True, stop=True)
                    B()
                    b0 = 7 - off
                    A.activation(out=E[:, 0:4, :], in_=PS[0][:, 0:256].rearrange("p (j c) -> p j c", c=64), func=AF.Copy, scale=0.125)
                    A.activation(out=E[:, 4:6, :], in_=PS[0][:, 256:384].rearrange("p (j c) -> p j c", c=64), func=AF.Exp, scale=0.125)
                    B()
                    for j in range(4):
                        V.tensor_tensor(out=E[:, j, :], in0=E[:, j, :], in1=NABh[:, b0 + 2 * j, :], op=ALU.add)
                    B()
                    A.activation(out=E[:, 0:4, :], in_=E[:, 0:4, :], func=AF.Exp)
                    B()
                    for j in range(6):
                        lhs = vsel(j) if j < 4 else Ve[:, j - 4, :]
                        PE.matmul(PS[1][0:65, slot * 64:(slot + 1) * 64], lhs, E[:, j, :], start=(j == 0), stop=(j == 5))
                    B()

                def static_row(r):
                    r0 = min(max(r - 4, 0), 24)
                    off = r - r0
                    if r0 % 2 == 0:
                        vs = lambda j: Ve[:, 2 + r0 // 2 + j, :]
                    else:
                        vs = lambda j: Vo[:, (r0 + 3) // 2 + j, :]
                    row(QT[:, NCTX + r * 64:NCTX + (r + 1) * 64],
                        lambda j: KT[:, NCTX + (r0 + 2 * j) * 64:NCTX + (r0 + 2 * j) * 64 + 128], off, vs, r % 2)
                    A.activation(out=OAll[:, r * 64:(r + 1) * 64], in_=PS[1][0:65, (r % 2) * 64:(r % 2 + 1) * 64], func=AF.Copy)

                for r in range(32):
                    static_row(r)
                B()
                for qt in range(4):
                    PE.matmul(PS[2 + qt][0:64, 0:512], SELD[0:65, 0:64], OAll[0:65, qt * 512:(qt + 1) * 512], start=True, stop=True)
                B()
                for qt in range(4):
                    V.reciprocal(out=E4[0:64, qt, :], in_=PS[2 + qt][0:64, 0:512])
                B()
                for qt in range(4):
                    V.tensor_tensor(out=OAll[0:64, qt * 512:(qt + 1) * 512], in0=OAll[0:64, qt * 512:(qt + 1) * 512],
                                    in1=E4[0:64, qt, :], op=ALU.mult)
                B()
                dq.start(YT64[h][:, NCTX:T], OAll[0:64, :])
                LW()
                attn_core(QT[:, 0:NCTX], KT, 64, NCTX, [0, 1], lambda c: Ve[:, c, :], 0.125, 4, E4)
                attn_finish(4, NCTX, OAS, RZ, YO)
                dq.start(YT64[h][:, 0:NCTX], YO[:, 0:NCTX])
                LW()

    def stage_ret(l):
        with ExitStack() as s2:
            def t(name, shape):
                return s2.enter_context(SBT(name, list(shape)))
            QR = t("r_qr", [64, 4, T]); KR = t("r_kr", [64, 4, T])
            RP = t("r_rp", [128, 8]); LG = t("r_lg", [128, 8])
            INTRA = t("r_intra", [128, 8, 128]); QDEC = t("r_qdec", [64, 8, 128]); KDEC = t("r_kdec", [128, 8, 64])
            CDv = t("r_cd", [64, 8]); KD1 = t("r_kd1", [128, 8])
            ST = t("r_st", [64, 8, 64]); Qd = t("r_qd", [64, 8, 128]); Am = t("r_am", [128, 8, 128]); Kd = t("r_kdm", [128, 8, 64])
            s_tab = ExitStack()
            RCn = s_tab.enter_context(SBT("r_rcn", [128, 4, 128])); RPOS = s_tab.enter_context(SBT("r_rpos", [128, 4, 128]))
            dq.start(RP[:], retp_in[l])
            dq.start(RCn[:], retc_in); dq.start(RPOS[:], retpos_in)
            LW()
            A.activation(out=LG[:], in_=RP[:], func=AF.Exp)
            B()
            V.tensor_scalar(out=LG[:], in0=LG[:], scalar1=-1.0, scalar2=1.0, op0=ALU.mult, op1=ALU.add)
            B()
            A.activation(out=LG[:], in_=LG[:], func=AF.Ln)
            B()
            for hd in range(8):
                dr = hd // 4
                A.activation(out=INTRA[:, hd, :], in_=RCn[:, 2 * dr, :], func=AF.Exp, scale=LG[:, hd:hd + 1])
                A.activation(out=QDEC[:, hd, :], in_=RPOS[0:64, dr, :], func=AF.Exp, scale=LG[0:64, hd:hd + 1])
                A.activation(out=KD1[:, hd:hd + 1], in_=RPOS[:, 2 + dr, 0:1], func=AF.Exp, scale=LG[:, hd:hd + 1])
            A.activation(out=CDv[:], in_=LG[0:64, :], func=AF.Exp, scale=128.0)
            B()
            for hd in range(8):
                dr = hd // 4
                V.scalar_tensor_tensor(out=INTRA[:, hd, :], in0=INTRA[:, hd, :], scalar=0.125, in1=RCn[:, 2 * dr + 1, :], op0=ALU.mult, op1=ALU.mult)
                V.tensor_scalar(out=KDEC[:, hd, :], in0=ones[:, 0:64], scalar1=KD1[:, hd:hd + 1], scalar2=0.125,
                                op0=ALU.mult, op1=ALU.mult)
            V.memset(ST[:], 0.0)
            B()
            s_tab.close()
            with ExitStack() as sp:
                def tp(name, shape):
                    return sp.enter_context(SBT(name, list(shape)))
                RC = tp("r_rc", [64, T]); RS = tp("r_rs", [64, T])
                Ab = tp("r_a", [64, T]); Bb = tp("r_b", [64, T]); t1 = None; t2 = None
                dq.start(RC[:], rope64_in[0]); dq.start(RS[:], rope64_in[1])
                LW()
                bufs = (Ab, Bb, t1, t2)
                for hh in range(4):
                    prep(QR[:, hh, :], (768 + hh * 64) // 16, 64, (RC[:], RS[:]), bufs)
                    prep(KR[:, hh, :], (1024 + hh * 64) // 16, 64, (RC[:], RS[:]), bufs)

            Vs = t("r_vs", [128, 2, 256])
            OAcc = t("r_oacc", [64, 4, T])
            V.memset(OAcc[:], 0.0)
            B()

            def step(cf, cb):
                csl = [slice(cf * 128, (cf + 1) * 128), slice(cb * 128, (cb + 1) * 128)]
                cidx = [cf, cb]
                dq.start(Vs[:, 0, :], VTc[:, cf, 256:512]); dq.start(Vs[:, 1, :], VTc[:, cb, 256:512])
                LW()
                for hd in range(8):
                    dr, hh = hd // 4, hd % 4
                    PE.matmul(PS[dr][:, hh * 128:(hh + 1) * 128], KR[:, hh, csl[dr]], QR[:, hh, csl[dr]], start=True, stop=True)
                    PE.matmul(PS[2][:, hd * 64:(hd + 1) * 64], KR[:, hh, csl[dr]], ident[0:64, 0:64], start=True, stop=True)
                for dr in range(2):
                    V.tensor_tensor(out=Qd[:, dr * 4:(dr + 1) * 4, :], in0=QR[:, :, csl[dr]], in1=QDEC[:, dr * 4:(dr + 1) * 4, :], op=ALU.mult)
                B()
                for dr in range(2):
                    V.tensor_tensor(out=Am[:, dr * 4:(dr + 1) * 4, :], in0=PS[dr][:, :].rearrange("p (h c) -> p h c", c=128),
                                    in1=INTRA[:, dr * 4:(dr + 1) * 4, :], op=ALU.mult)
                V.tensor_tensor(out=Kd[:], in0=PS[2][:, :].rearrange("p (h c) -> p h c", c=64), in1=KDEC[:], op=ALU.mult)
                B()
                for hd in range(8):
                    dr, hh = hd // 4, hd % 4
                    vch = Vs[:, dr, hh * 64:(hh + 1) * 64]
                    ob = PS[3 + dr][0:64, hh * 128:(hh + 1) * 128]
                    PE.matmul(ob, vch, Am[:, hd, :], start=True, stop=True)
                    PE.matmul(PS[6 + dr][0:64, hh * 128:(hh + 1) * 128], ST[:, hd, :], Qd[:, hd, :], start=True, stop=True)
                    PE.matmul(PS[5][0:64, hd * 64:(hd + 1) * 64], Kd[:, hd, :], vch, start=True, stop=True)
                B()
                for hd in range(8):
                    V.scalar_tensor_tensor(out=ST[:, hd, :], in0=ST[:, hd, :], scalar=CDv[:, hd:hd + 1],
                                           in1=PS[5][0:64, hd * 64:(hd + 1) * 64], op0=ALU.mult, op1=ALU.add)
                for dr in range(2):
                    V.tensor_tensor(out=OAcc[:, :, csl[dr]], in0=OAcc[:, :, csl[dr]],
                                    in1=PS[3 + dr][0:64, :].rearrange("p (h c) -> p h c", c=128), op=ALU.add)
                B()
                for dr in range(2):
                    V.tensor_tensor(out=OAcc[:, :, csl[dr]], in0=OAcc[:, :, csl[dr]],
                                    in1=PS[6 + dr][0:64, :].rearrange("p (h c) -> p h c", c=128), op=ALU.add)
                B()

            if not os.environ.get("RET_SKIPSTEPS"):
                step(0, 1)
                step(1, 0)
                for j in range(16):
                    step(j + 2, 17 - j)
            with ExitStack() as s3:
                G = s3.enter_context(SBT("r_g", [64, 512]))
                SQ = s3.enter_context(SBT("r_sq", [64, 512]))
                CEN = s3.enter_context(SBT("r_cen", [64, 512]))
                MSQ = s3.enter_context(SBT("r_msq", [64, 512]))
                for hh in range(4):
                    for ti, (t0, n) in enumerate(TILES):
                        O = OAcc[:, hh, t0:t0 + n]
                        dq.start(G[:, 0:n], PT64[(1536 // 64) + hh][:, t0:t0 + n])
                        A.activation(out=SQ[:, 0:n], in_=O, func=AF.Square)
                        LW()
                        A.activation(out=G[:, 0:n], in_=G[:, 0:n], func=AF.Silu)
                        PE.matmul(PS[0][0:64, 0:n], ones[0:64, 0:64], O, start=True, stop=True)
                        PE.matmul(PS[1][0:64, 0:n], ones[0:64, 0:64], SQ[:, 0:n], start=True, stop=True)
                        B()
                        A.activation(out=CEN[:, 0:n], in_=PS[0][0:64, 0:n], func=AF.Copy, scale=1.0 / 64)
                        A.activation(out=SQ[:, 0:n], in_=PS[1][0:64, 0:n], func=AF.Copy, scale=1.0 / 64)
                        B()
                        A.activation(out=MSQ[:, 0:n], in_=CEN[:, 0:n], func=AF.Square)
                        B()
                        V.tensor_tensor(out=CEN[:, 0:n], in0=O, in1=CEN[:, 0:n], op=ALU.subtract)
                        V.tensor_tensor(out=MSQ[:, 0:n], in0=SQ[:, 0:n], in1=MSQ[:, 0:n], op=ALU.subtract)
                        B()
                        A.activation(out=MSQ[:, 0:n], in_=MSQ[:, 0:n], func=AF.Sqrt, bias=epsc[0:64, :], scale=1.0)
                        B()
                        V.reciprocal(out=MSQ[:, 0:n], in_=MSQ[:, 0:n])
                        B()
                        V.tensor_tensor(out=CEN[:, 0:n], in0=CEN[:, 0:n], in1=MSQ[:, 0:n], op=ALU.mult)
                        B()
                        V.tensor_tensor(out=G[:, 0:n], in0=CEN[:, 0:n], in1=G[:, 0:n], op=ALU.mult)
                        B()
                        dq.start(YT64[4 + hh][:, t0:t0 + n], G[:, 0:n])
                        LW()

    def stage_merge(l):
        wb = w_branch[l].rearrange("n (h p) (m j) -> p n h m j", p=64, j=128)
        GTv = PT[3072:7168, :].rearrange("(n m p) t -> m p n t", n=4, p=128)
        YTv = YT.rearrange("(a p) t -> p a t", p=64)
        ATv = AT.rearrange("(m p) t -> m p t", p=128)
        with ExitStack() as s2:
            def t(name, shape):
                return s2.enter_context(SBT(name, list(shape)))
            Y = t("m_y", [64, 16, 512]); W = t("m_w", [64, 4, 4, 128]); Gt = t("m_g", [128, 4, 512])
            Tm = t("m_t", [128, 4, 512])
            for ti, (t0, n) in enumerate(TILES):
                dq.start(Y[:, :, 0:n], YTv[:, :, t0:t0 + n])
                LW()
                with MyFori(0, 8) as m:
                    for nb in range(4):
                        dq.start(W[:, nb, :, :], wb[:, nb, :, m, :])
                    dq.start(Gt[:, :, 0:n], GTv[m][:, :, t0:t0 + n])
                    LW()
                    for nb in range(4):
                        for hh in range(4):
                            PE.matmul(PS[nb][:, 0:n], W[:, nb, hh, :], Y[:, nb * 4 + hh, 0:n], start=(hh == 0), stop=(hh == 3))
                    B()
                    for nb in range(4):
                        V.tensor_tensor(out=Tm[:, nb, 0:n], in0=PS[nb][:, 0:n], in1=Gt[:, nb, 0:n], op=ALU.mult)
                    B()
                    V.tensor_tensor(out=Tm[:, 0, 0:n], in0=Tm[:, 0, 0:n], in1=Tm[:, 1, 0:n], op=ALU.add)
                    V.tensor_tensor(out=Tm[:, 2, 0:n], in0=Tm[:, 2, 0:n], in1=Tm[:, 3, 0:n], op=ALU.add)
                    B()
                    V.tensor_tensor(out=Tm[:, 0, 0:n], in0=Tm[:, 0, 0:n], in1=Tm[:, 2, 0:n], op=ALU.add)
                    B()
                    dq.start(ATv[m][:, t0:t0 + n], Tm[:, 0, 0:n])
                    LW()

    def stage_moe(l):
        XSv = XS.rearrange("(k p) t -> p k t", p=128)
        with ExitStack() as s2:
            def t(name, shape):
                return s2.enter_context(SBT(name, list(shape)))
            WT = t("e_wt", [16, T])
            with ExitStack() as s3:
                BIG = s3.enter_context(SBT("e_big", [128, 8, T]))
                stage_norm(BIG, lambda v, k: GG[:, v, 1, l, k:k + 1], lambda v, k: MODS[:, v, l, 24 + k:25 + k])
                dq.start(MTD.rearrange("(k p) t -> p k t", p=128), BIG[:])
                LW()
                WR = s3.enter_context(SBT("e_wr", [128, 8, 16]))
                RB = s3.enter_context(SBT("e_rb", [128, 16]))
                S = s3.enter_context(SBT("e_s", [128, 18, 16]))
                SEL = s3.enter_context(SBT("e_sel", [128, 18, 16]))
                SEL2 = s3.enter_context(SBT("e_sel2", [128, 18, 16]))
                EM = s3.enter_context(SBT("e_em", [128, 18, 16]))
                M1 = s3.enter_context(SBT("e_m1", [128, 72]))
                M2 = s3.enter_context(SBT("e_m2", [128, 72]))
                GS = s3.enter_context(SBT("e_gs", [128, 72]))
                GM = s3.enter_context(SBT("e_gm", [128, 72]))
                GX = s3.enter_context(SBT("e_gx", [128, 18]))
                dq.start(WR[:], w_router.rearrange("(k p) e -> p k e", p=128))
                dq.start(RB[:], rb_in)
                LW()
                for tc in range(18):
                    for k in range(8):
                        PE.matmul(PS[0][:, tc * 16:(tc + 1) * 16], BIG[:, k, tc * 128:(tc + 1) * 128], WR[:, k, :], start=(k == 0), stop=(k == 7))
                B()
                A.activation(out=S[:].rearrange("p c e -> p (c e)"), in_=PS[0][:, 0:288], func=AF.Sigmoid)
                B()
                for c in range(18):
                    V.tensor_tensor(out=SEL[:, c, :], in0=S[:, c, :], in1=RB[:], op=ALU.add)
                B()
                sel4 = SEL[:].rearrange("p c (g i) -> p (c g) i", i=4)
                sel24 = SEL2[:].rearrange("p c (g i) -> p (c g) i", i=4)
                em4 = EM[:].rearrange("p c (g i) -> p (c g) i", i=4)
                gs3 = GS[:].rearrange("p (c g) -> p c g", g=4)
                gm3 = GM[:].rearrange("p (c g) -> p c g", g=4)
                V.reduce_max(out=M1[:], in_=sel4, axis=AX.X)
                B()
                for i in range(4):
                    V.tensor_tensor(out=em4[:, :, i], in0=sel4[:, :, i], in1=M1[:, :], op=ALU.is_equal)
                B()
                V.tensor_scalar(out=sel24, in0=em4, scalar1=-1.0e9, scalar2=None, op0=ALU.mult)
                B()
                V.tensor_tensor(out=sel24, in0=sel24, in1=sel4, op=ALU.add)
                B()
                V.reduce_max(out=M2[:], in_=sel24, axis=AX.X)
                B()
                V.tensor_tensor(out=GS[:], in0=M1[:], in1=M2[:], op=ALU.add)
                B()
                V.reduce_max(out=GX[:], in_=gs3, axis=AX.X)
                B()
                for g in range(4):
                    V.tensor_tensor(out=gm3[:, :, g], in0=gs3[:, :, g], in1=GX[:, :], op=ALU.is_equal)
                for i in range(4):
                    V.tensor_tensor(out=em4[:, :, i], in0=sel4[:, :, i], in1=M2[:, :], op=ALU.is_ge)
                B()
                for i in range(4):
                    V.tensor_tensor(out=em4[:, :, i], in0=em4[:, :, i], in1=GM[:, :], op=ALU.mult)
                B()
                V.tensor_tensor(out=EM[:], in0=EM[:], in1=S[:], op=ALU.mult)
                B()
                V.reduce_sum(out=GX[:], in_=EM[:], axis=AX.X)
                B()
                V.reciprocal(out=GX[:], in_=GX[:])
                B()
                for ee in range(16):
                    V.tensor_tensor(out=EM[:, :, ee], in0=EM[:, :, ee], in1=GX[:, :], op=ALU.mult)
                B()
                for c in range(18):
                    PE.matmul(PS[c // 4][0:16, (c % 4) * 128:(c % 4 + 1) * 128], EM[:, c, :], ident[:, :], start=True, stop=True)
                B()
                for c in range(18):
                    A.activation(out=WT[:, c * 128:(c + 1) * 128], in_=PS[c // 4][0:16, (c % 4) * 128:(c % 4 + 1) * 128], func=AF.Copy)
                B()
            with ExitStack() as s3:
                def t3(name, shape):
                    return s3.enter_context(SBT(name, list(shape)))
                WG = t3("e_wg", [128, 8, 512]); WU = t3("e_wu", [128, 8, 512]); WD = t3("e_wd", [128, 4, D])
                SELS = t3("e_sels", [16, 128]); WBC = t3("e_wbc", [128, T])
                MTt = t3("e_mt", [128, 8, 512]); SG = t3("e_sg", [128, 4, 512]); HID = SG; WBCt = t3("e_wbct", [128, 512]); ACCt = t3("e_acct", [128, 8, 512])
                ATk = MTD.rearrange("(k p) t -> p k t", p=128)
                ACCk = ACCD.rearrange("(k p) t -> p k t", p=128)
                V.memset(ACCt[:], 0.0)
                B()
                for ti, (t0, n) in enumerate(TILES):
                    dq.start(ACCk[:, :, t0:t0 + n], ACCt[:, :, 0:n])
                LW()
                wgv = w_gate[l].rearrange("e (k p) f -> e p k f", p=128)
                wuv = w_up[l].rearrange("e (k p) f -> e p k f", p=128)
                wdv = w_down[l].rearrange("e (c p) f -> e p c f", p=128)

                def tile_body(sl, n):
                    dq.start(MTt[:, :, 0:n], sl(ATk))
                    V.tensor_copy(out=WBCt[:, 0:n], in_=sl(WBC))
                    dq.start(ACCt[:, :, 0:n], sl(ACCk))
                    LW()
                    for hc in range(4):
                        for k in range(8):
                            PE.matmul(PS[hc][:, 0:n], WG[:, k, hc * 128:(hc + 1) * 128], MTt[:, k, 0:n], start=(k == 0), stop=(k == 7))
                        for k in range(8):
                            PE.matmul(PS[4 + hc][:, 0:n], WU[:, k, hc * 128:(hc + 1) * 128], MTt[:, k, 0:n], start=(k == 0), stop=(k == 7))
                    B()
                    for hc in range(4):
                        A.activation(out=SG[:, hc, 0:n], in_=PS[hc][:, 0:n], func=AF.Silu)
                    B()
                    for hc in range(4):
                        V.tensor_tensor(out=HID[:, hc, 0:n], in0=PS[4 + hc][:, 0:n], in1=SG[:, hc, 0:n], op=ALU.mult)
                    B()
                    for hc in range(4):
                        V.tensor_tensor(out=HID[:, hc, 0:n], in0=HID[:, hc, 0:n], in1=WBCt[:, 0:n], op=ALU.mult)
                    B()
                    for m in range(8):
                        for hc in range(4):
                            PE.matmul(PS[m][:, 0:n], WD[:, hc, m * 128:(m + 1) * 128], HID[:, hc, 0:n], start=(hc == 0), stop=(hc == 3))
                    B()
                    for m in range(8):
                        V.tensor_tensor(out=ACCt[:, m, 0:n], in0=ACCt[:, m, 0:n], in1=PS[m][:, 0:n], op=ALU.add)
                    B()
                    dq.start(sl(ACCk), ACCt[:, :, 0:n])
                    LW()

                def sl_ctx(ap):
                    return ap[:, :, 0:NCTX] if len(ap.shape) == 3 else ap[:, 0:NCTX]

                with MyFori(0, 16) as e:
                    dq.start(WG[:], wgv[e]); dq.start(WU[:], wuv[e]); dq.start(WD[:], wdv[e])
                    dq.start(SELS[:], sel16_in[:, e, :])
                    LW()
                    for ti, (t0, n) in enumerate(TILES):
                        PE.matmul(PS[ti][:, 0:n], SELS[:, :], WT[:, t0:t0 + n], start=True, stop=True)
                    LW()
                    for ti, (t0, n) in enumerate(TILES):
                        A.activation(out=WBC[:, t0:t0 + n], in_=PS[ti][:, 0:n], func=AF.Copy)
                    B()
                    tile_body(sl_ctx, NCTX)
                    with MyFori(0, 4) as qt:
                        def sl_lat(ap):
                            if len(ap.shape) == 3:
                                return ap[:, :, NCTX:T][:, :, bass.ts(qt, 512)]
                            return ap[:, NCTX:T][:, bass.ts(qt, 512)]
                        tile_body(sl_lat, 512)

            with SBT("e_xs", [128, 8, 512]) as xs, SBT("e_ac", [128, 8, 512]) as ac:
                ACCk = ACCD.rearrange("(k p) t -> p k t", p=128)
                for ti, (t0, n) in enumerate(TILES):
                    v = 0 if ti == 0 else 1
                    dq.start(xs[:, :, 0:n], XSv[:, :, t0:t0 + n])
                    dq.start(ac[:, :, 0:n], ACCk[:, :, t0:t0 + n])
                    LW()
                    for k in range(8):
                        V.scalar_tensor_tensor(out=xs[:, k, 0:n], in0=ac[:, k, 0:n], scalar=MODS[:, v, l, 40 + k:41 + k],
                                               in1=xs[:, k, 0:n], op0=ALU.mult, op1=ALU.add)
                    B()
                    dq.start(XSv[:, :, t0:t0 + n], xs[:, :, 0:n])
                    LW()


    STAGES = ["norm1", "inproj", "vtok", "diff", "gqa", "ret", "na", "merge", "outproj", "moe"]

    def enabled(name):
        return stop_after is None or STAGES.index(name) <= STAGES.index(stop_after)

    from contextlib import nullcontext as _nullctx

    class _Const:
        def __enter__(self):
            return 0

        def __exit__(self, *a):
            return False

    with (MyFori(0, nsamp) if nsamp > 1 else _Const()) as s:
        dq.start(out[s], OUTS)
        dq.start(XS, xin[s])
        V.tensor_copy(out=MODS[:, 1, :, :], in_=MOD[:, :, :, s])
        V.tensor_copy(out=MODS[:, 0, :, :], in_=MOD[:, :, :, 4])
        LW()
        for v in range(2):
            for nrm in range(2):
                V.scalar_tensor_tensor(out=GG[:, v, nrm, :, :], in0=MODS[:, v, :, 8 + 24 * nrm:16 + 24 * nrm], scalar=1.0,
                                       in1=GN[:, 2 * nrm:2 * nrm + 2, :], op0=ALU.add, op1=ALU.mult)
        B()
        for l in range(nlayers):
            with SBT("BIG", [128, 8, T]) as BIG:
                if enabled("norm1"):
                    stage_norm(BIG, lambda v, k: GG[:, v, 0, l, k:k + 1], lambda v, k: MODS[:, v, l, k:k + 1])
                    if dbg and "HT" in dbg:
                        dq.start(dbg_t["HT"].rearrange("(k p) t -> p k t", p=128), BIG[:])
                        LW()
                wv = w_in[l].rearrange("(k p) (m j) -> p k m j", p=128, j=128)
                if enabled("inproj"):
                    stage_gemm(BIG, wv, 0, 24, 0, PT, ("copy",))
                    stage_gemm(BIG, wv, 24, 56, 0, PT, ("sigmoid",))
                if enabled("vtok"):
                    stage_vtok(BIG, l)
            if enabled("diff"):
                stage_diff(l)
            if enabled("gqa"):
                stage_gqa(l)
            if enabled("ret"):
                stage_ret(l)
            if enabled("na"):
                stage_na(l)
            if enabled("merge"):
                stage_merge(l)
            if enabled("outproj"):
                with SBT("BIG2", [128, 8, T]) as BIG:
                    dq.start(BIG[:], AT.rearrange("(k p) t -> p k t", p=128))
                    LW()
                    wo = w_out[l].rearrange("(k p) (m j) -> p k m j", p=128, j=128)
                    stage_gemm(BIG, wo, 0, 8, 0, XS, ("resid", lambda v, m: MODS[:, v, l, 16:24][:, bass.ts(m, 1)]))
            if dbg and "X1" in dbg:
                dq.start(dbg_t["X1"], XS)
                LW()
            if enabled("moe"):
                stage_moe(l)
        with SBT("BIG3", [128, 8, T]) as BIG:
            stage_norm(BIG, lambda v, k: GN[:, 4, k:k + 1], lambda v, k: ZERO8[:, k:k + 1])
            dq.start(OUTS.rearrange("(k p) t -> p k t", p=128), BIG[:, :, NCTX:T])
            LW()
    dq.start(out[nsamp], OUTS)
    LW()
    es.close()
    if os.environ.get('SBUF_PEAK'):
        print('SBUF peaks', {k: v // 1024 for k, v in _peak.items()})
    return nc


def _consts():
    c = {}
    c["ident"] = np.eye(128, dtype=np.float32)
    seld = np.zeros((65, 64), np.float32); seld[64, :] = 1.0
    c["seld"] = seld
    sel16 = np.zeros((16, 16, 128), np.float32)
    for e in range(16):
        sel16[e, e, :] = 1.0
    c["sel16"] = sel16
    k = np.arange(128)[:, None].astype(np.float32); q = np.arange(128)[None, :].astype(np.float32)
    retc = np.zeros((128, 4, 128), np.float32)
    retc[:, 0, :] = np.maximum(q - k, 0); retc[:, 1, :] = (q >= k)
    retc[:, 2, :] = np.maximum(k - q, 0); retc[:, 3, :] = (k >= q)
    c["retc"] = retc
    rp = np.zeros((128, 4, 128), np.float32)
    t = np.arange(128, dtype=np.float32)
    rp[:, 0, :] = (t + 1.0)[None, :]; rp[:, 1, :] = (128.0 - t)[None, :]
    rp[:, 2, 0] = 127.0 - t; rp[:, 3, 0] = t
    c["retpos"] = rp

    def rope_tab(d):
        half = d // 2; nf = half // 2
        inv = (1.0 / (10000.0 ** (np.arange(nf, dtype=np.float32) / nf))).astype(np.float32)
        tt = np.arange(L, dtype=np.int32)
        rows = (tt // 64).astype(np.float32); cols = (tt % 64).astype(np.float32)
        C = np.ones((d, T), np.float32); S = np.zeros((d, T), np.float32)
        for blk, pos in ((0, rows), (1, cols)):
            ang = (pos[None, :] * inv[:, None]).astype(np.float32)
            cs, sn = np.cos(ang).astype(np.float32), np.sin(ang).astype(np.float32)
            b0 = blk * half
            C[b0:b0 + nf, NCTX:] = cs; C[b0 + nf:b0 + half, NCTX:] = cs
            S[b0:b0 + nf, NCTX:] = -sn; S[b0 + nf:b0 + half, NCTX:] = sn
        return np.stack([C, S])
    c["rope64"] = rope_tab(64)
    c["rope32"] = rope_tab(32)
    cc = np.arange(64)[:, None]; kc = np.arange(64)[None, :]
    ws = np.clip(cc - 8, 0, 48)
    ok = (kc >= ws) & (kc < ws + 16)
    idx = np.clip(kc - cc, -15, 15) + 15
    msk = np.where(ok, 0.0, NEGM).astype(np.float32)
    c["namask"] = np.ascontiguousarray(np.concatenate([msk.T, msk.T], axis=0))
    c["_naidx"] = idx
    lamc = np.zeros((128, 2, 2), np.float32)
    c["lamc"] = lamc
    return c


_CACHE = {}


def _prep_inputs(inp, nsamp_per_core, ncores):
    c = _consts()
    f = lambda a: np.ascontiguousarray(a, dtype=np.float32)
    shared = {}
    shared["w_mod"] = f(inp["w_mod"]); shared["w_in"] = f(inp["w_in"]); shared["w_branch"] = f(inp["w_branch"])
    shared["w_out"] = f(inp["w_out"]); shared["w_router"] = f(inp["w_router"])
    shared["w_gate"] = f(inp["w_gate_e"]); shared["w_up"] = f(inp["w_up_e"]); shared["w_down"] = f(inp["w_down_e"])
    shared["bmod"] = f(inp["b_mod"].reshape(2, 48, 128).transpose(2, 0, 1))
    gn = np.stack([inp["g_norm1"][0], inp["g_norm1"][1], inp["g_norm2"][0], inp["g_norm2"][1], inp["g_final"]])
    shared["gn"] = f(gn.reshape(5, 8, 128).transpose(2, 0, 1))
    idx = c["_naidx"]
    rpb = inp["na_rpb"]
    g = rpb[:, :, :, idx]
    nab = np.zeros((2, 128, 4, 14, 64), np.float32)
    for jj in range(2):
        nab[:, jj * 64:(jj + 1) * 64] = g[:, :, jj:jj + 14].transpose(0, 4, 1, 2, 3)
    shared["nab"] = f(nab)
    shared["namask"] = c["namask"]
    shared["retp"] = f(np.broadcast_to(inp["ret_log_decay"].reshape(2, 1, 8), (2, 128, 8)))
    perm = np.concatenate([np.arange(16, 32), np.arange(0, 16), np.arange(48, 64), np.arange(32, 48)])
    qg, kg = inp["gqa_q_gain"], inp["gqa_k_gain"]
    shared["gqag"] = f(np.stack([qg, qg[:, perm], kg, kg[:, perm]], axis=-1))
    shared["dlam"] = f(np.broadcast_to(inp["diff_lambda"].reshape(2, 1, 128), (2, 128, 128)))
    shared["dsub"] = f(inp["diff_subln"].T)
    shared["lamc"] = c["lamc"]
    shared["rb"] = f(np.broadcast_to(inp["router_bias"][None, :], (128, 16)))
    for k in ("rope64", "rope32", "ident", "seld", "sel16", "retc", "retpos"):
        shared[k] = c[k]
    maps = []
    for ci in range(ncores):
        b0 = ci * nsamp_per_core
        bs = slice(b0, b0 + nsamp_per_core)
        xin = np.concatenate([inp["ctx"][bs].transpose(0, 2, 1), inp["x"][bs].transpose(0, 2, 1)], axis=2)
        cc = np.concatenate([inp["c"][bs], np.zeros((4 - nsamp_per_core, D), np.float32), inp["c_ctx"][None, :]], axis=0)
        m = dict(shared)
        m["xin"] = f(xin)
        m["cT"] = f(cc.T.reshape(8, 128, 5).transpose(1, 0, 2))
        maps.append(m)
    return maps


def kernel(**inputs):
    inp = {k: np.asarray(v) for k, v in inputs.items()}
    ncores = 8
    nb = inp["x"].shape[0] // ncores
    if "nc" not in _CACHE:
        _CACHE["nc"] = build(nsamp=nb, nlayers=DEPTH)
    nc = _CACHE["nc"]
    maps = _prep_inputs(inp, nb, ncores)
    res = run_bass_kernel_spmd(nc, maps, core_ids=list(range(ncores)))
    outs = [r["out"][1:] for r in res.results]
    o = np.concatenate(outs, axis=0).transpose(0, 2, 1)
    return np.ascontiguousarray(o, dtype=np.float32)
```

```python
import math
import numpy as np
import concourse.bass as bass
import concourse.mybir as mybir
from concourse.bass_utils import run_bass_kernel_spmd

F32 = mybir.dt.float32
BF16 = mybir.dt.bfloat16
AF = mybir.ActivationFunctionType
ALU = mybir.AluOpType
AX = mybir.AxisListType

D = 1024
T = 2304
NCTX = 256
L = 2048
EPS = 1e-6
TILES = [(0, 256), (256, 512), (768, 512), (1280, 512), (1792, 512)]
NEGM = -1.0e4
DEPTH = 2
import os
RET_STOP = int(os.environ.get('RET_STOP', '99'))
RET_STEP = int(os.environ.get('RET_STEP', '99'))


class Ctx:
    pass


def build(nsamp=4, nlayers=2, dbg=None, stop_after=None):
    nc = bass.Bass("TRN2", target_bir_lowering=False)
    K = Ctx()

    def din(name, shape):
        return nc.dram_tensor(name, list(shape), F32, kind="ExternalInput").ap()

    xin = din("xin", [nsamp, D, T])
    cT_in = din("cT", [128, 8, 5])
    w_mod = din("w_mod", [2, D, 6 * D])
    bmod_in = din("bmod", [128, 2, 48])
    gn_in = din("gn", [128, 5, 8])
    w_in = din("w_in", [2, D, 7168])
    nab_in = din("nab", [2, 128, 4, 14, 64])
    namask_in = din("namask", [128, 64])
    retp_in = din("retp", [2, 128, 8])
    gqag_in = din("gqag", [2, 64, 4])
    dlam_in = din("dlam", [2, 128, 128])
    dsub_in = din("dsub", [64, 2])
    lamc_in = din("lamc", [128, 2, 2])
    w_branch = din("w_branch", [2, 4, 256, D])
    w_out = din("w_out", [2, D, D])
    w_router = din("w_router", [D, 16])
    rb_in = din("rb", [128, 16])
    w_gate = din("w_gate", [2, 16, D, 512])
    w_up = din("w_up", [2, 16, D, 512])
    w_down = din("w_down", [2, 16, 512, D])
    rope64_in = din("rope64", [2, 64, T])
    rope32_in = din("rope32", [2, 32, T])
    ident_in = din("ident", [128, 128])
    seld_in = din("seld", [65, 64])
    sel16_in = din("sel16", [16, 16, 128])
    retc_in = din("retc", [128, 4, 128])
    retpos_in = din("retpos", [128, 4, 128])

    out = nc.dram_tensor("out", [nsamp + 1, D, L], F32, kind="ExternalOutput").ap()
    OUTS = nc.dram_tensor("OUTS", [D, L], F32, kind="Internal").ap()
    dbg_outs = {}
    dbg_t = dbg_outs

    def dscratch(name, shape):
        kind = "ExternalOutput" if (dbg and name in dbg) else "Internal"
        t = nc.dram_tensor(name, list(shape), F32, kind=kind).ap()
        if kind == "ExternalOutput":
            dbg_outs[name] = t
        return t

    XS = dscratch("XS", [D, T])
    PT = dscratch("PT", [7168, T])
    VT = dscratch("VT", [T, 896])
    YT = dscratch("YT", [1024, T])
    AT = dscratch("AT", [D, T])
    STG = dscratch("STG", [128, T])
    ACCD = dscratch("ACCD", [D, T])
    MTD = dscratch("MTD", [D, T])
    if dbg and "X1" in dbg:
        dscratch("X1", [D, T])
    if dbg and "HT" in dbg:
        dscratch("HT", [D, T])

    from contextlib import ExitStack
    es = ExitStack()
    es.__enter__()

    _cnt = [0]

    _peak = {}

    class _SBTW:
        def __init__(self, name, shape, dt=F32):
            self.name = name
            self.cm = nc.sbuf_tensor(f"sb{_cnt[0]}_{name}", list(shape), dt)

        def __enter__(self):
            r = self.cm.__enter__()
            used = 229376 - nc.sbuf_bytes_remaining
            key = self.name.split("_")[0]
            _peak[key] = max(_peak.get(key, 0), used)
            return r

        def __exit__(self, *a):
            return self.cm.__exit__(*a)

    def SBT(name, shape, dt=F32):
        _cnt[0] += 1
        return _SBTW(name, shape, dt)

    def sb(name, shape):
        return es.enter_context(SBT(name, list(shape)))

    PS = [es.enter_context(nc.psum_tensor(f"ps{i}", [128, 512], F32)) for i in range(8)]
    dsem = es.enter_context(nc.semaphore("dsem"))

    class DQ:
        n = 0

        def start(self, out, in_):
            nc.sync.dma_start(out=out, in_=in_).then_inc(dsem, 16)
            self.n += 1

        def wait(self):
            if self.n:
                nc.sync.wait_ge(dsem, 16 * self.n)
                nc.sync.sem_clear(dsem)
                self.n = 0

    dq = DQ()

    def B():
        nc.all_engine_barrier()

    def LW():
        dq.wait()
        B()

    V = nc.vector
    A = nc.scalar
    PE = nc.tensor

    import re as _re
    from contextlib import contextmanager as _cm
    _RH = bass.RegisterHandle
    _ENGS = {"Pool": mybir.EngineType.Pool, "Activation": mybir.EngineType.Activation, "PE": mybir.EngineType.PE,
             "DVE": mybir.EngineType.DVE, "SP": mybir.EngineType.SP}
    _ALLE = mybir.ALL_ENGINES
    _lc = [0]

    def _cur_id():
        h = nc.vector.alloc_register()
        m = _re.search(r"(\d+)$", h.name)
        nc.vector.free_register(h)
        return int(m.group(1))

    @_cm
    def MyFori(start, end):
        id0 = _cur_id()
        _lc[0] += 1
        name = f"mf{_lc[0]}"
        ls, le = name + "_loop", name + "_end"
        regs = nc.alloc_registers(name + "_i", engines=_ALLE)
        nc.regs_mov(regs, start)
        nc.br(ls, engines=_ALLE)
        with nc.body(ls, valid_engines=_ALLE):
            i = nc.snap(regs, min_val=start, max_val=end - 1)
            yield i
            nc.regs_alu(regs, regs, 1, op=mybir.AluOpType.add)
            nc.br_lt(regs, end, on_true=ls, on_false=le, engines=_ALLE)
        nc.switch_bb(le)
        for h in regs.handles:
            nc.free_register(h)
        if not isinstance(i, int):
            for e in nc.engines.values():
                al = e.get_value_cache().lookup(i)
                if al is not None:
                    nc.free_register(al.val)
        id1 = _cur_id()
        for en, et in _ENGS.items():
            for k in range(id0, id1 + 1):
                nm = f"{en}_tmp_{k}"
                try:
                    r = nc.lookup_reg(nm)
                except Exception:
                    r = None
                if r is not None and getattr(r, "allocated", False):
                    nc.free_register(_RH(name=nm, engine=et))


    ident = sb("ident", [128, 128])
    ones = sb("ones", [128, 128])
    epsc = sb("epsc", [128, 1])
    MOD = sb("MOD", [128, 2, 48, 5])
    MODS = sb("MODS", [128, 2, 2, 48])
    GG = sb("GG", [128, 2, 2, 2, 8])
    GN = sb("GN", [128, 5, 8])
    BMOD = sb("BMOD", [128, 2, 48])
    SELD = sb("SELD", [65, 64])
    ZERO8 = sb("ZERO8", [128, 8])
    LAMC = sb("LAMC", [128, 2, 2])
    DSUB = sb("DSUB", [64, 2])

    dq.start(ident[:], ident_in)
    dq.start(GN[:], gn_in)
    dq.start(BMOD[:], bmod_in)
    dq.start(SELD[:], seld_in)
    dq.start(LAMC[:], lamc_in)
    dq.start(DSUB[:], dsub_in)
    V.memset(ones[:], 1.0)
    V.memset(epsc[:], EPS)
    V.memset(ZERO8[:], 0.0)
    LW()

    def stage_mods():
        with SBT("scT", [128, 8, 5]) as scT, SBT("wm", [128, 8, 128]) as wm:
            dq.start(scT[:], cT_in)
            LW()
            A.activation(out=scT[:], in_=scT[:], func=AF.Silu)
            B()
            for l in range(2):
                wv = w_mod[l].rearrange("(k p) (m j) -> p k m j", p=128, j=128)
                with MyFori(0, 48) as m:
                    dq.start(wm[:], wv[:, :, m, :])
                    LW()
                    for k in range(8):
                        PE.matmul(PS[0][:, 0:5], wm[:, k, :], scT[:, k, :], start=(k == 0), stop=(k == 7))
                    B()
                    V.tensor_scalar(out=MOD[:, l, m, :], in0=PS[0][:, 0:5], scalar1=BMOD[:, l, bass.ts(m, 1)],
                                    scalar2=None, op0=ALU.add)
                    B()

    stage_mods()

    def stage_norm(BIG, gfun, shfun):
        XSv = XS.rearrange("(k p) t -> p k t", p=128)
        with SBT("n_xt", [128, 8, 512]) as xt, SBT("n_sq", [128, 8, 512]) as sq, \
                SBT("n_rs", [128, 512]) as rs:
            for ti, (t0, n) in enumerate(TILES):
                v = 0 if ti == 0 else 1
                dq.start(xt[:, :, 0:n], XSv[:, :, t0:t0 + n])
                LW()
                A.activation(out=sq[:, :, 0:n], in_=xt[:, :, 0:n], func=AF.Square)
                B()
                for k in range(8):
                    PE.matmul(PS[0][:, 0:n], ones[:, :], sq[:, k, 0:n], start=(k == 0), stop=(k == 7))
                B()
                A.activation(out=rs[:, 0:n], in_=PS[0][:, 0:n], func=AF.Sqrt, bias=epsc[:, :], scale=1.0 / D)
                B()
                V.reciprocal(out=rs[:, 0:n], in_=rs[:, 0:n])
                B()
                for k in range(8):
                    V.scalar_tensor_tensor(out=sq[:, k, 0:n], in0=xt[:, k, 0:n], scalar=gfun(v, k), in1=rs[:, 0:n],
                                           op0=ALU.mult, op1=ALU.mult)
                B()
                for k in range(8):
                    A.activation(out=BIG[:, k, t0:t0 + n], in_=sq[:, k, 0:n], func=AF.Identity, bias=shfun(v, k),
                                 scale=1.0)
                B()

    def stage_gemm(BIG, wview, m_lo, m_hi, orow0, dst, epi):
        dstv = dst.rearrange("(m p) t -> m p t", p=128)
        with SBT("g_w", [128, 8, 128]) as wt, SBT("g_o", [128, T]) as ot, \
                SBT("g_x", [128, T]) as xr:
            with MyFori(m_lo, m_hi) as m:
                dq.start(wt[:], wview[:, :, m, :])
                if epi[0] == "resid":
                    dq.start(xr[:], dstv[m + orow0])
                LW()
                for ti, (t0, n) in enumerate(TILES):
                    for k in range(8):
                        PE.matmul(PS[ti][:, 0:n], wt[:, k, :], BIG[:, k, t0:t0 + n], start=(k == 0), stop=(k == 7))
                B()
                for ti, (t0, n) in enumerate(TILES):
                    if epi[0] == "copy":
                        if ti % 2 == 0:
                            A.activation(out=ot[:, t0:t0 + n], in_=PS[ti][:, 0:n], func=AF.Copy)
                        else:
                            V.tensor_copy(out=ot[:, t0:t0 + n], in_=PS[ti][:, 0:n])
                    elif epi[0] == "sigmoid":
                        A.activation(out=ot[:, t0:t0 + n], in_=PS[ti][:, 0:n], func=AF.Sigmoid)
                    elif epi[0] == "resid":
                        v = 0 if ti == 0 else 1
                        V.scalar_tensor_tensor(out=ot[:, t0:t0 + n], in0=PS[ti][:, 0:n], scalar=epi[1](v, m),
                                               in1=xr[:, t0:t0 + n], op0=ALU.mult, op1=ALU.add)
                B()
                dq.start(dstv[m + orow0], ot[:])
                LW()

    VCOLS = [(512, 256), (1280, 256), (2176, 128), (2816, 256)]

    def stage_vtok(BIG, l):
        VTv = VT.rearrange("(c p) f -> c p f", p=128)
        wl = w_in[l].rearrange("(k p) f -> p k f", p=128)
        with SBT("v_w", [128, 8, 896]) as wv, SBT("v_o", [128, 896]) as vo, SBT("v_stg", [128, 8, 128]) as stg:
            c0 = 0
            for (col0, wd) in VCOLS:
                dq.start(wv[:, :, c0:c0 + wd], wl[:, :, col0:col0 + wd])
                c0 += wd
            LW()
            with MyFori(0, 18) as tc:
                V.tensor_copy(out=stg[:], in_=BIG[:, :, bass.ts(tc, 128)])
                B()
                for k in range(8):
                    PE.matmul(PS[0][:, 0:512], stg[:, k, :], wv[:, k, 0:512], start=(k == 0), stop=(k == 7))
                for k in range(8):
                    PE.matmul(PS[1][:, 0:384], stg[:, k, :], wv[:, k, 512:896], start=(k == 0), stop=(k == 7))
                B()
                A.activation(out=vo[:, 0:512], in_=PS[0][:, 0:512], func=AF.Copy)
                V.tensor_copy(out=vo[:, 512:896], in_=PS[1][:, 0:384])
                B()
                dq.start(VTv[tc], vo[:])
                LW()

    def prep(dst, blk, d, tabs, bufs, gains=None, norm=False):
        q = d // 4
        Ab, Bb, t1, t2 = bufs
        if callable(blk):
            rb = blk
        else:
            PTq = PT.rearrange("(a p) t -> a p t", p=q)
            rb = lambda i: PTq[blk + i]
        for i in range(4):
            dq.start(Ab[i * q:(i + 1) * q, :], rb(i))
        for i, j in enumerate((1, 0, 3, 2)):
            dq.start(Bb[i * q:(i + 1) * q, :], rb(j))
        LW()
        C, S = tabs
        if norm:
            A.activation(out=t2[0:d, :], in_=Ab[0:d, :], func=AF.Square)
            B()
            for ti, (t0, n) in enumerate(TILES):
                PE.matmul(PS[ti][0:d, 0:n], ones[0:d, 0:d], t2[0:d, t0:t0 + n], start=True, stop=True)
            B()
            for ti, (t0, n) in enumerate(TILES):
                A.activation(out=dst[0:d, t0:t0 + n], in_=PS[ti][0:d, 0:n], func=AF.Sqrt, bias=epsc[0:d, :], scale=1.0 / d)
            B()
            V.reciprocal(out=dst[0:d, :], in_=dst[0:d, :])
        if gains is not None:
            V.tensor_scalar(out=Ab[0:d, :], in0=Ab[0:d, :], scalar1=gains[0], scalar2=None, op0=ALU.mult)
            V.tensor_scalar(out=Bb[0:d, :], in0=Bb[0:d, :], scalar1=gains[1], scalar2=None, op0=ALU.mult)
        B()
        if t1 is None:
            t1, t2 = Ab, Bb
        V.tensor_tensor(out=t1[0:d, :], in0=Ab[0:d, :], in1=C, op=ALU.mult)
        V.tensor_tensor(out=t2[0:d, :], in0=Bb[0:d, :], in1=S, op=ALU.mult)
        B()
        if norm:
            V.tensor_tensor(out=t1[0:d, :], in0=t1[0:d, :], in1=t2[0:d, :], op=ALU.add)
            B()
            V.tensor_tensor(out=dst[0:d, :], in0=t1[0:d, :], in1=dst[0:d, :], op=ALU.mult)
        else:
            V.tensor_tensor(out=dst[0:d, :], in0=t1[0:d, :], in1=t2[0:d, :], op=ALU.add)
        B()

    def load_vaug(Vb, col0, nch=18, tok0=0):
        src = VT[tok0:tok0 + nch * 128, :].rearrange("(c p) f -> p c f", p=128)
        dq.start(Vb[:, 0:nch, 0:64], src[:, :, col0:col0 + 64] if isinstance(col0, int) else None)

    def attn_core(QRq, KR, dqk, n, kchunks, vfun, scale, oa_bank, E):
        groups = [kchunks[i:i + 4] for i in range(0, len(kchunks), 4)]
        npv = len(kchunks)
        pv_i = 0
        prev = None
        for g in groups + [None]:
            if prev is not None:
                for j, c in enumerate(prev):
                    PE.matmul(PS[oa_bank][0:65, 0:n], vfun(c), E[:, j, 0:n], start=(pv_i == 0), stop=(pv_i == npv - 1))
                    pv_i += 1
            if g is not None:
                for j, c in enumerate(g):
                    PE.matmul(PS[j][:, 0:n], KR[0:dqk, c * 128:(c + 1) * 128], QRq, start=True, stop=True)
            B()
            if g is not None:
                for j, c in enumerate(g):
                    A.activation(out=E[:, j, 0:n], in_=PS[j][:, 0:n], func=AF.Exp, scale=scale)
                B()
            prev = g

    def attn_finish(oa_bank, n, OAS, RZ, YO, zbank=6):
        A.activation(out=OAS[0:65, 0:n], in_=PS[oa_bank][0:65, 0:n], func=AF.Copy)
        B()
        PE.matmul(PS[zbank][0:64, 0:n], SELD[0:65, 0:64], OAS[0:65, 0:n], start=True, stop=True)
        B()
        V.reciprocal(out=RZ[0:64, 0:n], in_=PS[zbank][0:64, 0:n])
        B()
        V.tensor_tensor(out=YO[0:64, 0:n], in0=OAS[0:64, 0:n], in1=RZ[0:64, 0:n], op=ALU.mult)
        B()

    ALLK = list(range(18))
    YT64 = YT.rearrange("(a p) t -> a p t", p=64)
    PT64 = PT.rearrange("(a p) t -> a p t", p=64)
    VTc = VT.rearrange("(c p) f -> p c f", p=128)

    def stage_gqa(l):
        with ExitStack() as s2:
            def t(name, shape):
                return s2.enter_context(SBT(name, list(shape)))
            RC = t("q_rc", [64, T]); RS = t("q_rs", [64, T])
            Ab = t("q_a", [64, T]); Bb = t("q_b", [64, T]); t1 = t("q_t1", [64, T]); t2 = t("q_t2", [64, T])
            KR = t("q_kr", [64, T]); QR = t("q_qr", [64, T]); Vb = t("q_v", [128, 18, 65])
            E = t("q_e", [128, 4, 512]); OAS = t("q_oas", [65, 512]); RZ = t("q_rz", [64, 512]); YO = t("q_yo", [64, 512])
            GQ = t("q_g", [64, 4]); YH = t("q_yh", [64, T])
            dq.start(RC[:], rope64_in[0]); dq.start(RS[:], rope64_in[1]); dq.start(GQ[:], gqag_in[l])
            LW()
            bufs = (Ab, Bb, t1, t2)
            for kv in range(2):
                prep(KR, (2048 + kv * 64) // 16, 64, (RC[:], RS[:]), bufs, gains=(GQ[:, 2:3], GQ[:, 3:4]), norm=True)
                dq.start(Vb[:, :, 0:64], VTc[:, :, 512 + kv * 64:512 + kv * 64 + 64])
                V.memset(Vb[:, :, 64:65], 1.0)
                LW()
                for hh in range(2):
                    dq.start(STG[0:64, :], PT64[hh + (1792 + kv * 128) // 64])
                    LW()
                    prep(QR, lambda i: STG.rearrange("(a p) t -> a p t", p=16)[i], 64, (RC[:], RS[:]), bufs, gains=(GQ[:, 0:1], GQ[:, 1:2]), norm=True)
                    QRl = QR[:, NCTX:T]
                    with MyFori(0, 4) as qt:
                        attn_core(QRl[:, bass.ts(qt, 512)], KR, 64, 512, ALLK, lambda c: Vb[:, c, :], 0.125, 4, E)
                        attn_finish(4, 512, OAS, RZ, YO)
                        V.tensor_copy(out=YH[:, NCTX:T][:, bass.ts(qt, 512)], in_=YO[:, :])
                        B()
                    attn_core(QR[:, 0:NCTX], KR, 64, NCTX, [0, 1], lambda c: Vb[:, c, :], 0.125, 4, E)
                    attn_finish(4, NCTX, OAS, RZ, YO)
                    V.tensor_copy(out=YH[:, 0:NCTX], in_=YO[:, 0:NCTX])
                    B()
                    dq.start(YT64[8 + kv * 2 + hh], YH[:])
                    LW()

    def stage_diff(l):
        lam_init = 0.8 - 0.6 * math.exp(-0.3 * l)
        with ExitStack() as s2:
            def t(name, shape):
                return s2.enter_context(SBT(name, list(shape)))
            RC = t("d_rc", [32, T]); RS = t("d_rs", [32, T])
            Ab = t("d_a", [32, T]); Bb = t("d_b", [32, T]); t1 = None; t2 = None
            QK = [t(f"d_qk{i}", [32, T]) for i in range(4)]
            Vb = t("d_v", [128, 18, 65])
            E = t("d_e", [128, 4, 512]); OAS = t("d_oas", [65, 512]); RZ = t("d_rz", [64, 512])
            Y1 = t("d_y1", [64, 512]); Y2 = t("d_y2", [64, 512]); SQ = t("d_sq", [64, 512])
            LM = t("d_lm", [128, 128]); LV = t("d_lv", [128, 4]); YH = t("d_yh", [64, T])
            dq.start(RC[:], rope32_in[0]); dq.start(RS[:], rope32_in[1]); dq.start(LM[:], dlam_in[l])
            LW()
            V.tensor_tensor(out=LM[:, 0:32], in0=LM[:, 0:32], in1=LM[:, 32:64], op=ALU.mult)
            V.tensor_tensor(out=LM[:, 64:96], in0=LM[:, 64:96], in1=LM[:, 96:128], op=ALU.mult)
            B()
            V.reduce_sum(out=LV[:, 0:1], in_=LM[:, 0:32], axis=AX.X)
            V.reduce_sum(out=LV[:, 1:2], in_=LM[:, 64:96], axis=AX.X)
            B()
            A.activation(out=LV[:, 0:2], in_=LV[:, 0:2], func=AF.Exp)
            B()
            V.tensor_tensor(out=LV[:, 2:3], in0=LV[:, 1:2], in1=LV[:, 0:1], op=ALU.subtract)
            B()
            V.tensor_scalar(out=LV[:, 2:3], in0=LV[:, 2:3], scalar1=-lam_init, scalar2=None, op0=ALU.add)
            B()
            bufs = (Ab, Bb, t1, t2)
            sc = 32 ** -0.5
            with MyFori(0, 4) as h:
                dq.start(YT64[h + 11], YH[:])
                dq.start(STG[0:64, :], PT64[h + 36]); dq.start(STG[64:128, :], PT64[h + 40])
                LW()
                prep(QK[0], lambda i: STG.rearrange("(a p) t -> a p t", p=8)[0 + i], 32, (RC[:], RS[:]), bufs)
                prep(QK[1], lambda i: STG.rearrange("(a p) t -> a p t", p=8)[4 + i], 32, (RC[:], RS[:]), bufs)
                prep(QK[2], lambda i: STG.rearrange("(a p) t -> a p t", p=8)[8 + i], 32, (RC[:], RS[:]), bufs)
                prep(QK[3], lambda i: STG.rearrange("(a p) t -> a p t", p=8)[12 + i], 32, (RC[:], RS[:]), bufs)
                VTh = VT[:, 640:896].rearrange("(c p) (h f) -> h p c f", p=128, f=64)
                dq.start(Vb[:, :, 0:64], VTh[h])
                V.memset(Vb[:, :, 64:65], 1.0)
                LW()

                def one_tile(q0sl, n, kch, dst):
                    attn_core(QK[0][:, q0sl] if not callable(q0sl) else q0sl(QK[0]), QK[2], 32, n, kch, lambda c: Vb[:, c, :], sc, 4, E)
                    attn_finish(4, n, OAS, RZ, Y1)
                    attn_core(QK[1][:, q0sl] if not callable(q0sl) else q0sl(QK[1]), QK[3], 32, n, kch, lambda c: Vb[:, c, :], sc, 5, E)
                    attn_finish(5, n, OAS, RZ, Y2)
                    V.scalar_tensor_tensor(out=Y1[:, 0:n], in0=Y2[:, 0:n], scalar=LV[0:64, 2:3], in1=Y1[:, 0:n],
                                           op0=ALU.mult, op1=ALU.add)
                    B()
                    A.activation(out=SQ[:, 0:n], in_=Y1[:, 0:n], func=AF.Square)
                    B()
                    PE.matmul(PS[6][0:64, 0:n], ones[0:64, 0:64], SQ[:, 0:n], start=True, stop=True)
                    B()
                    A.activation(out=SQ[:, 0:n], in_=PS[6][0:64, 0:n], func=AF.Sqrt, bias=epsc[0:64, :], scale=1.0 / 64)
                    B()
                    V.reciprocal(out=SQ[:, 0:n], in_=SQ[:, 0:n])
                    B()
                    V.scalar_tensor_tensor(out=Y2[:, 0:n], in0=Y1[:, 0:n], scalar=DSUB[:, l:l + 1], in1=SQ[:, 0:n],
                                           op0=ALU.mult, op1=ALU.mult)
                    B()
                    A.activation(out=Y2[:, 0:n], in_=Y2[:, 0:n], func=AF.Copy, scale=(1.0 - lam_init))
                    B()
                    V.tensor_copy(out=dst, in_=Y2[:, 0:n])
                    B()

                with MyFori(0, 4) as qt:
                    one_tile(lambda Q: Q[:, NCTX:T][:, bass.ts(qt, 512)], 512, ALLK,
                             YH[:, NCTX:T][:, bass.ts(qt, 512)])
                one_tile(slice(0, NCTX), NCTX, [0, 1], YH[:, 0:NCTX])
            dq.start(YT64[15], YH[:])
            LW()

    def stage_na(l):
        with ExitStack() as s2:
            def t(name, shape):
                return s2.enter_context(SBT(name, list(shape)))
            NAB = t("a_nab", [128, 4, 14, 64]); NABh = t("a_nabh", [128, 14, 64]); MSK = t("a_msk", [128, 64])
            QT = t("a_q", [64, T]); KT = t("a_k", [64, T])
            Ve = t("a_ve", [128, 18, 65]); Vo = t("a_vo", [128, 17, 65])
            E = t("a_e", [128, 6, 64]); OAll = t("a_oall", [65, L])
            E4 = t("a_e4", [128, 4, 512])
            OAS = t("a_oas", [65, 512]); RZ = t("a_rz", [64, 512]); YO = t("a_yo", [64, 512])
            dq.start(NAB[:], nab_in[l]); dq.start(MSK[:], namask_in)
            LW()
            for hh in range(4):
                for dd in range(14):
                    V.tensor_tensor(out=NAB[:, hh, dd, :], in0=NAB[:, hh, dd, :], in1=MSK[:, :], op=ALU.add)
            B()
            VT_e = VT[:, 0:256].rearrange("(c p) (h f) -> h p c f", p=128, f=64)
            VT_o = VT[64:64 + 17 * 128, 0:256].rearrange("(c p) (h f) -> h p c f", p=128, f=64)
            with MyFori(0, 4) as h:
                dq.start(QT[:], PT64[h]); dq.start(KT[:], PT64[4 + h])
                dq.start(Ve[:, :, 0:64], VT_e[h]); dq.start(Vo[:, :, 0:64], VT_o[h])
                V.memset(Ve[:, :, 64:65], 1.0)
                V.memset(Vo[:, :, 64:65], 1.0)
                V.tensor_copy(out=NABh[:], in_=NAB[:, h, :, :])
                LW()

                def row(qcols, r0tok, off, vsel, slot):
                    for j in range(4):
                        PE.matmul(PS[0][:, j * 64:(j + 1) * 64], r0tok(j), qcols, start=True, stop=True)
                    for j in range(2):
                        PE.matmul(PS[0][:, (4 + j) * 64:(5 + j) * 64], KT[:, j * 128:(j + 1) * 128], qcols, start=True, stop=True)
                    B()
                    b0 = 7 - off
                    A.activation(out=E[:, 0:4, :], in_=PS[0][:, 0:256].rearrange("p (j c) -> p j c", c=64), func=AF.Copy, scale=0.125)
                    A.activation(out=E[:, 4:6, :], in_=PS[0][:, 256:384].rearrange("p (j c) -> p j c", c=64), func=AF.Exp, scale=0.125)
                    B()
                    for j in range(4):
                        V.tensor_tensor(out=E[:, j, :], in0=E[:, j, :], in1=NABh[:, b0 + 2 * j, :], op=ALU.add)
                    B()
                    A.activation(out=E[:, 0:4, :], in_=E[:, 0:4, :], func=AF.Exp)
                    B()
                    for j in range(6):
                        lhs = vsel(j) if j < 4 else Ve[:, j - 4, :]
                        PE.matmul(PS[1][0:65, slot * 64:(slot + 1) * 64], lhs, E[:, j, :], start=(j == 0), stop=(j == 5))
                    B()

                def static_row(r):
                    r0 = min(max(r - 4, 0), 24)
                    off = r - r0
                    if r0 % 2 == 0:
                        vs = lambda j: Ve[:, 2 + r0 // 2 + j, :]
                    else:
                        vs = lambda j: Vo[:, (r0 + 3) // 2 + j, :]
                    row(QT[:, NCTX + r * 64:NCTX + (r + 1) * 64],
                        lambda j: KT[:, NCTX + (r0 + 2 * j) * 64:NCTX + (r0 + 2 * j) * 64 + 128], off, vs, r % 2)
                    A.activation(out=OAll[:, r * 64:(r + 1) * 64], in_=PS[1][0:65, (r % 2) * 64:(r % 2 + 1) * 64], func=AF.Copy)

                for r in range(32):
                    static_row(r)
                B()
                for qt in range(4):
                    PE.matmul(PS[2 + qt][0:64, 0:512], SELD[0:65, 0:64], OAll[0:65, qt * 512:(qt + 1) * 512], start=True, stop=True)
                B()
                for qt in range(4):
                    V.reciprocal(out=E4[0:64, qt, :], in_=PS[2 + qt][0:64, 0:512])
                B()
                for qt in range(4):
                    V.tensor_tensor(out=OAll[0:64, qt * 512:(qt + 1) * 512], in0=OAll[0:64, qt * 512:(qt + 1) * 512],
                                    in1=E4[0:64, qt, :], op=ALU.mult)
                B()
                dq.start(YT64[h][:, NCTX:T], OAll[0:64, :])
                LW()
                attn_core(QT[:, 0:NCTX], KT, 64, NCTX, [0, 1], lambda c: Ve[:, c, :], 0.125, 4, E4)
                attn_finish(4, NCTX, OAS, RZ, YO)
                dq.start(YT64[h][:, 0:NCTX], YO[:, 0:NCTX])
                LW()

    def stage_ret(l):
        with ExitStack() as s2:
            def t(name, shape):
                return s2.enter_context(SBT(name, list(shape)))
            QR = t("r_qr", [64, 4, T]); KR = t("r_kr", [64, 4, T])
            RP = t("r_rp", [128, 8]); LG = t("r_lg", [128, 8])
            INTRA = t("r_intra", [128, 8, 128]); QDEC = t("r_qdec", [64, 8, 128]); KDEC = t("r_kdec", [128, 8, 64])
            CDv = t("r_cd", [64, 8]); KD1 = t("r_kd1", [128, 8])
            ST = t("r_st", [64, 8, 64]); Qd = t("r_qd", [64, 8, 128]); Am = t("r_am", [128, 8, 128]); Kd = t("r_kdm", [128, 8, 64])
            s_tab = ExitStack()
            RCn = s_tab.enter_context(SBT("r_rcn", [128, 4, 128])); RPOS = s_tab.enter_context(SBT("r_rpos", [128, 4, 128]))
            dq.start(RP[:], retp_in[l])
            dq.start(RCn[:], retc_in); dq.start(RPOS[:], retpos_in)
            LW()
            A.activation(out=LG[:], in_=RP[:], func=AF.Exp)
            B()
            V.tensor_scalar(out=LG[:], in0=LG[:], scalar1=-1.0, scalar2=1.0, op0=ALU.mult, op1=ALU.add)
            B()
            A.activation(out=LG[:], in_=LG[:], func=AF.Ln)
            B()
            for hd in range(8):
                dr = hd // 4
                A.activation(out=INTRA[:, hd, :], in_=RCn[:, 2 * dr, :], func=AF.Exp, scale=LG[:, hd:hd + 1])
                A.activation(out=QDEC[:, hd, :], in_=RPOS[0:64, dr, :], func=AF.Exp, scale=LG[0:64, hd:hd + 1])
                A.activation(out=KD1[:, hd:hd + 1], in_=RPOS[:, 2 + dr, 0:1], func=AF.Exp, scale=LG[:, hd:hd + 1])
            A.activation(out=CDv[:], in_=LG[0:64, :], func=AF.Exp, scale=128.0)
            B()
            for hd in range(8):
                dr = hd // 4
                V.scalar_tensor_tensor(out=INTRA[:, hd, :], in0=INTRA[:, hd, :], scalar=0.125, in1=RCn[:, 2 * dr + 1, :], op0=ALU.mult, op1=ALU.mult)
                V.tensor_scalar(out=KDEC[:, hd, :], in0=ones[:, 0:64], scalar1=KD1[:, hd:hd + 1], scalar2=0.125,
                                op0=ALU.mult, op1=ALU.mult)
            V.memset(ST[:], 0.0)
            B()
            s_tab.close()
            with ExitStack() as sp:
                def tp(name, shape):
                    return sp.enter_context(SBT(name, list(shape)))
                RC = tp("r_rc", [64, T]); RS = tp("r_rs", [64, T])
                Ab = tp("r_a", [64, T]); Bb = tp("r_b", [64, T]); t1 = None; t2 = None
                dq.start(RC[:], rope64_in[0]); dq.start(RS[:], rope64_in[1])
                LW()
                bufs = (Ab, Bb, t1, t2)
                for hh in range(4):
                    prep(QR[:, hh, :], (768 + hh * 64) // 16, 64, (RC[:], RS[:]), bufs)
                    prep(KR[:, hh, :], (1024 + hh * 64) // 16, 64, (RC[:], RS[:]), bufs)

            Vs = t("r_vs", [128, 2, 256])
            OAcc = t("r_oacc", [64, 4, T])
            V.memset(OAcc[:], 0.0)
            B()

            def step(cf, cb):
                csl = [slice(cf * 128, (cf + 1) * 128), slice(cb * 128, (cb + 1) * 128)]
                cidx = [cf, cb]
                dq.start(Vs[:, 0, :], VTc[:, cf, 256:512]); dq.start(Vs[:, 1, :], VTc[:, cb, 256:512])
                LW()
                for hd in range(8):
                    dr, hh = hd // 4, hd % 4
                    PE.matmul(PS[dr][:, hh * 128:(hh + 1) * 128], KR[:, hh, csl[dr]], QR[:, hh, csl[dr]], start=True, stop=True)
                    PE.matmul(PS[2][:, hd * 64:(hd + 1) * 64], KR[:, hh, csl[dr]], ident[0:64, 0:64], start=True, stop=True)
                for dr in range(2):
                    V.tensor_tensor(out=Qd[:, dr * 4:(dr + 1) * 4, :], in0=QR[:, :, csl[dr]], in1=QDEC[:, dr * 4:(dr + 1) * 4, :], op=ALU.mult)
                B()
                for dr in range(2):
                    V.tensor_tensor(out=Am[:, dr * 4:(dr + 1) * 4, :], in0=PS[dr][:, :].rearrange("p (h c) -> p h c", c=128),
                                    in1=INTRA[:, dr * 4:(dr + 1) * 4, :], op=ALU.mult)
                V.tensor_tensor(out=Kd[:], in0=PS[2][:, :].rearrange("p (h c) -> p h c", c=64), in1=KDEC[:], op=ALU.mult)
                B()
                for hd in range(8):
                    dr, hh = hd // 4, hd % 4
                    vch = Vs[:, dr, hh * 64:(hh + 1) * 64]
                    ob = PS[3 + dr][0:64, hh * 128:(hh + 1) * 128]
                    PE.matmul(ob, vch, Am[:, hd, :], start=True, stop=True)
                    PE.matmul(PS[6 + dr][0:64, hh * 128:(hh + 1) * 128], ST[:, hd, :], Qd[:, hd, :], start=True, stop=True)
                    PE.matmul(PS[5][0:64, hd * 64:(hd + 1) * 64], Kd[:, hd, :], vch, start=True, stop=True)
                B()
                for hd in range(8):
                    V.scalar_tensor_tensor(out=ST[:, hd, :], in0=ST[:, hd, :], scalar=CDv[:, hd:hd + 1],
                                           in1=PS[5][0:64, hd * 64:(hd + 1) * 64], op0=ALU.mult, op1=ALU.add)
                for dr in range(2):
                    V.tensor_tensor(out=OAcc[:, :, csl[dr]], in0=OAcc[:, :, csl[dr]],
                                    in1=PS[3 + dr][0:64, :].rearrange("p (h c) -> p h c", c=128), op=ALU.add)
                B()
                for dr in range(2):
                    V.tensor_tensor(out=OAcc[:, :, csl[dr]], in0=OAcc[:, :, csl[dr]],
                                    in1=PS[6 + dr][0:64, :].rearrange("p (h c) -> p h c", c=128), op=ALU.add)
                B()

            if not os.environ.get("RET_SKIPSTEPS"):
                step(0, 1)
                step(1, 0)
                for j in range(16):
                    step(j + 2, 17 - j)
            with ExitStack() as s3:
                G = s3.enter_context(SBT("r_g", [64, 512]))
                SQ = s3.enter_context(SBT("r_sq", [64, 512]))
                CEN = s3.enter_context(SBT("r_cen", [64, 512]))
                MSQ = s3.enter_context(SBT("r_msq", [64, 512]))
                for hh in range(4):
                    for ti, (t0, n) in enumerate(TILES):
                        O = OAcc[:, hh, t0:t0 + n]
                        dq.start(G[:, 0:n], PT64[(1536 // 64) + hh][:, t0:t0 + n])
                        A.activation(out=SQ[:, 0:n], in_=O, func=AF.Square)
                        LW()
                        A.activation(out=G[:, 0:n], in_=G[:, 0:n], func=AF.Silu)
                        PE.matmul(PS[0][0:64, 0:n], ones[0:64, 0:64], O, start=True, stop=True)
                        PE.matmul(PS[1][0:64, 0:n], ones[0:64, 0:64], SQ[:, 0:n], start=True, stop=True)
                        B()
                        A.activation(out=CEN[:, 0:n], in_=PS[0][0:64, 0:n], func=AF.Copy, scale=1.0 / 64)
                        A.activation(out=SQ[:, 0:n], in_=PS[1][0:64, 0:n], func=AF.Copy, scale=1.0 / 64)
                        B()
                        A.activation(out=MSQ[:, 0:n], in_=CEN[:, 0:n], func=AF.Square)
                        B()
                        V.tensor_tensor(out=CEN[:, 0:n], in0=O, in1=CEN[:, 0:n], op=ALU.subtract)
                        V.tensor_tensor(out=MSQ[:, 0:n], in0=SQ[:, 0:n], in1=MSQ[:, 0:n], op=ALU.subtract)
                        B()
                        A.activation(out=MSQ[:, 0:n], in_=MSQ[:, 0:n], func=AF.Sqrt, bias=epsc[0:64, :], scale=1.0)
                        B()
                        V.reciprocal(out=MSQ[:, 0:n], in_=MSQ[:, 0:n])
                        B()
                        V.tensor_tensor(out=CEN[:, 0:n], in0=CEN[:, 0:n], in1=MSQ[:, 0:n], op=ALU.mult)
                        B()
                        V.tensor_tensor(out=G[:, 0:n], in0=CEN[:, 0:n], in1=G[:, 0:n], op=ALU.mult)
                        B()
                        dq.start(YT64[4 + hh][:, t0:t0 + n], G[:, 0:n])
                        LW()

    def stage_merge(l):
        wb = w_branch[l].rearrange("n (h p) (m j) -> p n h m j", p=64, j=128)
        GTv = PT[3072:7168, :].rearrange("(n m p) t -> m p n t", n=4, p=128)
        YTv = YT.rearrange("(a p) t -> p a t", p=64)
        ATv = AT.rearrange("(m p) t -> m p t", p=128)
        with ExitStack() as s2:
            def t(name, shape):
                return s2.enter_context(SBT(name, list(shape)))
            Y = t("m_y", [64, 16, 512]); W = t("m_w", [64, 4, 4, 128]); Gt = t("m_g", [128, 4, 512])
            Tm = t("m_t", [128, 4, 512])
            for ti, (t0, n) in enumerate(TILES):
                dq.start(Y[:, :, 0:n], YTv[:, :, t0:t0 + n])
                LW()
                with MyFori(0, 8) as m:
                    for nb in range(4):
                        dq.start(W[:, nb, :, :], wb[:, nb, :, m, :])
                    dq.start(Gt[:, :, 0:n], GTv[m][:, :, t0:t0 + n])
                    LW()
                    for nb in range(4):
                        for hh in range(4):
                            PE.matmul(PS[nb][:, 0:n], W[:, nb, hh, :], Y[:, nb * 4 + hh, 0:n], start=(hh == 0), stop=(hh == 3))
                    B()
                    for nb in range(4):
                        V.tensor_tensor(out=Tm[:, nb, 0:n], in0=PS[nb][:, 0:n], in1=Gt[:, nb, 0:n], op=ALU.mult)
                    B()
                    V.tensor_tensor(out=Tm[:, 0, 0:n], in0=Tm[:, 0, 0:n], in1=Tm[:, 1, 0:n], op=ALU.add)
                    V.tensor_tensor(out=Tm[:, 2, 0:n], in0=Tm[:, 2, 0:n], in1=Tm[:, 3, 0:n], op=ALU.add)
                    B()
                    V.tensor_tensor(out=Tm[:, 0, 0:n], in0=Tm[:, 0, 0:n], in1=Tm[:, 2, 0:n], op=ALU.add)
                    B()
                    dq.start(ATv[m][:, t0:t0 + n], Tm[:, 0, 0:n])
                    LW()

    def stage_moe(l):
        XSv = XS.rearrange("(k p) t -> p k t", p=128)
        with ExitStack() as s2:
            def t(name, shape):
                return s2.enter_context(SBT(name, list(shape)))
            WT = t("e_wt", [16, T])
            with ExitStack() as s3:
                BIG = s3.enter_context(SBT("e_big", [128, 8, T]))
                stage_norm(BIG, lambda v, k: GG[:, v, 1, l, k:k + 1], lambda v, k: MODS[:, v, l, 24 + k:25 + k])
                dq.start(MTD.rearrange("(k p) t -> p k t", p=128), BIG[:])
                LW()
                WR = s3.enter_context(SBT("e_wr", [128, 8, 16]))
                RB = s3.enter_context(SBT("e_rb", [128, 16]))
                S = s3.enter_context(SBT("e_s", [128, 18, 16]))
                SEL = s3.enter_context(SBT("e_sel", [128, 18, 16]))
                SEL2 = s3.enter_context(SBT("e_sel2", [128, 18, 16]))
                EM = s3.enter_context(SBT("e_em", [128, 18, 16]))
                M1 = s3.enter_context(SBT("e_m1", [128, 72]))
                M2 = s3.enter_context(SBT("e_m2", [128, 72]))
                GS = s3.enter_context(SBT("e_gs", [128, 72]))
                GM = s3.enter_context(SBT("e_gm", [128, 72]))
                GX = s3.enter_context(SBT("e_gx", [128, 18]))
                dq.start(WR[:], w_router.rearrange("(k p) e -> p k e", p=128))
                dq.start(RB[:], rb_in)
                LW()
                for tc in range(18):
                    for k in range(8):
                        PE.matmul(PS[0][:, tc * 16:(tc + 1) * 16], BIG[:, k, tc * 128:(tc + 1) * 128], WR[:, k, :], start=(k == 0), stop=(k == 7))
                B()
                A.activation(out=S[:].rearrange("p c e -> p (c e)"), in_=PS[0][:, 0:288], func=AF.Sigmoid)
                B()
                for c in range(18):
                    V.tensor_tensor(out=SEL[:, c, :], in0=S[:, c, :], in1=RB[:], op=ALU.add)
                B()
                sel4 = SEL[:].rearrange("p c (g i) -> p (c g) i", i=4)
                sel24 = SEL2[:].rearrange("p c (g i) -> p (c g) i", i=4)
                em4 = EM[:].rearrange("p c (g i) -> p (c g) i", i=4)
                gs3 = GS[:].rearrange("p (c g) -> p c g", g=4)
                gm3 = GM[:].rearrange("p (c g) -> p c g", g=4)
                V.reduce_max(out=M1[:], in_=sel4, axis=AX.X)
                B()
                for i in range(4):
                    V.tensor_tensor(out=em4[:, :, i], in0=sel4[:, :, i], in1=M1[:, :], op=ALU.is_equal)
                B()
                V.tensor_scalar(out=sel24, in0=em4, scalar1=-1.0e9, scalar2=None, op0=ALU.mult)
                B()
                V.tensor_tensor(out=sel24, in0=sel24, in1=sel4, op=ALU.add)
                B()
                V.reduce_max(out=M2[:], in_=sel24, axis=AX.X)
                B()
                V.tensor_tensor(out=GS[:], in0=M1[:], in1=M2[:], op=ALU.add)
                B()
                V.reduce_max(out=GX[:], in_=gs3, axis=AX.X)
                B()
                for g in range(4):
                    V.tensor_tensor(out=gm3[:, :, g], in0=gs3[:, :, g], in1=GX[:, :], op=ALU.is_equal)
                for i in range(4):
                    V.tensor_tensor(out=em4[:, :, i], in0=sel4[:, :, i], in1=M2[:, :], op=ALU.is_ge)
                B()
                for i in range(4):
                    V.tensor_tensor(out=em4[:, :, i], in0=em4[:, :, i], in1=GM[:, :], op=ALU.mult)
                B()
                V.tensor_tensor(out=EM[:], in0=EM[:], in1=S[:], op=ALU.mult)
                B()
                V.reduce_sum(out=GX[:], in_=EM[:], axis=AX.X)
                B()
                V.reciprocal(out=GX[:], in_=GX[:])
                B()
                for ee in range(16):
                    V.tensor_tensor(out=EM[:, :, ee], in0=EM[:, :, ee], in1=GX[:, :], op=ALU.mult)
                B()
                for c in range(18):
                    PE.matmul(PS[c // 4][0:16, (c % 4) * 128:(c % 4 + 1) * 128], EM[:, c, :], ident[:, :], start=True, stop=True)
                B()
                for c in range(18):
                    A.activation(out=WT[:, c * 128:(c + 1) * 128], in_=PS[c // 4][0:16, (c % 4) * 128:(c % 4 + 1) * 128], func=AF.Copy)
                B()
            with ExitStack() as s3:
                def t3(name, shape):
                    return s3.enter_context(SBT(name, list(shape)))
                WG = t3("e_wg", [128, 8, 512]); WU = t3("e_wu", [128, 8, 512]); WD = t3("e_wd", [128, 4, D])
                WGb = s3.enter_context(SBT("e_wgb", [128, 8, 512], BF16)); WUb = s3.enter_context(SBT("e_wub", [128, 8, 512], BF16))
                WDb = s3.enter_context(SBT("e_wdb", [128, 4, D], BF16))
                MTb = s3.enter_context(SBT("e_mtb", [128, 8, 512], BF16)); HIDb = s3.enter_context(SBT("e_hidb", [128, 4, 512], BF16))
                SELS = t3("e_sels", [16, 128]); WBC = t3("e_wbc", [128, T])
                MTt = t3("e_mt", [128, 8, 512]); SG = t3("e_sg", [128, 4, 512]); HID = SG; WBCt = t3("e_wbct", [128, 512]); ACCt = t3("e_acct", [128, 8, 512])
                ATk = MTD.rearrange("(k p) t -> p k t", p=128)
                ACCk = ACCD.rearrange("(k p) t -> p k t", p=128)
                V.memset(ACCt[:], 0.0)
                B()
                for ti, (t0, n) in enumerate(TILES):
                    dq.start(ACCk[:, :, t0:t0 + n], ACCt[:, :, 0:n])
                LW()
                wgv = w_gate[l].rearrange("e (k p) f -> e p k f", p=128)
                wuv = w_up[l].rearrange("e (k p) f -> e p k f", p=128)
                wdv = w_down[l].rearrange("e (c p) f -> e p c f", p=128)

                def tile_body(sl, n):
                    dq.start(MTt[:, :, 0:n], sl(ATk))
                    V.tensor_copy(out=WBCt[:, 0:n], in_=sl(WBC))
                    dq.start(ACCt[:, :, 0:n], sl(ACCk))
                    LW()
                    A.activation(out=MTb[:, 0:4, 0:n], in_=MTt[:, 0:4, 0:n], func=AF.Copy)
                    V.tensor_copy(out=MTb[:, 4:8, 0:n], in_=MTt[:, 4:8, 0:n])
                    B()
                    for hc in range(4):
                        for k in range(8):
                            PE.matmul(PS[hc][:, 0:n], WGb[:, k, hc * 128:(hc + 1) * 128], MTb[:, k, 0:n], start=(k == 0), stop=(k == 7))
                        for k in range(8):
                            PE.matmul(PS[4 + hc][:, 0:n], WUb[:, k, hc * 128:(hc + 1) * 128], MTb[:, k, 0:n], start=(k == 0), stop=(k == 7))
                    B()
                    for hc in range(4):
                        A.activation(out=SG[:, hc, 0:n], in_=PS[hc][:, 0:n], func=AF.Silu)
                    B()
                    for hc in range(4):
                        V.tensor_tensor(out=HID[:, hc, 0:n], in0=PS[4 + hc][:, 0:n], in1=SG[:, hc, 0:n], op=ALU.mult)
                    B()
                    for hc in range(4):
                        V.tensor_tensor(out=HIDb[:, hc, 0:n], in0=HID[:, hc, 0:n], in1=WBCt[:, 0:n], op=ALU.mult)
                    B()
                    for m in range(8):
                        for hc in range(4):
                            PE.matmul(PS[m][:, 0:n], WDb[:, hc, m * 128:(m + 1) * 128], HIDb[:, hc, 0:n], start=(hc == 0), stop=(hc == 3))
                    B()
                    for m in range(8):
                        V.tensor_tensor(out=ACCt[:, m, 0:n], in0=ACCt[:, m, 0:n], in1=PS[m][:, 0:n], op=ALU.add)
                    B()
                    dq.start(sl(ACCk), ACCt[:, :, 0:n])
                    LW()

                def sl_ctx(ap):
                    return ap[:, :, 0:NCTX] if len(ap.shape) == 3 else ap[:, 0:NCTX]

                with MyFori(0, 16) as e:
                    dq.start(WG[:], wgv[e]); dq.start(WU[:], wuv[e]); dq.start(WD[:], wdv[e])
                    dq.start(SELS[:], sel16_in[:, e, :])
                    LW()
                    for ti, (t0, n) in enumerate(TILES):
                        PE.matmul(PS[ti][:, 0:n], SELS[:, :], WT[:, t0:t0 + n], start=True, stop=True)
                    LW()
                    for ti, (t0, n) in enumerate(TILES):
                        A.activation(out=WBC[:, t0:t0 + n], in_=PS[ti][:, 0:n], func=AF.Copy)
                    V.tensor_copy(out=WGb[:], in_=WG[:])
                    nc.gpsimd.tensor_copy(out=WUb[:], in_=WU[:])
                    A.activation(out=WDb[:, 0:2, :], in_=WD[:, 0:2, :], func=AF.Copy)
                    V.tensor_copy(out=WDb[:, 2:4, :], in_=WD[:, 2:4, :])
                    B()
                    tile_body(sl_ctx, NCTX)
                    with MyFori(0, 4) as qt:
                        def sl_lat(ap):
                            if len(ap.shape) == 3:
                                return ap[:, :, NCTX:T][:, :, bass.ts(qt, 512)]
                            return ap[:, NCTX:T][:, bass.ts(qt, 512)]
                        tile_body(sl_lat, 512)

            with SBT("e_xs", [128, 8, 512]) as xs, SBT("e_ac", [128, 8, 512]) as ac:
                ACCk = ACCD.rearrange("(k p) t -> p k t", p=128)
                for ti, (t0, n) in enumerate(TILES):
                    v = 0 if ti == 0 else 1
                    dq.start(xs[:, :, 0:n], XSv[:, :, t0:t0 + n])
                    dq.start(ac[:, :, 0:n], ACCk[:, :, t0:t0 + n])
                    LW()
                    for k in range(8):
                        V.scalar_tensor_tensor(out=xs[:, k, 0:n], in0=ac[:, k, 0:n], scalar=MODS[:, v, l, 40 + k:41 + k],
                                               in1=xs[:, k, 0:n], op0=ALU.mult, op1=ALU.add)
                    B()
                    dq.start(XSv[:, :, t0:t0 + n], xs[:, :, 0:n])
                    LW()


    STAGES = ["norm1", "inproj", "vtok", "diff", "gqa", "ret", "na", "merge", "outproj", "moe"]

    def enabled(name):
        return stop_after is None or STAGES.index(name) <= STAGES.index(stop_after)

    from contextlib import nullcontext as _nullctx

    class _Const:
        def __enter__(self):
            return 0

        def __exit__(self, *a):
            return False

    with (MyFori(0, nsamp) if nsamp > 1 else _Const()) as s:
        dq.start(out[s], OUTS)
        dq.start(XS, xin[s])
        V.tensor_copy(out=MODS[:, 1, :, :], in_=MOD[:, :, :, s])
        V.tensor_copy(out=MODS[:, 0, :, :], in_=MOD[:, :, :, 4])
        LW()
        for v in range(2):
            for nrm in range(2):
                V.scalar_tensor_tensor(out=GG[:, v, nrm, :, :], in0=MODS[:, v, :, 8 + 24 * nrm:16 + 24 * nrm], scalar=1.0,
                                       in1=GN[:, 2 * nrm:2 * nrm + 2, :], op0=ALU.add, op1=ALU.mult)
        B()
        for l in range(nlayers):
            with SBT("BIG", [128, 8, T]) as BIG:
                if enabled("norm1"):
                    stage_norm(BIG, lambda v, k: GG[:, v, 0, l, k:k + 1], lambda v, k: MODS[:, v, l, k:k + 1])
                    if dbg and "HT" in dbg:
                        dq.start(dbg_t["HT"].rearrange("(k p) t -> p k t", p=128), BIG[:])
                        LW()
                wv = w_in[l].rearrange("(k p) (m j) -> p k m j", p=128, j=128)
                if enabled("inproj"):
                    stage_gemm(BIG, wv, 0, 24, 0, PT, ("copy",))
                    stage_gemm(BIG, wv, 24, 56, 0, PT, ("sigmoid",))
                if enabled("vtok"):
                    stage_vtok(BIG, l)
            if enabled("diff"):
                stage_diff(l)
            if enabled("gqa"):
                stage_gqa(l)
            if enabled("ret"):
                stage_ret(l)
            if enabled("na"):
                stage_na(l)
            if enabled("merge"):
                stage_merge(l)
            if enabled("outproj"):
                with SBT("BIG2", [128, 8, T]) as BIG:
                    dq.start(BIG[:], AT.rearrange("(k p) t -> p k t", p=128))
                    LW()
                    wo = w_out[l].rearrange("(k p) (m j) -> p k m j", p=128, j=128)
                    stage_gemm(BIG, wo, 0, 8, 0, XS, ("resid", lambda v, m: MODS[:, v, l, 16:24][:, bass.ts(m, 1)]))
            if dbg and "X1" in dbg:
                dq.start(dbg_t["X1"], XS)
                LW()
            if enabled("moe"):
                stage_moe(l)
        with SBT("BIG3", [128, 8, T]) as BIG:
            stage_norm(BIG, lambda v, k: GN[:, 4, k:k + 1], lambda v, k: ZERO8[:, k:k + 1])
            dq.start(OUTS.rearrange("(k p) t -> p k t", p=128), BIG[:, :, NCTX:T])
            LW()
    dq.start(out[nsamp], OUTS)
    LW()
    es.close()
    if os.environ.get('SBUF_PEAK'):
        print('SBUF peaks', {k: v // 1024 for k, v in _peak.items()})
    return nc


def _consts():
    c = {}
    c["ident"] = np.eye(128, dtype=np.float32)
    seld = np.zeros((65, 64), np.float32); seld[64, :] = 1.0
    c["seld"] = seld
    sel16 = np.zeros((16, 16, 128), np.float32)
    for e in range(16):
        sel16[e, e, :] = 1.0
    c["sel16"] = sel16
    k = np.arange(128)[:, None].astype(np.float32); q = np.arange(128)[None, :].astype(np.float32)
    retc = np.zeros((128, 4, 128), np.float32)
    retc[:, 0, :] = np.maximum(q - k, 0); retc[:, 1, :] = (q >= k)
    retc[:, 2, :] = np.maximum(k - q, 0); retc[:, 3, :] = (k >= q)
    c["retc"] = retc
    rp = np.zeros((128, 4, 128), np.float32)
    t = np.arange(128, dtype=np.float32)
    rp[:, 0, :] = (t + 1.0)[None, :]; rp[:, 1, :] = (128.0 - t)[None, :]
    rp[:, 2, 0] = 127.0 - t; rp[:, 3, 0] = t
    c["retpos"] = rp

    def rope_tab(d):
        half = d // 2; nf = half // 2
        inv = (1.0 / (10000.0 ** (np.arange(nf, dtype=np.float32) / nf))).astype(np.float32)
        tt = np.arange(L, dtype=np.int32)
        rows = (tt // 64).astype(np.float32); cols = (tt % 64).astype(np.float32)
        C = np.ones((d, T), np.float32); S = np.zeros((d, T), np.float32)
        for blk, pos in ((0, rows), (1, cols)):
            ang = (pos[None, :] * inv[:, None]).astype(np.float32)
            cs, sn = np.cos(ang).astype(np.float32), np.sin(ang).astype(np.float32)
            b0 = blk * half
            C[b0:b0 + nf, NCTX:] = cs; C[b0 + nf:b0 + half, NCTX:] = cs
            S[b0:b0 + nf, NCTX:] = -sn; S[b0 + nf:b0 + half, NCTX:] = sn
        return np.stack([C, S])
    c["rope64"] = rope_tab(64)
    c["rope32"] = rope_tab(32)
    cc = np.arange(64)[:, None]; kc = np.arange(64)[None, :]
    ws = np.clip(cc - 8, 0, 48)
    ok = (kc >= ws) & (kc < ws + 16)
    idx = np.clip(kc - cc, -15, 15) + 15
    msk = np.where(ok, 0.0, NEGM).astype(np.float32)
    c["namask"] = np.ascontiguousarray(np.concatenate([msk.T, msk.T], axis=0))
    c["_naidx"] = idx
    lamc = np.zeros((128, 2, 2), np.float32)
    c["lamc"] = lamc
    return c


_CACHE = {}


def _prep_inputs(inp, nsamp_per_core, ncores):
    c = _consts()
    f = lambda a: np.ascontiguousarray(a, dtype=np.float32)
    shared = {}
    shared["w_mod"] = f(inp["w_mod"]); shared["w_in"] = f(inp["w_in"]); shared["w_branch"] = f(inp["w_branch"])
    shared["w_out"] = f(inp["w_out"]); shared["w_router"] = f(inp["w_router"])
    shared["w_gate"] = f(inp["w_gate_e"]); shared["w_up"] = f(inp["w_up_e"]); shared["w_down"] = f(inp["w_down_e"])
    shared["bmod"] = f(inp["b_mod"].reshape(2, 48, 128).transpose(2, 0, 1))
    gn = np.stack([inp["g_norm1"][0], inp["g_norm1"][1], inp["g_norm2"][0], inp["g_norm2"][1], inp["g_final"]])
    shared["gn"] = f(gn.reshape(5, 8, 128).transpose(2, 0, 1))
    idx = c["_naidx"]
    rpb = inp["na_rpb"]
    g = rpb[:, :, :, idx]
    nab = np.zeros((2, 128, 4, 14, 64), np.float32)
    for jj in range(2):
        nab[:, jj * 64:(jj + 1) * 64] = g[:, :, jj:jj + 14].transpose(0, 4, 1, 2, 3)
    shared["nab"] = f(nab)
    shared["namask"] = c["namask"]
    shared["retp"] = f(np.broadcast_to(inp["ret_log_decay"].reshape(2, 1, 8), (2, 128, 8)))
    perm = np.concatenate([np.arange(16, 32), np.arange(0, 16), np.arange(48, 64), np.arange(32, 48)])
    qg, kg = inp["gqa_q_gain"], inp["gqa_k_gain"]
    shared["gqag"] = f(np.stack([qg, qg[:, perm], kg, kg[:, perm]], axis=-1))
    shared["dlam"] = f(np.broadcast_to(inp["diff_lambda"].reshape(2, 1, 128), (2, 128, 128)))
    shared["dsub"] = f(inp["diff_subln"].T)
    shared["lamc"] = c["lamc"]
    shared["rb"] = f(np.broadcast_to(inp["router_bias"][None, :], (128, 16)))
    for k in ("rope64", "rope32", "ident", "seld", "sel16", "retc", "retpos"):
        shared[k] = c[k]
    maps = []
    for ci in range(ncores):
        b0 = ci * nsamp_per_core
        bs = slice(b0, b0 + nsamp_per_core)
        xin = np.concatenate([inp["ctx"][bs].transpose(0, 2, 1), inp["x"][bs].transpose(0, 2, 1)], axis=2)
        cc = np.concatenate([inp["c"][bs], np.zeros((4 - nsamp_per_core, D), np.float32), inp["c_ctx"][None, :]], axis=0)
        m = dict(shared)
        m["xin"] = f(xin)
        m["cT"] = f(cc.T.reshape(8, 128, 5).transpose(1, 0, 2))
        maps.append(m)
    return maps


def kernel(**inputs):
    inp = {k: np.asarray(v) for k, v in inputs.items()}
    ncores = 8
    nb = inp["x"].shape[0] // ncores
    if "nc" not in _CACHE:
        _CACHE["nc"] = build(nsamp=nb, nlayers=DEPTH)
    nc = _CACHE["nc"]
    maps = _prep_inputs(inp, nb, ncores)
    res = run_bass_kernel_spmd(nc, maps, core_ids=list(range(ncores)))
    outs = [r["out"][1:] for r in res.results]
    o = np.concatenate(outs, axis=0).transpose(0, 2, 1)
    return np.ascontiguousarray(o, dtype=np.float32)
```

```python
import math
import numpy as np
import concourse.bass as bass
import concourse.mybir as mybir
from concourse.bass_utils import run_bass_kernel_spmd

F32 = mybir.dt.float32
BF16 = mybir.dt.bfloat16
AF = mybir.ActivationFunctionType
ALU = mybir.AluOpType
AX = mybir.AxisListType

D = 1024
T = 2304
NCTX = 256
L = 2048
EPS = 1e-6
TILES = [(0, 256), (256, 512), (768, 512), (1280, 512), (1792, 512)]
NEGM = -1.0e4
DEPTH = 2
import os
RET_STOP = int(os.environ.get('RET_STOP', '99'))
RET_STEP = int(os.environ.get('RET_STEP', '99'))


class Ctx:
    pass


def build(nsamp=4, nlayers=2, dbg=None, stop_after=None):
    nc = bass.Bass("TRN2", target_bir_lowering=False)
    K = Ctx()

    def din(name, shape):
        return nc.dram_tensor(name, list(shape), F32, kind="ExternalInput").ap()

    xin = din("xin", [nsamp, D, T])
    cT_in = din("cT", [128, 8, 5])
    w_mod = din("w_mod", [2, D, 6 * D])
    bmod_in = din("bmod", [128, 2, 48])
    gn_in = din("gn", [128, 5, 8])
    w_in = din("w_in", [2, D, 7168])
    nab_in = din("nab", [2, 128, 4, 14, 64])
    namask_in = din("namask", [128, 64])
    retp_in = din("retp", [2, 128, 8])
    gqag_in = din("gqag", [2, 64, 4])
    dlam_in = din("dlam", [2, 128, 128])
    dsub_in = din("dsub", [64, 2])
    lamc_in = din("lamc", [128, 2, 2])
    w_branch = din("w_branch", [2, 4, 256, D])
    w_out = din("w_out", [2, D, D])
    w_router = din("w_router", [D, 16])
    rb_in = din("rb", [128, 16])
    w_gate = din("w_gate", [2, 16, D, 512])
    w_up = din("w_up", [2, 16, D, 512])
    w_down = din("w_down", [2, 16, 512, D])
    rope64_in = din("rope64", [2, 64, T])
    rope32_in = din("rope32", [2, 32, T])
    ident_in = din("ident", [128, 128])
    seld_in = din("seld", [65, 64])
    sel16_in = din("sel16", [16, 16, 128])
    retc_in = din("retc", [128, 4, 128])
    retpos_in = din("retpos", [128, 4, 128])

    out = nc.dram_tensor("out", [nsamp + 1, D, L], F32, kind="ExternalOutput").ap()
    OUTS = nc.dram_tensor("OUTS", [D, L], F32, kind="Internal").ap()
    dbg_outs = {}
    dbg_t = dbg_outs

    def dscratch(name, shape):
        kind = "ExternalOutput" if (dbg and name in dbg) else "Internal"
        t = nc.dram_tensor(name, list(shape), F32, kind=kind).ap()
        if kind == "ExternalOutput":
            dbg_outs[name] = t
        return t

    XS = dscratch("XS", [D, T])
    PT = dscratch("PT", [7168, T])
    VT = dscratch("VT", [T, 896])
    YT = dscratch("YT", [1024, T])
    AT = dscratch("AT", [D, T])
    STG = dscratch("STG", [128, T])
    ACCD = dscratch("ACCD", [D, T])
    MTD = dscratch("MTD", [D, T])
    if dbg and "X1" in dbg:
        dscratch("X1", [D, T])
    if dbg and "HT" in dbg:
        dscratch("HT", [D, T])

    from contextlib import ExitStack
    es = ExitStack()
    es.__enter__()

    _cnt = [0]

    _peak = {}

    class _SBTW:
        def __init__(self, name, shape, dt=F32):
            self.name = name
            self.cm = nc.sbuf_tensor(f"sb{_cnt[0]}_{name}", list(shape), dt)

        def __enter__(self):
            r = self.cm.__enter__()
            used = 229376 - nc.sbuf_bytes_remaining
            key = self.name.split("_")[0]
            _peak[key] = max(_peak.get(key, 0), used)
            return r

        def __exit__(self, *a):
            return self.cm.__exit__(*a)

    def SBT(name, shape, dt=F32):
        _cnt[0] += 1
        return _SBTW(name, shape, dt)

    def sb(name, shape):
        return es.enter_context(SBT(name, list(shape)))

    PS = [es.enter_context(nc.psum_tensor(f"ps{i}", [128, 512], F32)) for i in range(8)]
    dsem = es.enter_context(nc.semaphore("dsem"))

    class DQ:
        n = 0

        def start(self, out, in_):
            nc.sync.dma_start(out=out, in_=in_).then_inc(dsem, 16)
            self.n += 1

        def wait(self):
            if self.n:
                nc.sync.wait_ge(dsem, 16 * self.n)
                nc.sync.sem_clear(dsem)
                self.n = 0

    dq = DQ()

    def B():
        nc.all_engine_barrier()

    def LW():
        dq.wait()
        B()

    V = nc.vector
    A = nc.scalar
    PE = nc.tensor

    import re as _re
    from contextlib import contextmanager as _cm
    _RH = bass.RegisterHandle
    _ENGS = {"Pool": mybir.EngineType.Pool, "Activation": mybir.EngineType.Activation, "PE": mybir.EngineType.PE,
             "DVE": mybir.EngineType.DVE, "SP": mybir.EngineType.SP}
    _ALLE = mybir.ALL_ENGINES
    _lc = [0]

    def _cur_id():
        h = nc.vector.alloc_register()
        m = _re.search(r"(\d+)$", h.name)
        nc.vector.free_register(h)
        return int(m.group(1))

    @_cm
    def MyFori(start, end):
        id0 = _cur_id()
        _lc[0] += 1
        name = f"mf{_lc[0]}"
        ls, le = name + "_loop", name + "_end"
        regs = nc.alloc_registers(name + "_i", engines=_ALLE)
        nc.regs_mov(regs, start)
        nc.br(ls, engines=_ALLE)
        with nc.body(ls, valid_engines=_ALLE):
            i = nc.snap(regs, min_val=start, max_val=end - 1)
            yield i
            nc.regs_alu(regs, regs, 1, op=mybir.AluOpType.add)
            nc.br_lt(regs, end, on_true=ls, on_false=le, engines=_ALLE)
        nc.switch_bb(le)
        for h in regs.handles:
            nc.free_register(h)
        if not isinstance(i, int):
            for e in nc.engines.values():
                al = e.get_value_cache().lookup(i)
                if al is not None:
                    nc.free_register(al.val)
        id1 = _cur_id()
        for en, et in _ENGS.items():
            for k in range(id0, id1 + 1):
                nm = f"{en}_tmp_{k}"
                try:
                    r = nc.lookup_reg(nm)
                except Exception:
                    r = None
                if r is not None and getattr(r, "allocated", False):
                    nc.free_register(_RH(name=nm, engine=et))


    ident = sb("ident", [128, 128])
    ones = sb("ones", [128, 128])
    epsc = sb("epsc", [128, 1])
    MOD = sb("MOD", [128, 2, 48, 5])
    MODS = sb("MODS", [128, 2, 2, 48])
    GG = sb("GG", [128, 2, 2, 2, 8])
    GN = sb("GN", [128, 5, 8])
    BMOD = sb("BMOD", [128, 2, 48])
    SELD = sb("SELD", [65, 64])
    ZERO8 = sb("ZERO8", [128, 8])
    LAMC = sb("LAMC", [128, 2, 2])
    DSUB = sb("DSUB", [64, 2])

    dq.start(ident[:], ident_in)
    dq.start(GN[:], gn_in)
    dq.start(BMOD[:], bmod_in)
    dq.start(SELD[:], seld_in)
    dq.start(LAMC[:], lamc_in)
    dq.start(DSUB[:], dsub_in)
    V.memset(ones[:], 1.0)
    V.memset(epsc[:], EPS)
    V.memset(ZERO8[:], 0.0)
    LW()

    def stage_mods():
        with SBT("scT", [128, 8, 5]) as scT, SBT("wm", [128, 8, 128]) as wm:
            dq.start(scT[:], cT_in)
            LW()
            A.activation(out=scT[:], in_=scT[:], func=AF.Silu)
            B()
            for l in range(2):
                wv = w_mod[l].rearrange("(k p) (m j) -> p k m j", p=128, j=128)
                with MyFori(0, 48) as m:
                    dq.start(wm[:], wv[:, :, m, :])
                    LW()
                    for k in range(8):
                        PE.matmul(PS[0][:, 0:5], wm[:, k, :], scT[:, k, :], start=(k == 0), stop=(k == 7))
                    B()
                    V.tensor_scalar(out=MOD[:, l, m, :], in0=PS[0][:, 0:5], scalar1=BMOD[:, l, bass.ts(m, 1)],
                                    scalar2=None, op0=ALU.add)
                    B()

    stage_mods()

    def stage_norm(BIG, gfun, shfun):
        XSv = XS.rearrange("(k p) t -> p k t", p=128)
        with SBT("n_xt", [128, 8, 512]) as xt, SBT("n_sq", [128, 8, 512]) as sq, \
                SBT("n_rs", [128, 512]) as rs:
            for ti, (t0, n) in enumerate(TILES):
                v = 0 if ti == 0 else 1
                dq.start(xt[:, :, 0:n], XSv[:, :, t0:t0 + n])
                LW()
                A.activation(out=sq[:, :, 0:n], in_=xt[:, :, 0:n], func=AF.Square)
                B()
                for k in range(8):
                    PE.matmul(PS[0][:, 0:n], ones[:, :], sq[:, k, 0:n], start=(k == 0), stop=(k == 7))
                B()
                A.activation(out=rs[:, 0:n], in_=PS[0][:, 0:n], func=AF.Sqrt, bias=epsc[:, :], scale=1.0 / D)
                B()
                V.reciprocal(out=rs[:, 0:n], in_=rs[:, 0:n])
                B()
                for k in range(8):
                    V.scalar_tensor_tensor(out=sq[:, k, 0:n], in0=xt[:, k, 0:n], scalar=gfun(v, k), in1=rs[:, 0:n],
                                           op0=ALU.mult, op1=ALU.mult)
                B()
                for k in range(8):
                    A.activation(out=BIG[:, k, t0:t0 + n], in_=sq[:, k, 0:n], func=AF.Identity, bias=shfun(v, k),
                                 scale=1.0)
                B()

    def cast_big(BIG, BIGb):
        for k in range(8):
            if k % 3 == 0:
                A.activation(out=BIGb[:, k, :], in_=BIG[:, k, :], func=AF.Copy)
            elif k % 3 == 1:
                V.tensor_copy(out=BIGb[:, k, :], in_=BIG[:, k, :])
            else:
                nc.gpsimd.tensor_copy(out=BIGb[:, k, :], in_=BIG[:, k, :])
        B()

    def stage_gemm(BIG, wview, m_lo, m_hi, orow0, dst, epi):
        dstv = dst.rearrange("(m p) t -> m p t", p=128)
        with SBT("g_w", [128, 8, 128]) as wt, SBT("g_o", [128, T]) as ot, \
                SBT("g_x", [128, T]) as xr, SBT("g_wb", [128, 8, 128], BF16) as wtb:
            with MyFori(m_lo, m_hi) as m:
                dq.start(wt[:], wview[:, :, m, :])
                if epi[0] == "resid":
                    dq.start(xr[:], dstv[m + orow0])
                LW()
                V.tensor_copy(out=wtb[:], in_=wt[:])
                B()
                for ti, (t0, n) in enumerate(TILES):
                    for k in range(8):
                        PE.matmul(PS[ti][:, 0:n], wtb[:, k, :], BIG[:, k, t0:t0 + n], start=(k == 0), stop=(k == 7))
                B()
                for ti, (t0, n) in enumerate(TILES):
                    if epi[0] == "copy":
                        if ti % 2 == 0:
                            A.activation(out=ot[:, t0:t0 + n], in_=PS[ti][:, 0:n], func=AF.Copy)
                        else:
                            V.tensor_copy(out=ot[:, t0:t0 + n], in_=PS[ti][:, 0:n])
                    elif epi[0] == "sigmoid":
                        A.activation(out=ot[:, t0:t0 + n], in_=PS[ti][:, 0:n], func=AF.Sigmoid)
                    elif epi[0] == "resid":
                        v = 0 if ti == 0 else 1
                        V.scalar_tensor_tensor(out=ot[:, t0:t0 + n], in0=PS[ti][:, 0:n], scalar=epi[1](v, m),
                                               in1=xr[:, t0:t0 + n], op0=ALU.mult, op1=ALU.add)
                B()
                dq.start(dstv[m + orow0], ot[:])
                LW()

    VCOLS = [(512, 256), (1280, 256), (2176, 128), (2816, 256)]

    def stage_vtok(BIG, l):
        VTv = VT.rearrange("(c p) f -> c p f", p=128)
        wl = w_in[l].rearrange("(k p) f -> p k f", p=128)
        with SBT("v_w", [128, 8, 896]) as wv, SBT("v_o", [128, 896]) as vo, SBT("v_stg", [128, 8, 128]) as stg:
            c0 = 0
            for (col0, wd) in VCOLS:
                dq.start(wv[:, :, c0:c0 + wd], wl[:, :, col0:col0 + wd])
                c0 += wd
            LW()
            with MyFori(0, 18) as tc:
                V.tensor_copy(out=stg[:], in_=BIG[:, :, bass.ts(tc, 128)])
                B()
                for k in range(8):
                    PE.matmul(PS[0][:, 0:512], stg[:, k, :], wv[:, k, 0:512], start=(k == 0), stop=(k == 7))
                for k in range(8):
                    PE.matmul(PS[1][:, 0:384], stg[:, k, :], wv[:, k, 512:896], start=(k == 0), stop=(k == 7))
                B()
                A.activation(out=vo[:, 0:512], in_=PS[0][:, 0:512], func=AF.Copy)
                V.tensor_copy(out=vo[:, 512:896], in_=PS[1][:, 0:384])
                B()
                dq.start(VTv[tc], vo[:])
                LW()

    def prep(dst, blk, d, tabs, bufs, gains=None, norm=False):
        q = d // 4
        Ab, Bb, t1, t2 = bufs
        if callable(blk):
            rb = blk
        else:
            PTq = PT.rearrange("(a p) t -> a p t", p=q)
            rb = lambda i: PTq[blk + i]
        for i in range(4):
            dq.start(Ab[i * q:(i + 1) * q, :], rb(i))
        for i, j in enumerate((1, 0, 3, 2)):
            dq.start(Bb[i * q:(i + 1) * q, :], rb(j))
        LW()
        C, S = tabs
        if norm:
            A.activation(out=t2[0:d, :], in_=Ab[0:d, :], func=AF.Square)
            B()
            for ti, (t0, n) in enumerate(TILES):
                PE.matmul(PS[ti][0:d, 0:n], ones[0:d, 0:d], t2[0:d, t0:t0 + n], start=True, stop=True)
            B()
            for ti, (t0, n) in enumerate(TILES):
                A.activation(out=dst[0:d, t0:t0 + n], in_=PS[ti][0:d, 0:n], func=AF.Sqrt, bias=epsc[0:d, :], scale=1.0 / d)
            B()
            V.reciprocal(out=dst[0:d, :], in_=dst[0:d, :])
        if gains is not None:
            V.tensor_scalar(out=Ab[0:d, :], in0=Ab[0:d, :], scalar1=gains[0], scalar2=None, op0=ALU.mult)
            V.tensor_scalar(out=Bb[0:d, :], in0=Bb[0:d, :], scalar1=gains[1], scalar2=None, op0=ALU.mult)
        B()
        if t1 is None:
            t1, t2 = Ab, Bb
        V.tensor_tensor(out=t1[0:d, :], in0=Ab[0:d, :], in1=C, op=ALU.mult)
        V.tensor_tensor(out=t2[0:d, :], in0=Bb[0:d, :], in1=S, op=ALU.mult)
        B()
        if norm:
            V.tensor_tensor(out=t1[0:d, :], in0=t1[0:d, :], in1=t2[0:d, :], op=ALU.add)
            B()
            V.tensor_tensor(out=dst[0:d, :], in0=t1[0:d, :], in1=dst[0:d, :], op=ALU.mult)
        else:
            V.tensor_tensor(out=dst[0:d, :], in0=t1[0:d, :], in1=t2[0:d, :], op=ALU.add)
        B()

    def load_vaug(Vb, col0, nch=18, tok0=0):
        src = VT[tok0:tok0 + nch * 128, :].rearrange("(c p) f -> p c f", p=128)
        dq.start(Vb[:, 0:nch, 0:64], src[:, :, col0:col0 + 64] if isinstance(col0, int) else None)

    def attn_core(QRq, KR, dqk, n, kchunks, vfun, scale, oa_bank, E):
        groups = [kchunks[i:i + 4] for i in range(0, len(kchunks), 4)]
        npv = len(kchunks)
        pv_i = 0
        prev = None
        for g in groups + [None]:
            if prev is not None:
                for j, c in enumerate(prev):
                    PE.matmul(PS[oa_bank][0:65, 0:n], vfun(c), E[:, j, 0:n], start=(pv_i == 0), stop=(pv_i == npv - 1))
                    pv_i += 1
            if g is not None:
                for j, c in enumerate(g):
                    PE.matmul(PS[j][:, 0:n], KR[0:dqk, c * 128:(c + 1) * 128], QRq, start=True, stop=True)
            B()
            if g is not None:
                for j, c in enumerate(g):
                    A.activation(out=E[:, j, 0:n], in_=PS[j][:, 0:n], func=AF.Exp, scale=scale)
                B()
            prev = g

    def attn_finish(oa_bank, n, OAS, RZ, YO, zbank=6):
        A.activation(out=OAS[0:65, 0:n], in_=PS[oa_bank][0:65, 0:n], func=AF.Copy)
        B()
        PE.matmul(PS[zbank][0:64, 0:n], SELD[0:65, 0:64], OAS[0:65, 0:n], start=True, stop=True)
        B()
        V.reciprocal(out=RZ[0:64, 0:n], in_=PS[zbank][0:64, 0:n])
        B()
        V.tensor_tensor(out=YO[0:64, 0:n], in0=OAS[0:64, 0:n], in1=RZ[0:64, 0:n], op=ALU.mult)
        B()

    ALLK = list(range(18))
    YT64 = YT.rearrange("(a p) t -> a p t", p=64)
    PT64 = PT.rearrange("(a p) t -> a p t", p=64)
    VTc = VT.rearrange("(c p) f -> p c f", p=128)

    def stage_gqa(l):
        with ExitStack() as s2:
            def t(name, shape):
                return s2.enter_context(SBT(name, list(shape)))
            RC = t("q_rc", [64, T]); RS = t("q_rs", [64, T])
            Ab = t("q_a", [64, T]); Bb = t("q_b", [64, T]); t1 = t("q_t1", [64, T]); t2 = t("q_t2", [64, T])
            KR = t("q_kr", [64, T]); QR = t("q_qr", [64, T]); Vb = t("q_v", [128, 18, 65])
            E = t("q_e", [128, 4, 512]); OAS = t("q_oas", [65, 512]); RZ = t("q_rz", [64, 512]); YO = t("q_yo", [64, 512])
            GQ = t("q_g", [64, 4]); YH = t("q_yh", [64, T])
            dq.start(RC[:], rope64_in[0]); dq.start(RS[:], rope64_in[1]); dq.start(GQ[:], gqag_in[l])
            LW()
            bufs = (Ab, Bb, t1, t2)
            for kv in range(2):
                prep(KR, (2048 + kv * 64) // 16, 64, (RC[:], RS[:]), bufs, gains=(GQ[:, 2:3], GQ[:, 3:4]), norm=True)
                dq.start(Vb[:, :, 0:64], VTc[:, :, 512 + kv * 64:512 + kv * 64 + 64])
                V.memset(Vb[:, :, 64:65], 1.0)
                LW()
                for hh in range(2):
                    dq.start(STG[0:64, :], PT64[hh + (1792 + kv * 128) // 64])
                    LW()
                    prep(QR, lambda i: STG.rearrange("(a p) t -> a p t", p=16)[i], 64, (RC[:], RS[:]), bufs, gains=(GQ[:, 0:1], GQ[:, 1:2]), norm=True)
                    QRl = QR[:, NCTX:T]
                    with MyFori(0, 4) as qt:
                        attn_core(QRl[:, bass.ts(qt, 512)], KR, 64, 512, ALLK, lambda c: Vb[:, c, :], 0.125, 4, E)
                        attn_finish(4, 512, OAS, RZ, YO)
                        V.tensor_copy(out=YH[:, NCTX:T][:, bass.ts(qt, 512)], in_=YO[:, :])
                        B()
                    attn_core(QR[:, 0:NCTX], KR, 64, NCTX, [0, 1], lambda c: Vb[:, c, :], 0.125, 4, E)
                    attn_finish(4, NCTX, OAS, RZ, YO)
                    V.tensor_copy(out=YH[:, 0:NCTX], in_=YO[:, 0:NCTX])
                    B()
                    dq.start(YT64[8 + kv * 2 + hh], YH[:])
                    LW()

    def stage_diff(l):
        lam_init = 0.8 - 0.6 * math.exp(-0.3 * l)
        with ExitStack() as s2:
            def t(name, shape):
                return s2.enter_context(SBT(name, list(shape)))
            RC = t("d_rc", [32, T]); RS = t("d_rs", [32, T])
            Ab = t("d_a", [32, T]); Bb = t("d_b", [32, T]); t1 = None; t2 = None
            QK = [t(f"d_qk{i}", [32, T]) for i in range(4)]
            Vb = t("d_v", [128, 18, 65])
            E = t("d_e", [128, 4, 512]); OAS = t("d_oas", [65, 512]); RZ = t("d_rz", [64, 512])
            Y1 = t("d_y1", [64, 512]); Y2 = t("d_y2", [64, 512]); SQ = t("d_sq", [64, 512])
            LM = t("d_lm", [128, 128]); LV = t("d_lv", [128, 4]); YH = t("d_yh", [64, T])
            dq.start(RC[:], rope32_in[0]); dq.start(RS[:], rope32_in[1]); dq.start(LM[:], dlam_in[l])
            LW()
            V.tensor_tensor(out=LM[:, 0:32], in0=LM[:, 0:32], in1=LM[:, 32:64], op=ALU.mult)
            V.tensor_tensor(out=LM[:, 64:96], in0=LM[:, 64:96], in1=LM[:, 96:128], op=ALU.mult)
            B()
            V.reduce_sum(out=LV[:, 0:1], in_=LM[:, 0:32], axis=AX.X)
            V.reduce_sum(out=LV[:, 1:2], in_=LM[:, 64:96], axis=AX.X)
            B()
            A.activation(out=LV[:, 0:2], in_=LV[:, 0:2], func=AF.Exp)
            B()
            V.tensor_tensor(out=LV[:, 2:3], in0=LV[:, 1:2], in1=LV[:, 0:1], op=ALU.subtract)
            B()
            V.tensor_scalar(out=LV[:, 2:3], in0=LV[:, 2:3], scalar1=-lam_init, scalar2=None, op0=ALU.add)
            B()
            bufs = (Ab, Bb, t1, t2)
            sc = 32 ** -0.5
            with MyFori(0, 4) as h:
                dq.start(YT64[h + 11], YH[:])
                dq.start(STG[0:64, :], PT64[h + 36]); dq.start(STG[64:128, :], PT64[h + 40])
                LW()
                prep(QK[0], lambda i: STG.rearrange("(a p) t -> a p t", p=8)[0 + i], 32, (RC[:], RS[:]), bufs)
                prep(QK[1], lambda i: STG.rearrange("(a p) t -> a p t", p=8)[4 + i], 32, (RC[:], RS[:]), bufs)
                prep(QK[2], lambda i: STG.rearrange("(a p) t -> a p t", p=8)[8 + i], 32, (RC[:], RS[:]), bufs)
                prep(QK[3], lambda i: STG.rearrange("(a p) t -> a p t", p=8)[12 + i], 32, (RC[:], RS[:]), bufs)
                VTh = VT[:, 640:896].rearrange("(c p) (h f) -> h p c f", p=128, f=64)
                dq.start(Vb[:, :, 0:64], VTh[h])
                V.memset(Vb[:, :, 64:65], 1.0)
                LW()

                def one_tile(q0sl, n, kch, dst):
                    attn_core(QK[0][:, q0sl] if not callable(q0sl) else q0sl(QK[0]), QK[2], 32, n, kch, lambda c: Vb[:, c, :], sc, 4, E)
                    attn_finish(4, n, OAS, RZ, Y1)
                    attn_core(QK[1][:, q0sl] if not callable(q0sl) else q0sl(QK[1]), QK[3], 32, n, kch, lambda c: Vb[:, c, :], sc, 5, E)
                    attn_finish(5, n, OAS, RZ, Y2)
                    V.scalar_tensor_tensor(out=Y1[:, 0:n], in0=Y2[:, 0:n], scalar=LV[0:64, 2:3], in1=Y1[:, 0:n],
                                           op0=ALU.mult, op1=ALU.add)
                    B()
                    A.activation(out=SQ[:, 0:n], in_=Y1[:, 0:n], func=AF.Square)
                    B()
                    PE.matmul(PS[6][0:64, 0:n], ones[0:64, 0:64], SQ[:, 0:n], start=True, stop=True)
                    B()
                    A.activation(out=SQ[:, 0:n], in_=PS[6][0:64, 0:n], func=AF.Sqrt, bias=epsc[0:64, :], scale=1.0 / 64)
                    B()
                    V.reciprocal(out=SQ[:, 0:n], in_=SQ[:, 0:n])
                    B()
                    V.scalar_tensor_tensor(out=Y2[:, 0:n], in0=Y1[:, 0:n], scalar=DSUB[:, l:l + 1], in1=SQ[:, 0:n],
                                           op0=ALU.mult, op1=ALU.mult)
                    B()
                    A.activation(out=Y2[:, 0:n], in_=Y2[:, 0:n], func=AF.Copy, scale=(1.0 - lam_init))
                    B()
                    V.tensor_copy(out=dst, in_=Y2[:, 0:n])
                    B()

                with MyFori(0, 4) as qt:
                    one_tile(lambda Q: Q[:, NCTX:T][:, bass.ts(qt, 512)], 512, ALLK,
                             YH[:, NCTX:T][:, bass.ts(qt, 512)])
                one_tile(slice(0, NCTX), NCTX, [0, 1], YH[:, 0:NCTX])
            dq.start(YT64[15], YH[:])
            LW()

    def stage_na(l):
        with ExitStack() as s2:
            def t(name, shape):
                return s2.enter_context(SBT(name, list(shape)))
            NAB = t("a_nab", [128, 4, 14, 64]); NABh = t("a_nabh", [128, 14, 64]); MSK = t("a_msk", [128, 64])
            QT = t("a_q", [64, T]); KT = t("a_k", [64, T])
            Ve = t("a_ve", [128, 18, 65]); Vo = t("a_vo", [128, 17, 65])
            E = t("a_e", [128, 6, 64]); OAll = t("a_oall", [65, L])
            E4 = t("a_e4", [128, 4, 512])
            OAS = t("a_oas", [65, 512]); RZ = t("a_rz", [64, 512]); YO = t("a_yo", [64, 512])
            dq.start(NAB[:], nab_in[l]); dq.start(MSK[:], namask_in)
            LW()
            for hh in range(4):
                for dd in range(14):
                    V.tensor_tensor(out=NAB[:, hh, dd, :], in0=NAB[:, hh, dd, :], in1=MSK[:, :], op=ALU.add)
            B()
            VT_e = VT[:, 0:256].rearrange("(c p) (h f) -> h p c f", p=128, f=64)
            VT_o = VT[64:64 + 17 * 128, 0:256].rearrange("(c p) (h f) -> h p c f", p=128, f=64)
            with MyFori(0, 4) as h:
                dq.start(QT[:], PT64[h]); dq.start(KT[:], PT64[4 + h])
                dq.start(Ve[:, :, 0:64], VT_e[h]); dq.start(Vo[:, :, 0:64], VT_o[h])
                V.memset(Ve[:, :, 64:65], 1.0)
                V.memset(Vo[:, :, 64:65], 1.0)
                V.tensor_copy(out=NABh[:], in_=NAB[:, h, :, :])
                LW()

                def row(qcols, r0tok, off, vsel, slot):
                    for j in range(4):
                        PE.matmul(PS[0][:, j * 64:(j + 1) * 64], r0tok(j), qcols, start=True, stop=True)
                    for j in range(2):
                        PE.matmul(PS[0][:, (4 + j) * 64:(5 + j) * 64], KT[:, j * 128:(j + 1) * 128], qcols, start=True, stop=True)
                    B()
                    b0 = 7 - off
                    A.activation(out=E[:, 0:4, :], in_=PS[0][:, 0:256].rearrange("p (j c) -> p j c", c=64), func=AF.Copy, scale=0.125)
                    A.activation(out=E[:, 4:6, :], in_=PS[0][:, 256:384].rearrange("p (j c) -> p j c", c=64), func=AF.Exp, scale=0.125)
                    B()
                    for j in range(4):
                        V.tensor_tensor(out=E[:, j, :], in0=E[:, j, :], in1=NABh[:, b0 + 2 * j, :], op=ALU.add)
                    B()
                    A.activation(out=E[:, 0:4, :], in_=E[:, 0:4, :], func=AF.Exp)
                    B()
                    for j in range(6):
                        lhs = vsel(j) if j < 4 else Ve[:, j - 4, :]
                        PE.matmul(PS[1][0:65, slot * 64:(slot + 1) * 64], lhs, E[:, j, :], start=(j == 0), stop=(j == 5))
                    B()

                def static_row(r):
                    r0 = min(max(r - 4, 0), 24)
                    off = r - r0
                    if r0 % 2 == 0:
                        vs = lambda j: Ve[:, 2 + r0 // 2 + j, :]
                    else:
                        vs = lambda j: Vo[:, (r0 + 3) // 2 + j, :]
                    row(QT[:, NCTX + r * 64:NCTX + (r + 1) * 64],
                        lambda j: KT[:, NCTX + (r0 + 2 * j) * 64:NCTX + (r0 + 2 * j) * 64 + 128], off, vs, r % 2)
                    A.activation(out=OAll[:, r * 64:(r + 1) * 64], in_=PS[1][0:65, (r % 2) * 64:(r % 2 + 1) * 64], func=AF.Copy)

                for r in range(32):
                    static_row(r)
                B()
                for qt in range(4):
                    PE.matmul(PS[2 + qt][0:64, 0:512], SELD[0:65, 0:64], OAll[0:65, qt * 512:(qt + 1) * 512], start=True, stop=True)
                B()
                for qt in range(4):
                    V.reciprocal(out=E4[0:64, qt, :], in_=PS[2 + qt][0:64, 0:512])
                B()
                for qt in range(4):
                    V.tensor_tensor(out=OAll[0:64, qt * 512:(qt + 1) * 512], in0=OAll[0:64, qt * 512:(qt + 1) * 512],
                                    in1=E4[0:64, qt, :], op=ALU.mult)
                B()
                dq.start(YT64[h][:, NCTX:T], OAll[0:64, :])
                LW()
                attn_core(QT[:, 0:NCTX], KT, 64, NCTX, [0, 1], lambda c: Ve[:, c, :], 0.125, 4, E4)
                attn_finish(4, NCTX, OAS, RZ, YO)
                dq.start(YT64[h][:, 0:NCTX], YO[:, 0:NCTX])
                LW()

    def stage_ret(l):
        with ExitStack() as s2:
            def t(name, shape):
                return s2.enter_context(SBT(name, list(shape)))
            QR = t("r_qr", [64, 4, T]); KR = t("r_kr", [64, 4, T])
            RP = t("r_rp", [128, 8]); LG = t("r_lg", [128, 8])
            INTRA = t("r_intra", [128, 8, 128]); QDEC = t("r_qdec", [64, 8, 128]); KDEC = t("r_kdec", [128, 8, 64])
            CDv = t("r_cd", [64, 8]); KD1 = t("r_kd1", [128, 8])
            ST = t("r_st", [64, 8, 64]); Qd = t("r_qd", [64, 8, 128]); Am = t("r_am", [128, 8, 128]); Kd = t("r_kdm", [128, 8, 64])
            s_tab = ExitStack()
            RCn = s_tab.enter_context(SBT("r_rcn", [128, 4, 128])); RPOS = s_tab.enter_context(SBT("r_rpos", [128, 4, 128]))
            dq.start(RP[:], retp_in[l])
            dq.start(RCn[:], retc_in); dq.start(RPOS[:], retpos_in)
            LW()
            A.activation(out=LG[:], in_=RP[:], func=AF.Exp)
            B()
            V.tensor_scalar(out=LG[:], in0=LG[:], scalar1=-1.0, scalar2=1.0, op0=ALU.mult, op1=ALU.add)
            B()
            A.activation(out=LG[:], in_=LG[:], func=AF.Ln)
            B()
            for hd in range(8):
                dr = hd // 4
                A.activation(out=INTRA[:, hd, :], in_=RCn[:, 2 * dr, :], func=AF.Exp, scale=LG[:, hd:hd + 1])
                A.activation(out=QDEC[:, hd, :], in_=RPOS[0:64, dr, :], func=AF.Exp, scale=LG[0:64, hd:hd + 1])
                A.activation(out=KD1[:, hd:hd + 1], in_=RPOS[:, 2 + dr, 0:1], func=AF.Exp, scale=LG[:, hd:hd + 1])
            A.activation(out=CDv[:], in_=LG[0:64, :], func=AF.Exp, scale=128.0)
            B()
            for hd in range(8):
                dr = hd // 4
                V.scalar_tensor_tensor(out=INTRA[:, hd, :], in0=INTRA[:, hd, :], scalar=0.125, in1=RCn[:, 2 * dr + 1, :], op0=ALU.mult, op1=ALU.mult)
                V.tensor_scalar(out=KDEC[:, hd, :], in0=ones[:, 0:64], scalar1=KD1[:, hd:hd + 1], scalar2=0.125,
                                op0=ALU.mult, op1=ALU.mult)
            V.memset(ST[:], 0.0)
            B()
            s_tab.close()
            with ExitStack() as sp:
                def tp(name, shape):
                    return sp.enter_context(SBT(name, list(shape)))
                RC = tp("r_rc", [64, T]); RS = tp("r_rs", [64, T])
                Ab = tp("r_a", [64, T]); Bb = tp("r_b", [64, T]); t1 = None; t2 = None
                dq.start(RC[:], rope64_in[0]); dq.start(RS[:], rope64_in[1])
                LW()
                bufs = (Ab, Bb, t1, t2)
                for hh in range(4):
                    prep(QR[:, hh, :], (768 + hh * 64) // 16, 64, (RC[:], RS[:]), bufs)
                    prep(KR[:, hh, :], (1024 + hh * 64) // 16, 64, (RC[:], RS[:]), bufs)

            Vs = t("r_vs", [128, 2, 256])
            OAcc = t("r_oacc", [64, 4, T])
            V.memset(OAcc[:], 0.0)
            B()

            def step(cf, cb):
                csl = [slice(cf * 128, (cf + 1) * 128), slice(cb * 128, (cb + 1) * 128)]
                cidx = [cf, cb]
                dq.start(Vs[:, 0, :], VTc[:, cf, 256:512]); dq.start(Vs[:, 1, :], VTc[:, cb, 256:512])
                LW()
                for hd in range(8):
                    dr, hh = hd // 4, hd % 4
                    PE.matmul(PS[dr][:, hh * 128:(hh + 1) * 128], KR[:, hh, csl[dr]], QR[:, hh, csl[dr]], start=True, stop=True)
                    PE.matmul(PS[2][:, hd * 64:(hd + 1) * 64], KR[:, hh, csl[dr]], ident[0:64, 0:64], start=True, stop=True)
                for dr in range(2):
                    V.tensor_tensor(out=Qd[:, dr * 4:(dr + 1) * 4, :], in0=QR[:, :, csl[dr]], in1=QDEC[:, dr * 4:(dr + 1) * 4, :], op=ALU.mult)
                B()
                for dr in range(2):
                    V.tensor_tensor(out=Am[:, dr * 4:(dr + 1) * 4, :], in0=PS[dr][:, :].rearrange("p (h c) -> p h c", c=128),
                                    in1=INTRA[:, dr * 4:(dr + 1) * 4, :], op=ALU.mult)
                V.tensor_tensor(out=Kd[:], in0=PS[2][:, :].rearrange("p (h c) -> p h c", c=64), in1=KDEC[:], op=ALU.mult)
                B()
                for hd in range(8):
                    dr, hh = hd // 4, hd % 4
                    vch = Vs[:, dr, hh * 64:(hh + 1) * 64]
                    ob = PS[3 + dr][0:64, hh * 128:(hh + 1) * 128]
                    PE.matmul(ob, vch, Am[:, hd, :], start=True, stop=True)
                    PE.matmul(PS[6 + dr][0:64, hh * 128:(hh + 1) * 128], ST[:, hd, :], Qd[:, hd, :], start=True, stop=True)
                    PE.matmul(PS[5][0:64, hd * 64:(hd + 1) * 64], Kd[:, hd, :], vch, start=True, stop=True)
                B()
                for hd in range(8):
                    V.scalar_tensor_tensor(out=ST[:, hd, :], in0=ST[:, hd, :], scalar=CDv[:, hd:hd + 1],
                                           in1=PS[5][0:64, hd * 64:(hd + 1) * 64], op0=ALU.mult, op1=ALU.add)
                for dr in range(2):
                    V.tensor_tensor(out=OAcc[:, :, csl[dr]], in0=OAcc[:, :, csl[dr]],
                                    in1=PS[3 + dr][0:64, :].rearrange("p (h c) -> p h c", c=128), op=ALU.add)
                B()
                for dr in range(2):
                    V.tensor_tensor(out=OAcc[:, :, csl[dr]], in0=OAcc[:, :, csl[dr]],
                                    in1=PS[6 + dr][0:64, :].rearrange("p (h c) -> p h c", c=128), op=ALU.add)
                B()

            if not os.environ.get("RET_SKIPSTEPS"):
                step(0, 1)
                step(1, 0)
                for j in range(16):
                    step(j + 2, 17 - j)
            with ExitStack() as s3:
                G = s3.enter_context(SBT("r_g", [64, 512]))
                SQ = s3.enter_context(SBT("r_sq", [64, 512]))
                CEN = s3.enter_context(SBT("r_cen", [64, 512]))
                MSQ = s3.enter_context(SBT("r_msq", [64, 512]))
                for hh in range(4):
                    for ti, (t0, n) in enumerate(TILES):
                        O = OAcc[:, hh, t0:t0 + n]
                        dq.start(G[:, 0:n], PT64[(1536 // 64) + hh][:, t0:t0 + n])
                        A.activation(out=SQ[:, 0:n], in_=O, func=AF.Square)
                        LW()
                        A.activation(out=G[:, 0:n], in_=G[:, 0:n], func=AF.Silu)
                        PE.matmul(PS[0][0:64, 0:n], ones[0:64, 0:64], O, start=True, stop=True)
                        PE.matmul(PS[1][0:64, 0:n], ones[0:64, 0:64], SQ[:, 0:n], start=True, stop=True)
                        B()
                        A.activation(out=CEN[:, 0:n], in_=PS[0][0:64, 0:n], func=AF.Copy, scale=1.0 / 64)
                        A.activation(out=SQ[:, 0:n], in_=PS[1][0:64, 0:n], func=AF.Copy, scale=1.0 / 64)
                        B()
                        A.activation(out=MSQ[:, 0:n], in_=CEN[:, 0:n], func=AF.Square)
                        B()
                        V.tensor_tensor(out=CEN[:, 0:n], in0=O, in1=CEN[:, 0:n], op=ALU.subtract)
                        V.tensor_tensor(out=MSQ[:, 0:n], in0=SQ[:, 0:n], in1=MSQ[:, 0:n], op=ALU.subtract)
                        B()
                        A.activation(out=MSQ[:, 0:n], in_=MSQ[:, 0:n], func=AF.Sqrt, bias=epsc[0:64, :], scale=1.0)
                        B()
                        V.reciprocal(out=MSQ[:, 0:n], in_=MSQ[:, 0:n])
                        B()
                        V.tensor_tensor(out=CEN[:, 0:n], in0=CEN[:, 0:n], in1=MSQ[:, 0:n], op=ALU.mult)
                        B()
                        V.tensor_tensor(out=G[:, 0:n], in0=CEN[:, 0:n], in1=G[:, 0:n], op=ALU.mult)
                        B()
                        dq.start(YT64[4 + hh][:, t0:t0 + n], G[:, 0:n])
                        LW()

    def stage_merge(l):
        wb = w_branch[l].rearrange("n (h p) (m j) -> p n h m j", p=64, j=128)
        GTv = PT[3072:7168, :].rearrange("(n m p) t -> m p n t", n=4, p=128)
        YTv = YT.rearrange("(a p) t -> p a t", p=64)
        ATv = AT.rearrange("(m p) t -> m p t", p=128)
        with ExitStack() as s2:
            def t(name, shape):
                return s2.enter_context(SBT(name, list(shape)))
            Y = t("m_y", [64, 16, 512]); W = t("m_w", [64, 4, 4, 128]); Gt = t("m_g", [128, 4, 512])
            Tm = t("m_t", [128, 4, 512])
            for ti, (t0, n) in enumerate(TILES):
                dq.start(Y[:, :, 0:n], YTv[:, :, t0:t0 + n])
                LW()
                with MyFori(0, 8) as m:
                    for nb in range(4):
                        dq.start(W[:, nb, :, :], wb[:, nb, :, m, :])
                    dq.start(Gt[:, :, 0:n], GTv[m][:, :, t0:t0 + n])
                    LW()
                    for nb in range(4):
                        for hh in range(4):
                            PE.matmul(PS[nb][:, 0:n], W[:, nb, hh, :], Y[:, nb * 4 + hh, 0:n], start=(hh == 0), stop=(hh == 3))
                    B()
                    for nb in range(4):
                        V.tensor_tensor(out=Tm[:, nb, 0:n], in0=PS[nb][:, 0:n], in1=Gt[:, nb, 0:n], op=ALU.mult)
                    B()
                    V.tensor_tensor(out=Tm[:, 0, 0:n], in0=Tm[:, 0, 0:n], in1=Tm[:, 1, 0:n], op=ALU.add)
                    V.tensor_tensor(out=Tm[:, 2, 0:n], in0=Tm[:, 2, 0:n], in1=Tm[:, 3, 0:n], op=ALU.add)
                    B()
                    V.tensor_tensor(out=Tm[:, 0, 0:n], in0=Tm[:, 0, 0:n], in1=Tm[:, 2, 0:n], op=ALU.add)
                    B()
                    dq.start(ATv[m][:, t0:t0 + n], Tm[:, 0, 0:n])
                    LW()

    def stage_moe(l):
        XSv = XS.rearrange("(k p) t -> p k t", p=128)
        with ExitStack() as s2:
            def t(name, shape):
                return s2.enter_context(SBT(name, list(shape)))
            WT = t("e_wt", [16, T])
            with ExitStack() as s3:
                BIG = s3.enter_context(SBT("e_big", [128, 8, T]))
                stage_norm(BIG, lambda v, k: GG[:, v, 1, l, k:k + 1], lambda v, k: MODS[:, v, l, 24 + k:25 + k])
                dq.start(MTD.rearrange("(k p) t -> p k t", p=128), BIG[:])
                LW()
                WR = s3.enter_context(SBT("e_wr", [128, 8, 16]))
                RB = s3.enter_context(SBT("e_rb", [128, 16]))
                S = s3.enter_context(SBT("e_s", [128, 18, 16]))
                SEL = s3.enter_context(SBT("e_sel", [128, 18, 16]))
                SEL2 = s3.enter_context(SBT("e_sel2", [128, 18, 16]))
                EM = s3.enter_context(SBT("e_em", [128, 18, 16]))
                M1 = s3.enter_context(SBT("e_m1", [128, 72]))
                M2 = s3.enter_context(SBT("e_m2", [128, 72]))
                GS = s3.enter_context(SBT("e_gs", [128, 72]))
                GM = s3.enter_context(SBT("e_gm", [128, 72]))
                GX = s3.enter_context(SBT("e_gx", [128, 18]))
                dq.start(WR[:], w_router.rearrange("(k p) e -> p k e", p=128))
                dq.start(RB[:], rb_in)
                LW()
                for tc in range(18):
                    for k in range(8):
                        PE.matmul(PS[0][:, tc * 16:(tc + 1) * 16], BIG[:, k, tc * 128:(tc + 1) * 128], WR[:, k, :], start=(k == 0), stop=(k == 7))
                B()
                A.activation(out=S[:].rearrange("p c e -> p (c e)"), in_=PS[0][:, 0:288], func=AF.Sigmoid)
                B()
                for c in range(18):
                    V.tensor_tensor(out=SEL[:, c, :], in0=S[:, c, :], in1=RB[:], op=ALU.add)
                B()
                sel4 = SEL[:].rearrange("p c (g i) -> p (c g) i", i=4)
                sel24 = SEL2[:].rearrange("p c (g i) -> p (c g) i", i=4)
                em4 = EM[:].rearrange("p c (g i) -> p (c g) i", i=4)
                gs3 = GS[:].rearrange("p (c g) -> p c g", g=4)
                gm3 = GM[:].rearrange("p (c g) -> p c g", g=4)
                V.reduce_max(out=M1[:], in_=sel4, axis=AX.X)
                B()
                for i in range(4):
                    V.tensor_tensor(out=em4[:, :, i], in0=sel4[:, :, i], in1=M1[:, :], op=ALU.is_equal)
                B()
                V.tensor_scalar(out=sel24, in0=em4, scalar1=-1.0e9, scalar2=None, op0=ALU.mult)
                B()
                V.tensor_tensor(out=sel24, in0=sel24, in1=sel4, op=ALU.add)
                B()
                V.reduce_max(out=M2[:], in_=sel24, axis=AX.X)
                B()
                V.tensor_tensor(out=GS[:], in0=M1[:], in1=M2[:], op=ALU.add)
                B()
                V.reduce_max(out=GX[:], in_=gs3, axis=AX.X)
                B()
                for g in range(4):
                    V.tensor_tensor(out=gm3[:, :, g], in0=gs3[:, :, g], in1=GX[:, :], op=ALU.is_equal)
                for i in range(4):
                    V.tensor_tensor(out=em4[:, :, i], in0=sel4[:, :, i], in1=M2[:, :], op=ALU.is_ge)
                B()
                for i in range(4):
                    V.tensor_tensor(out=em4[:, :, i], in0=em4[:, :, i], in1=GM[:, :], op=ALU.mult)
                B()
                V.tensor_tensor(out=EM[:], in0=EM[:], in1=S[:], op=ALU.mult)
                B()
                V.reduce_sum(out=GX[:], in_=EM[:], axis=AX.X)
                B()
                V.reciprocal(out=GX[:], in_=GX[:])
                B()
                for ee in range(16):
                    V.tensor_tensor(out=EM[:, :, ee], in0=EM[:, :, ee], in1=GX[:, :], op=ALU.mult)
                B()
                for c in range(18):
                    PE.matmul(PS[c // 4][0:16, (c % 4) * 128:(c % 4 + 1) * 128], EM[:, c, :], ident[:, :], start=True, stop=True)
                B()
                for c in range(18):
                    A.activation(out=WT[:, c * 128:(c + 1) * 128], in_=PS[c // 4][0:16, (c % 4) * 128:(c % 4 + 1) * 128], func=AF.Copy)
                B()
            with ExitStack() as s3:
                def t3(name, shape):
                    return s3.enter_context(SBT(name, list(shape)))
                WG = t3("e_wg", [128, 8, 512]); WU = t3("e_wu", [128, 8, 512]); WD = t3("e_wd", [128, 4, D])
                WGb = s3.enter_context(SBT("e_wgb", [128, 8, 512], BF16)); WUb = s3.enter_context(SBT("e_wub", [128, 8, 512], BF16))
                WDb = s3.enter_context(SBT("e_wdb", [128, 4, D], BF16))
                MTb = s3.enter_context(SBT("e_mtb", [128, 8, 512], BF16)); HIDb = s3.enter_context(SBT("e_hidb", [128, 4, 512], BF16))
                SELS = t3("e_sels", [16, 128]); WBC = t3("e_wbc", [128, T])
                MTt = t3("e_mt", [128, 8, 512]); SG = t3("e_sg", [128, 4, 512]); HID = SG; WBCt = t3("e_wbct", [128, 512]); ACCt = t3("e_acct", [128, 8, 512])
                ATk = MTD.rearrange("(k p) t -> p k t", p=128)
                ACCk = ACCD.rearrange("(k p) t -> p k t", p=128)
                V.memset(ACCt[:], 0.0)
                B()
                for ti, (t0, n) in enumerate(TILES):
                    dq.start(ACCk[:, :, t0:t0 + n], ACCt[:, :, 0:n])
                LW()
                wgv = w_gate[l].rearrange("e (k p) f -> e p k f", p=128)
                wuv = w_up[l].rearrange("e (k p) f -> e p k f", p=128)
                wdv = w_down[l].rearrange("e (c p) f -> e p c f", p=128)

                def tile_body(sl, n):
                    dq.start(MTt[:, :, 0:n], sl(ATk))
                    V.tensor_copy(out=WBCt[:, 0:n], in_=sl(WBC))
                    dq.start(ACCt[:, :, 0:n], sl(ACCk))
                    LW()
                    A.activation(out=MTb[:, 0:4, 0:n], in_=MTt[:, 0:4, 0:n], func=AF.Copy)
                    V.tensor_copy(out=MTb[:, 4:8, 0:n], in_=MTt[:, 4:8, 0:n])
                    B()
                    for hc in range(4):
                        for k in range(8):
                            PE.matmul(PS[hc][:, 0:n], WGb[:, k, hc * 128:(hc + 1) * 128], MTb[:, k, 0:n], start=(k == 0), stop=(k == 7))
                        for k in range(8):
                            PE.matmul(PS[4 + hc][:, 0:n], WUb[:, k, hc * 128:(hc + 1) * 128], MTb[:, k, 0:n], start=(k == 0), stop=(k == 7))
                    B()
                    for hc in range(4):
                        A.activation(out=SG[:, hc, 0:n], in_=PS[hc][:, 0:n], func=AF.Silu)
                    B()
                    for hc in range(4):
                        V.tensor_tensor(out=HID[:, hc, 0:n], in0=PS[4 + hc][:, 0:n], in1=SG[:, hc, 0:n], op=ALU.mult)
                    B()
                    for hc in range(4):
                        V.tensor_tensor(out=HIDb[:, hc, 0:n], in0=HID[:, hc, 0:n], in1=WBCt[:, 0:n], op=ALU.mult)
                    B()
                    for m in range(8):
                        for hc in range(4):
                            PE.matmul(PS[m][:, 0:n], WDb[:, hc, m * 128:(m + 1) * 128], HIDb[:, hc, 0:n], start=(hc == 0), stop=(hc == 3))
                    B()
                    for m in range(8):
                        V.tensor_tensor(out=ACCt[:, m, 0:n], in0=ACCt[:, m, 0:n], in1=PS[m][:, 0:n], op=ALU.add)
                    B()
                    dq.start(sl(ACCk), ACCt[:, :, 0:n])
                    LW()

                def sl_ctx(ap):
                    return ap[:, :, 0:NCTX] if len(ap.shape) == 3 else ap[:, 0:NCTX]

                with MyFori(0, 16) as e:
                    dq.start(WG[:], wgv[e]); dq.start(WU[:], wuv[e]); dq.start(WD[:], wdv[e])
                    dq.start(SELS[:], sel16_in[:, e, :])
                    LW()
                    for ti, (t0, n) in enumerate(TILES):
                        PE.matmul(PS[ti][:, 0:n], SELS[:, :], WT[:, t0:t0 + n], start=True, stop=True)
                    LW()
                    for ti, (t0, n) in enumerate(TILES):
                        A.activation(out=WBC[:, t0:t0 + n], in_=PS[ti][:, 0:n], func=AF.Copy)
                    V.tensor_copy(out=WGb[:], in_=WG[:])
                    nc.gpsimd.tensor_copy(out=WUb[:], in_=WU[:])
                    A.activation(out=WDb[:, 0:2, :], in_=WD[:, 0:2, :], func=AF.Copy)
                    V.tensor_copy(out=WDb[:, 2:4, :], in_=WD[:, 2:4, :])
                    B()
                    tile_body(sl_ctx, NCTX)
                    with MyFori(0, 4) as qt:
                        def sl_lat(ap):
                            if len(ap.shape) == 3:
                                return ap[:, :, NCTX:T][:, :, bass.ts(qt, 512)]
                            return ap[:, NCTX:T][:, bass.ts(qt, 512)]
                        tile_body(sl_lat, 512)

            with SBT("e_xs", [128, 8, 512]) as xs, SBT("e_ac", [128, 8, 512]) as ac:
                ACCk = ACCD.rearrange("(k p) t -> p k t", p=128)
                for ti, (t0, n) in enumerate(TILES):
                    v = 0 if ti == 0 else 1
                    dq.start(xs[:, :, 0:n], XSv[:, :, t0:t0 + n])
                    dq.start(ac[:, :, 0:n], ACCk[:, :, t0:t0 + n])
                    LW()
                    for k in range(8):
                        V.scalar_tensor_tensor(out=xs[:, k, 0:n], in0=ac[:, k, 0:n], scalar=MODS[:, v, l, 40 + k:41 + k],
                                               in1=xs[:, k, 0:n], op0=ALU.mult, op1=ALU.add)
                    B()
                    dq.start(XSv[:, :, t0:t0 + n], xs[:, :, 0:n])
                    LW()


    STAGES = ["norm1", "inproj", "vtok", "diff", "gqa", "ret", "na", "merge", "outproj", "moe"]

    def enabled(name):
        return stop_after is None or STAGES.index(name) <= STAGES.index(stop_after)

    from contextlib import nullcontext as _nullctx

    class _Const:
        def __enter__(self):
            return 0

        def __exit__(self, *a):
            return False

    with (MyFori(0, nsamp) if nsamp > 1 else _Const()) as s:
        dq.start(out[s], OUTS)
        dq.start(XS, xin[s])
        V.tensor_copy(out=MODS[:, 1, :, :], in_=MOD[:, :, :, s])
        V.tensor_copy(out=MODS[:, 0, :, :], in_=MOD[:, :, :, 4])
        LW()
        for v in range(2):
            for nrm in range(2):
                V.scalar_tensor_tensor(out=GG[:, v, nrm, :, :], in0=MODS[:, v, :, 8 + 24 * nrm:16 + 24 * nrm], scalar=1.0,
                                       in1=GN[:, 2 * nrm:2 * nrm + 2, :], op0=ALU.add, op1=ALU.mult)
        B()
        for l in range(nlayers):
            with SBT("BIG", [128, 8, T]) as BIG:
                if enabled("norm1"):
                    stage_norm(BIG, lambda v, k: GG[:, v, 0, l, k:k + 1], lambda v, k: MODS[:, v, l, k:k + 1])
                    if dbg and "HT" in dbg:
                        dq.start(dbg_t["HT"].rearrange("(k p) t -> p k t", p=128), BIG[:])
                        LW()
                wv = w_in[l].rearrange("(k p) (m j) -> p k m j", p=128, j=128)
                if enabled("inproj"):
                    with SBT("BIGb", [128, 8, T], BF16) as BIGb:
                        cast_big(BIG, BIGb)
                        stage_gemm(BIGb, wv, 0, 24, 0, PT, ("copy",))
                        stage_gemm(BIGb, wv, 24, 56, 0, PT, ("sigmoid",))
                if enabled("vtok"):
                    stage_vtok(BIG, l)
            if enabled("diff"):
                stage_diff(l)
            if enabled("gqa"):
                stage_gqa(l)
            if enabled("ret"):
                stage_ret(l)
            if enabled("na"):
                stage_na(l)
            if enabled("merge"):
                stage_merge(l)
            if enabled("outproj"):
                with SBT("BIG2", [128, 8, T]) as BIG:
                    dq.start(BIG[:], AT.rearrange("(k p) t -> p k t", p=128))
                    LW()
                    wo = w_out[l].rearrange("(k p) (m j) -> p k m j", p=128, j=128)
                    with SBT("BIGb2", [128, 8, T], BF16) as BIGb:
                        cast_big(BIG, BIGb)
                        stage_gemm(BIGb, wo, 0, 8, 0, XS, ("resid", lambda v, m: MODS[:, v, l, 16:24][:, bass.ts(m, 1)]))
            if dbg and "X1" in dbg:
                dq.start(dbg_t["X1"], XS)
                LW()
            if enabled("moe"):
                stage_moe(l)
        with SBT("BIG3", [128, 8, T]) as BIG:
            stage_norm(BIG, lambda v, k: GN[:, 4, k:k + 1], lambda v, k: ZERO8[:, k:k + 1])
            dq.start(OUTS.rearrange("(k p) t -> p k t", p=128), BIG[:, :, NCTX:T])
            LW()
    dq.start(out[nsamp], OUTS)
    LW()
    es.close()
    if os.environ.get('SBUF_PEAK'):
        print('SBUF peaks', {k: v // 1024 for k, v in _peak.items()})
    return nc


def _consts():
    c = {}
    c["ident"] = np.eye(128, dtype=np.float32)
    seld = np.zeros((65, 64), np.float32); seld[64, :] = 1.0
    c["seld"] = seld
    sel16 = np.zeros((16, 16, 128), np.float32)
    for e in range(16):
        sel16[e, e, :] = 1.0
    c["sel16"] = sel16
    k = np.arange(128)[:, None].astype(np.float32); q = np.arange(128)[None, :].astype(np.float32)
    retc = np.zeros((128, 4, 128), np.float32)
    retc[:, 0, :] = np.maximum(q - k, 0); retc[:, 1, :] = (q >= k)
    retc[:, 2, :] = np.maximum(k - q, 0); retc[:, 3, :] = (k >= q)
    c["retc"] = retc
    rp = np.zeros((128, 4, 128), np.float32)
    t = np.arange(128, dtype=np.float32)
    rp[:, 0, :] = (t + 1.0)[None, :]; rp[:, 1, :] = (128.0 - t)[None, :]
    rp[:, 2, 0] = 127.0 - t; rp[:, 3, 0] = t
    c["retpos"] = rp

    def rope_tab(d):
        half = d // 2; nf = half // 2
        inv = (1.0 / (10000.0 ** (np.arange(nf, dtype=np.float32) / nf))).astype(np.float32)
        tt = np.arange(L, dtype=np.int32)
        rows = (tt // 64).astype(np.float32); cols = (tt % 64).astype(np.float32)
        C = np.ones((d, T), np.float32); S = np.zeros((d, T), np.float32)
        for blk, pos in ((0, rows), (1, cols)):
            ang = (pos[None, :] * inv[:, None]).astype(np.float32)
            cs, sn = np.cos(ang).astype(np.float32), np.sin(ang).astype(np.float32)
            b0 = blk * half
            C[b0:b0 + nf, NCTX:] = cs; C[b0 + nf:b0 + half, NCTX:] = cs
            S[b0:b0 + nf, NCTX:] = -sn; S[b0 + nf:b0 + half, NCTX:] = sn
        return np.stack([C, S])
    c["rope64"] = rope_tab(64)
    c["rope32"] = rope_tab(32)
    cc = np.arange(64)[:, None]; kc = np.arange(64)[None, :]
    ws = np.clip(cc - 8, 0, 48)
    ok = (kc >= ws) & (kc < ws + 16)
    idx = np.clip(kc - cc, -15, 15) + 15
    msk = np.where(ok, 0.0, NEGM).astype(np.float32)
    c["namask"] = np.ascontiguousarray(np.concatenate([msk.T, msk.T], axis=0))
    c["_naidx"] = idx
    lamc = np.zeros((128, 2, 2), np.float32)
    c["lamc"] = lamc
    return c


_CACHE = {}


def _prep_inputs(inp, nsamp_per_core, ncores):
    c = _consts()
    f = lambda a: np.ascontiguousarray(a, dtype=np.float32)
    shared = {}
    shared["w_mod"] = f(inp["w_mod"]); shared["w_in"] = f(inp["w_in"]); shared["w_branch"] = f(inp["w_branch"])
    shared["w_out"] = f(inp["w_out"]); shared["w_router"] = f(inp["w_router"])
    shared["w_gate"] = f(inp["w_gate_e"]); shared["w_up"] = f(inp["w_up_e"]); shared["w_down"] = f(inp["w_down_e"])
    shared["bmod"] = f(inp["b_mod"].reshape(2, 48, 128).transpose(2, 0, 1))
    gn = np.stack([inp["g_norm1"][0], inp["g_norm1"][1], inp["g_norm2"][0], inp["g_norm2"][1], inp["g_final"]])
    shared["gn"] = f(gn.reshape(5, 8, 128).transpose(2, 0, 1))
    idx = c["_naidx"]
    rpb = inp["na_rpb"]
    g = rpb[:, :, :, idx]
    nab = np.zeros((2, 128, 4, 14, 64), np.float32)
    for jj in range(2):
        nab[:, jj * 64:(jj + 1) * 64] = g[:, :, jj:jj + 14].transpose(0, 4, 1, 2, 3)
    shared["nab"] = f(nab)
    shared["namask"] = c["namask"]
    shared["retp"] = f(np.broadcast_to(inp["ret_log_decay"].reshape(2, 1, 8), (2, 128, 8)))
    perm = np.concatenate([np.arange(16, 32), np.arange(0, 16), np.arange(48, 64), np.arange(32, 48)])
    qg, kg = inp["gqa_q_gain"], inp["gqa_k_gain"]
    shared["gqag"] = f(np.stack([qg, qg[:, perm], kg, kg[:, perm]], axis=-1))
    shared["dlam"] = f(np.broadcast_to(inp["diff_lambda"].reshape(2, 1, 128), (2, 128, 128)))
    shared["dsub"] = f(inp["diff_subln"].T)
    shared["lamc"] = c["lamc"]
    shared["rb"] = f(np.broadcast_to(inp["router_bias"][None, :], (128, 16)))
    for k in ("rope64", "rope32", "ident", "seld", "sel16", "retc", "retpos"):
        shared[k] = c[k]
    maps = []
    for ci in range(ncores):
        b0 = ci * nsamp_per_core
        bs = slice(b0, b0 + nsamp_per_core)
        xin = np.concatenate([inp["ctx"][bs].transpose(0, 2, 1), inp["x"][bs].transpose(0, 2, 1)], axis=2)
        cc = np.concatenate([inp["c"][bs], np.zeros((4 - nsamp_per_core, D), np.float32), inp["c_ctx"][None, :]], axis=0)
        m = dict(shared)
        m["xin"] = f(xin)
        m["cT"] = f(cc.T.reshape(8, 128, 5).transpose(1, 0, 2))
        maps.append(m)
    return maps


def kernel(**inputs):
    inp = {k: np.asarray(v) for k, v in inputs.items()}
    ncores = 8
    nb = inp["x"].shape[0] // ncores
    if "nc" not in _CACHE:
        _CACHE["nc"] = build(nsamp=nb, nlayers=DEPTH)
    nc = _CACHE["nc"]
    maps = _prep_inputs(inp, nb, ncores)
    res = run_bass_kernel_spmd(nc, maps, core_ids=list(range(ncores)))
    outs = [r["out"][1:] for r in res.results]
    o = np.concatenate(outs, axis=0).transpose(0, 2, 1)
    return np.ascontiguousarray(o, dtype=np.float32)
```

```python
import math
import numpy as np
import concourse.bass as bass
import concourse.mybir as mybir
from concourse.bass_utils import run_bass_kernel_spmd

F32 = mybir.dt.float32
BF16 = mybir.dt.bfloat16
AF = mybir.ActivationFunctionType
ALU = mybir.AluOpType
AX = mybir.AxisListType

D = 1024
T = 2304
NCTX = 256
L = 2048
EPS = 1e-6
TILES = [(0, 256), (256, 512), (768, 512), (1280, 512), (1792, 512)]
NEGM = -1.0e4
DEPTH = 2
import os
RET_STOP = int(os.environ.get('RET_STOP', '99'))
RET_STEP = int(os.environ.get('RET_STEP', '99'))


class Ctx:
    pass


def build(nsamp=4, nlayers=2, dbg=None, stop_after=None):
    nc = bass.Bass("TRN2", target_bir_lowering=False)
    K = Ctx()

    def din(name, shape):
        return nc.dram_tensor(name, list(shape), F32, kind="ExternalInput").ap()

    xin = din("xin", [nsamp, D, T])
    cT_in = din("cT", [128, 8, 5])
    w_mod = din("w_mod", [2, D, 6 * D])
    bmod_in = din("bmod", [128, 2, 48])
    gn_in = din("gn", [128, 5, 8])
    w_in = din("w_in", [2, D, 7168])
    nab_in = din("nab", [2, 128, 4, 14, 64])
    namask_in = din("namask", [128, 64])
    retp_in = din("retp", [2, 128, 8])
    gqag_in = din("gqag", [2, 64, 4])
    dlam_in = din("dlam", [2, 128, 128])
    dsub_in = din("dsub", [64, 2])
    lamc_in = din("lamc", [128, 2, 2])
    w_branch = din("w_branch", [2, 4, 256, D])
    w_out = din("w_out", [2, D, D])
    w_router = din("w_router", [D, 16])
    rb_in = din("rb", [128, 16])
    w_gate = din("w_gate", [2, 16, D, 512])
    w_up = din("w_up", [2, 16, D, 512])
    w_down = din("w_down", [2, 16, 512, D])
    rope64_in = din("rope64", [2, 64, T])
    rope32_in = din("rope32", [2, 32, T])
    ident_in = din("ident", [128, 128])
    seld_in = din("seld", [65, 64])
    sel16_in = din("sel16", [16, 16, 128])
    retc_in = din("retc", [128, 4, 128])
    retpos_in = din("retpos", [128, 4, 128])

    out = nc.dram_tensor("out", [nsamp + 1, D, L], F32, kind="ExternalOutput").ap()
    OUTS = nc.dram_tensor("OUTS", [D, L], F32, kind="Internal").ap()
    dbg_outs = {}
    dbg_t = dbg_outs

    def dscratch(name, shape):
        kind = "ExternalOutput" if (dbg and name in dbg) else "Internal"
        t = nc.dram_tensor(name, list(shape), F32, kind=kind).ap()
        if kind == "ExternalOutput":
            dbg_outs[name] = t
        return t

    XS = dscratch("XS", [D, T])
    PT = dscratch("PT", [7168, T])
    VT = dscratch("VT", [T, 896])
    YT = dscratch("YT", [1024, T])
    AT = dscratch("AT", [D, T])
    STG = dscratch("STG", [128, T])
    ACCD = dscratch("ACCD", [D, T])
    MTD = dscratch("MTD", [D, T])
    if dbg and "X1" in dbg:
        dscratch("X1", [D, T])
    if dbg and "HT" in dbg:
        dscratch("HT", [D, T])

    from contextlib import ExitStack
    es = ExitStack()
    es.__enter__()

    _cnt = [0]

    _peak = {}

    class _SBTW:
        def __init__(self, name, shape, dt=F32):
            self.name = name
            self.cm = nc.sbuf_tensor(f"sb{_cnt[0]}_{name}", list(shape), dt)

        def __enter__(self):
            r = self.cm.__enter__()
            used = 229376 - nc.sbuf_bytes_remaining
            key = self.name.split("_")[0]
            _peak[key] = max(_peak.get(key, 0), used)
            return r

        def __exit__(self, *a):
            return self.cm.__exit__(*a)

    def SBT(name, shape, dt=F32):
        _cnt[0] += 1
        return _SBTW(name, shape, dt)

    def sb(name, shape):
        return es.enter_context(SBT(name, list(shape)))

    PS = [es.enter_context(nc.psum_tensor(f"ps{i}", [128, 512], F32)) for i in range(8)]
    dsem = es.enter_context(nc.semaphore("dsem"))

    class DQ:
        n = 0

        def start(self, out, in_):
            nc.sync.dma_start(out=out, in_=in_).then_inc(dsem, 16)
            self.n += 1

        def wait(self):
            if self.n:
                nc.sync.wait_ge(dsem, 16 * self.n)
                nc.sync.sem_clear(dsem)
                self.n = 0

    dq = DQ()

    def B():
        nc.all_engine_barrier()

    def LW():
        dq.wait()
        B()

    V = nc.vector
    A = nc.scalar
    PE = nc.tensor

    import re as _re
    from contextlib import contextmanager as _cm
    _RH = bass.RegisterHandle
    _ENGS = {"Pool": mybir.EngineType.Pool, "Activation": mybir.EngineType.Activation, "PE": mybir.EngineType.PE,
             "DVE": mybir.EngineType.DVE, "SP": mybir.EngineType.SP}
    _ALLE = mybir.ALL_ENGINES
    _lc = [0]

    def _cur_id():
        h = nc.vector.alloc_register()
        m = _re.search(r"(\d+)$", h.name)
        nc.vector.free_register(h)
        return int(m.group(1))

    @_cm
    def MyFori(start, end):
        id0 = _cur_id()
        _lc[0] += 1
        name = f"mf{_lc[0]}"
        ls, le = name + "_loop", name + "_end"
        regs = nc.alloc_registers(name + "_i", engines=_ALLE)
        nc.regs_mov(regs, start)
        nc.br(ls, engines=_ALLE)
        with nc.body(ls, valid_engines=_ALLE):
            i = nc.snap(regs, min_val=start, max_val=end - 1)
            yield i
            nc.regs_alu(regs, regs, 1, op=mybir.AluOpType.add)
            nc.br_lt(regs, end, on_true=ls, on_false=le, engines=_ALLE)
        nc.switch_bb(le)
        for h in regs.handles:
            nc.free_register(h)
        if not isinstance(i, int):
            for e in nc.engines.values():
                al = e.get_value_cache().lookup(i)
                if al is not None:
                    nc.free_register(al.val)
        id1 = _cur_id()
        for en, et in _ENGS.items():
            for k in range(id0, id1 + 1):
                nm = f"{en}_tmp_{k}"
                try:
                    r = nc.lookup_reg(nm)
                except Exception:
                    r = None
                if r is not None and getattr(r, "allocated", False):
                    nc.free_register(_RH(name=nm, engine=et))


    ident = sb("ident", [128, 128])
    ones = sb("ones", [128, 128])
    epsc = sb("epsc", [128, 1])
    MOD = sb("MOD", [128, 2, 48, 5])
    MODS = sb("MODS", [128, 2, 2, 48])
    GG = sb("GG", [128, 2, 2, 2, 8])
    GN = sb("GN", [128, 5, 8])
    BMOD = sb("BMOD", [128, 2, 48])
    SELD = sb("SELD", [65, 64])
    ZERO8 = sb("ZERO8", [128, 8])
    LAMC = sb("LAMC", [128, 2, 2])
    DSUB = sb("DSUB", [64, 2])

    dq.start(ident[:], ident_in)
    dq.start(GN[:], gn_in)
    dq.start(BMOD[:], bmod_in)
    dq.start(SELD[:], seld_in)
    dq.start(LAMC[:], lamc_in)
    dq.start(DSUB[:], dsub_in)
    V.memset(ones[:], 1.0)
    V.memset(epsc[:], EPS)
    V.memset(ZERO8[:], 0.0)
    LW()

    def stage_mods():
        with SBT("scT", [128, 8, 5]) as scT, SBT("wm", [128, 8, 128]) as wm:
            dq.start(scT[:], cT_in)
            LW()
            A.activation(out=scT[:], in_=scT[:], func=AF.Silu)
            B()
            for l in range(2):
                wv = w_mod[l].rearrange("(k p) (m j) -> p k m j", p=128, j=128)
                with MyFori(0, 48) as m:
                    dq.start(wm[:], wv[:, :, m, :])
                    LW()
                    for k in range(8):
                        PE.matmul(PS[0][:, 0:5], wm[:, k, :], scT[:, k, :], start=(k == 0), stop=(k == 7))
                    B()
                    V.tensor_scalar(out=MOD[:, l, m, :], in0=PS[0][:, 0:5], scalar1=BMOD[:, l, bass.ts(m, 1)],
                                    scalar2=None, op0=ALU.add)
                    B()

    stage_mods()

    def stage_norm(BIG, gfun, shfun):
        XSv = XS.rearrange("(k p) t -> p k t", p=128)
        with SBT("n_xt", [128, 8, 512]) as xt, SBT("n_sq", [128, 8, 512]) as sq, \
                SBT("n_rs", [128, 512]) as rs:
            for ti, (t0, n) in enumerate(TILES):
                v = 0 if ti == 0 else 1
                dq.start(xt[:, :, 0:n], XSv[:, :, t0:t0 + n])
                LW()
                A.activation(out=sq[:, :, 0:n], in_=xt[:, :, 0:n], func=AF.Square)
                B()
                for k in range(8):
                    PE.matmul(PS[0][:, 0:n], ones[:, :], sq[:, k, 0:n], start=(k == 0), stop=(k == 7))
                B()
                A.activation(out=rs[:, 0:n], in_=PS[0][:, 0:n], func=AF.Sqrt, bias=epsc[:, :], scale=1.0 / D)
                B()
                V.reciprocal(out=rs[:, 0:n], in_=rs[:, 0:n])
                B()
                for k in range(8):
                    V.scalar_tensor_tensor(out=sq[:, k, 0:n], in0=xt[:, k, 0:n], scalar=gfun(v, k), in1=rs[:, 0:n],
                                           op0=ALU.mult, op1=ALU.mult)
                B()
                for k in range(8):
                    A.activation(out=BIG[:, k, t0:t0 + n], in_=sq[:, k, 0:n], func=AF.Identity, bias=shfun(v, k),
                                 scale=1.0)
                B()

    def cast_big(BIG, BIGb):
        for k in range(8):
            if k % 3 == 0:
                A.activation(out=BIGb[:, k, :], in_=BIG[:, k, :], func=AF.Copy)
            elif k % 3 == 1:
                V.tensor_copy(out=BIGb[:, k, :], in_=BIG[:, k, :])
            else:
                nc.gpsimd.tensor_copy(out=BIGb[:, k, :], in_=BIG[:, k, :])
        B()

    def stage_gemm(BIG, wview, m_lo, m_hi, orow0, dst, epi):
        dstv = dst.rearrange("(m p) t -> m p t", p=128)
        with SBT("g_w", [128, 8, 128]) as wt, SBT("g_o", [128, T]) as ot, \
                SBT("g_x", [128, T]) as xr, SBT("g_wb", [128, 8, 128], BF16) as wtb:
            with MyFori(m_lo, m_hi) as m:
                dq.start(wt[:], wview[:, :, m, :])
                if epi[0] == "resid":
                    dq.start(xr[:], dstv[m + orow0])
                LW()
                V.tensor_copy(out=wtb[:], in_=wt[:])
                B()
                for ti, (t0, n) in enumerate(TILES):
                    for k in range(8):
                        PE.matmul(PS[ti][:, 0:n], wtb[:, k, :], BIG[:, k, t0:t0 + n], start=(k == 0), stop=(k == 7))
                B()
                for ti, (t0, n) in enumerate(TILES):
                    if epi[0] == "copy":
                        if ti % 2 == 0:
                            A.activation(out=ot[:, t0:t0 + n], in_=PS[ti][:, 0:n], func=AF.Copy)
                        else:
                            V.tensor_copy(out=ot[:, t0:t0 + n], in_=PS[ti][:, 0:n])
                    elif epi[0] == "sigmoid":
                        A.activation(out=ot[:, t0:t0 + n], in_=PS[ti][:, 0:n], func=AF.Sigmoid)
                    elif epi[0] == "resid":
                        v = 0 if ti == 0 else 1
                        V.scalar_tensor_tensor(out=ot[:, t0:t0 + n], in0=PS[ti][:, 0:n], scalar=epi[1](v, m),
                                               in1=xr[:, t0:t0 + n], op0=ALU.mult, op1=ALU.add)
                B()
                dq.start(dstv[m + orow0], ot[:])
                LW()

    VCOLS = [(512, 256), (1280, 256), (2176, 128), (2816, 256)]

    def stage_vtok(BIG, l):
        VTv = VT.rearrange("(c p) f -> c p f", p=128)
        wl = w_in[l].rearrange("(k p) f -> p k f", p=128)
        with SBT("v_w", [128, 8, 896]) as wv, SBT("v_o", [128, 896]) as vo, SBT("v_stg", [128, 8, 128]) as stg:
            c0 = 0
            for (col0, wd) in VCOLS:
                dq.start(wv[:, :, c0:c0 + wd], wl[:, :, col0:col0 + wd])
                c0 += wd
            LW()
            with MyFori(0, 18) as tc:
                V.tensor_copy(out=stg[:], in_=BIG[:, :, bass.ts(tc, 128)])
                B()
                for k in range(8):
                    PE.matmul(PS[0][:, 0:512], stg[:, k, :], wv[:, k, 0:512], start=(k == 0), stop=(k == 7))
                for k in range(8):
                    PE.matmul(PS[1][:, 0:384], stg[:, k, :], wv[:, k, 512:896], start=(k == 0), stop=(k == 7))
                B()
                A.activation(out=vo[:, 0:512], in_=PS[0][:, 0:512], func=AF.Copy)
                V.tensor_copy(out=vo[:, 512:896], in_=PS[1][:, 0:384])
                B()
                dq.start(VTv[tc], vo[:])
                LW()

    def prep(dst, blk, d, tabs, bufs, gains=None, norm=False):
        q = d // 4
        Ab, Bb, t1, t2 = bufs
        if callable(blk):
            rb = blk
        else:
            PTq = PT.rearrange("(a p) t -> a p t", p=q)
            rb = lambda i: PTq[blk + i]
        for i in range(4):
            dq.start(Ab[i * q:(i + 1) * q, :], rb(i))
        for i, j in enumerate((1, 0, 3, 2)):
            dq.start(Bb[i * q:(i + 1) * q, :], rb(j))
        LW()
        C, S = tabs
        if norm:
            A.activation(out=t2[0:d, :], in_=Ab[0:d, :], func=AF.Square)
            B()
            for ti, (t0, n) in enumerate(TILES):
                PE.matmul(PS[ti][0:d, 0:n], ones[0:d, 0:d], t2[0:d, t0:t0 + n], start=True, stop=True)
            B()
            for ti, (t0, n) in enumerate(TILES):
                A.activation(out=dst[0:d, t0:t0 + n], in_=PS[ti][0:d, 0:n], func=AF.Sqrt, bias=epsc[0:d, :], scale=1.0 / d)
            B()
            V.reciprocal(out=dst[0:d, :], in_=dst[0:d, :])
        if gains is not None:
            V.tensor_scalar(out=Ab[0:d, :], in0=Ab[0:d, :], scalar1=gains[0], scalar2=None, op0=ALU.mult)
            V.tensor_scalar(out=Bb[0:d, :], in0=Bb[0:d, :], scalar1=gains[1], scalar2=None, op0=ALU.mult)
        B()
        if t1 is None:
            t1, t2 = Ab, Bb
        V.tensor_tensor(out=t1[0:d, :], in0=Ab[0:d, :], in1=C, op=ALU.mult)
        V.tensor_tensor(out=t2[0:d, :], in0=Bb[0:d, :], in1=S, op=ALU.mult)
        B()
        if norm:
            V.tensor_tensor(out=t1[0:d, :], in0=t1[0:d, :], in1=t2[0:d, :], op=ALU.add)
            B()
            V.tensor_tensor(out=dst[0:d, :], in0=t1[0:d, :], in1=dst[0:d, :], op=ALU.mult)
        else:
            V.tensor_tensor(out=dst[0:d, :], in0=t1[0:d, :], in1=t2[0:d, :], op=ALU.add)
        B()

    def load_vaug(Vb, col0, nch=18, tok0=0):
        src = VT[tok0:tok0 + nch * 128, :].rearrange("(c p) f -> p c f", p=128)
        dq.start(Vb[:, 0:nch, 0:64], src[:, :, col0:col0 + 64] if isinstance(col0, int) else None)

    def attn_core(QRq, KR, dqk, n, kchunks, vfun, scale, oa_bank, E):
        groups = [kchunks[i:i + 4] for i in range(0, len(kchunks), 4)]
        npv = len(kchunks)
        pv_i = 0
        prev = None
        for g in groups + [None]:
            if prev is not None:
                for j, c in enumerate(prev):
                    PE.matmul(PS[oa_bank][0:65, 0:n], vfun(c), E[:, j, 0:n], start=(pv_i == 0), stop=(pv_i == npv - 1))
                    pv_i += 1
            if g is not None:
                for j, c in enumerate(g):
                    PE.matmul(PS[j][:, 0:n], KR[0:dqk, c * 128:(c + 1) * 128], QRq, start=True, stop=True)
            B()
            if g is not None:
                for j, c in enumerate(g):
                    A.activation(out=E[:, j, 0:n], in_=PS[j][:, 0:n], func=AF.Exp, scale=scale)
                B()
            prev = g

    def attn_finish(oa_bank, n, OAS, RZ, YO, zbank=6):
        A.activation(out=OAS[0:65, 0:n], in_=PS[oa_bank][0:65, 0:n], func=AF.Copy)
        B()
        PE.matmul(PS[zbank][0:64, 0:n], SELD[0:65, 0:64], OAS[0:65, 0:n], start=True, stop=True)
        B()
        V.reciprocal(out=RZ[0:64, 0:n], in_=PS[zbank][0:64, 0:n])
        B()
        V.tensor_tensor(out=YO[0:64, 0:n], in0=OAS[0:64, 0:n], in1=RZ[0:64, 0:n], op=ALU.mult)
        B()

    ALLK = list(range(18))
    YT64 = YT.rearrange("(a p) t -> a p t", p=64)
    PT64 = PT.rearrange("(a p) t -> a p t", p=64)
    VTc = VT.rearrange("(c p) f -> p c f", p=128)

    def stage_gqa(l):
        with ExitStack() as s2:
            def t(name, shape):
                return s2.enter_context(SBT(name, list(shape)))
            RC = t("q_rc", [64, T]); RS = t("q_rs", [64, T])
            Ab = t("q_a", [64, T]); Bb = t("q_b", [64, T]); t1 = t("q_t1", [64, T]); t2 = t("q_t2", [64, T])
            KR = t("q_kr", [64, T]); QR = t("q_qr", [64, T]); Vb = t("q_v", [128, 18, 65])
            E = t("q_e", [128, 4, 512]); OAS = t("q_oas", [65, 512]); RZ = t("q_rz", [64, 512]); YO = t("q_yo", [64, 512])
            GQ = t("q_g", [64, 4]); YH = t("q_yh", [64, T])
            dq.start(RC[:], rope64_in[0]); dq.start(RS[:], rope64_in[1]); dq.start(GQ[:], gqag_in[l])
            LW()
            bufs = (Ab, Bb, t1, t2)
            for kv in range(2):
                prep(KR, (2048 + kv * 64) // 16, 64, (RC[:], RS[:]), bufs, gains=(GQ[:, 2:3], GQ[:, 3:4]), norm=True)
                dq.start(Vb[:, :, 0:64], VTc[:, :, 512 + kv * 64:512 + kv * 64 + 64])
                V.memset(Vb[:, :, 64:65], 1.0)
                LW()
                for hh in range(2):
                    dq.start(STG[0:64, :], PT64[hh + (1792 + kv * 128) // 64])
                    LW()
                    prep(QR, lambda i: STG.rearrange("(a p) t -> a p t", p=16)[i], 64, (RC[:], RS[:]), bufs, gains=(GQ[:, 0:1], GQ[:, 1:2]), norm=True)
                    QRl = QR[:, NCTX:T]
                    with MyFori(0, 4) as qt:
                        attn_core(QRl[:, bass.ts(qt, 512)], KR, 64, 512, ALLK, lambda c: Vb[:, c, :], 0.125, 4, E)
                        attn_finish(4, 512, OAS, RZ, YO)
                        V.tensor_copy(out=YH[:, NCTX:T][:, bass.ts(qt, 512)], in_=YO[:, :])
                        B()
                    attn_core(QR[:, 0:NCTX], KR, 64, NCTX, [0, 1], lambda c: Vb[:, c, :], 0.125, 4, E)
                    attn_finish(4, NCTX, OAS, RZ, YO)
                    V.tensor_copy(out=YH[:, 0:NCTX], in_=YO[:, 0:NCTX])
                    B()
                    dq.start(YT64[8 + kv * 2 + hh], YH[:])
                    LW()

    def stage_diff(l):
        lam_init = 0.8 - 0.6 * math.exp(-0.3 * l)
        with ExitStack() as s2:
            def t(name, shape):
                return s2.enter_context(SBT(name, list(shape)))
            RC = t("d_rc", [32, T]); RS = t("d_rs", [32, T])
            Ab = t("d_a", [32, T]); Bb = t("d_b", [32, T]); t1 = None; t2 = None
            QK = [t(f"d_qk{i}", [32, T]) for i in range(4)]
            Vb = t("d_v", [128, 18, 65])
            E = t("d_e", [128, 4, 512]); OAS = t("d_oas", [65, 512]); RZ = t("d_rz", [64, 512])
            Y1 = t("d_y1", [64, 512]); Y2 = t("d_y2", [64, 512]); SQ = t("d_sq", [64, 512])
            LM = t("d_lm", [128, 128]); LV = t("d_lv", [128, 4]); YH = t("d_yh", [64, T])
            dq.start(RC[:], rope32_in[0]); dq.start(RS[:], rope32_in[1]); dq.start(LM[:], dlam_in[l])
            LW()
            V.tensor_tensor(out=LM[:, 0:32], in0=LM[:, 0:32], in1=LM[:, 32:64], op=ALU.mult)
            V.tensor_tensor(out=LM[:, 64:96], in0=LM[:, 64:96], in1=LM[:, 96:128], op=ALU.mult)
            B()
            V.reduce_sum(out=LV[:, 0:1], in_=LM[:, 0:32], axis=AX.X)
            V.reduce_sum(out=LV[:, 1:2], in_=LM[:, 64:96], axis=AX.X)
            B()
            A.activation(out=LV[:, 0:2], in_=LV[:, 0:2], func=AF.Exp)
            B()
            V.tensor_tensor(out=LV[:, 2:3], in0=LV[:, 1:2], in1=LV[:, 0:1], op=ALU.subtract)
            B()
            V.tensor_scalar(out=LV[:, 2:3], in0=LV[:, 2:3], scalar1=-lam_init, scalar2=None, op0=ALU.add)
            B()
            bufs = (Ab, Bb, t1, t2)
            sc = 32 ** -0.5
            with MyFori(0, 4) as h:
                dq.start(YT64[h + 11], YH[:])
                dq.start(STG[0:64, :], PT64[h + 36]); dq.start(STG[64:128, :], PT64[h + 40])
                LW()
                prep(QK[0], lambda i: STG.rearrange("(a p) t -> a p t", p=8)[0 + i], 32, (RC[:], RS[:]), bufs)
                prep(QK[1], lambda i: STG.rearrange("(a p) t -> a p t", p=8)[4 + i], 32, (RC[:], RS[:]), bufs)
                prep(QK[2], lambda i: STG.rearrange("(a p) t -> a p t", p=8)[8 + i], 32, (RC[:], RS[:]), bufs)
                prep(QK[3], lambda i: STG.rearrange("(a p) t -> a p t", p=8)[12 + i], 32, (RC[:], RS[:]), bufs)
                VTh = VT[:, 640:896].rearrange("(c p) (h f) -> h p c f", p=128, f=64)
                dq.start(Vb[:, :, 0:64], VTh[h])
                V.memset(Vb[:, :, 64:65], 1.0)
                LW()

                def one_tile(q0sl, n, kch, dst):
                    attn_core(QK[0][:, q0sl] if not callable(q0sl) else q0sl(QK[0]), QK[2], 32, n, kch, lambda c: Vb[:, c, :], sc, 4, E)
                    attn_finish(4, n, OAS, RZ, Y1)
                    attn_core(QK[1][:, q0sl] if not callable(q0sl) else q0sl(QK[1]), QK[3], 32, n, kch, lambda c: Vb[:, c, :], sc, 5, E)
                    attn_finish(5, n, OAS, RZ, Y2)
                    V.scalar_tensor_tensor(out=Y1[:, 0:n], in0=Y2[:, 0:n], scalar=LV[0:64, 2:3], in1=Y1[:, 0:n],
                                           op0=ALU.mult, op1=ALU.add)
                    B()
                    A.activation(out=SQ[:, 0:n], in_=Y1[:, 0:n], func=AF.Square)
                    B()
                    PE.matmul(PS[6][0:64, 0:n], ones[0:64, 0:64], SQ[:, 0:n], start=True, stop=True)
                    B()
                    A.activation(out=SQ[:, 0:n], in_=PS[6][0:64, 0:n], func=AF.Sqrt, bias=epsc[0:64, :], scale=1.0 / 64)
                    B()
                    V.reciprocal(out=SQ[:, 0:n], in_=SQ[:, 0:n])
                    B()
                    V.scalar_tensor_tensor(out=Y2[:, 0:n], in0=Y1[:, 0:n], scalar=DSUB[:, l:l + 1], in1=SQ[:, 0:n],
                                           op0=ALU.mult, op1=ALU.mult)
                    B()
                    A.activation(out=Y2[:, 0:n], in_=Y2[:, 0:n], func=AF.Copy, scale=(1.0 - lam_init))
                    B()
                    V.tensor_copy(out=dst, in_=Y2[:, 0:n])
                    B()

                with MyFori(0, 4) as qt:
                    one_tile(lambda Q: Q[:, NCTX:T][:, bass.ts(qt, 512)], 512, ALLK,
                             YH[:, NCTX:T][:, bass.ts(qt, 512)])
                one_tile(slice(0, NCTX), NCTX, [0, 1], YH[:, 0:NCTX])
            dq.start(YT64[15], YH[:])
            LW()

    def stage_na(l):
        with ExitStack() as s2:
            def t(name, shape):
                return s2.enter_context(SBT(name, list(shape)))
            NAB = t("a_nab", [128, 4, 14, 64]); NABh = t("a_nabh", [128, 14, 64]); MSK = t("a_msk", [128, 64])
            QT = t("a_q", [64, T]); KT = t("a_k", [64, T])
            Ve = t("a_ve", [128, 18, 65]); Vo = t("a_vo", [128, 17, 65])
            E = t("a_e", [128, 6, 64]); OAll = t("a_oall", [65, L])
            E4 = t("a_e4", [128, 4, 512])
            OAS = t("a_oas", [65, 512]); RZ = t("a_rz", [64, 512]); YO = t("a_yo", [64, 512])
            dq.start(NAB[:], nab_in[l]); dq.start(MSK[:], namask_in)
            LW()
            for hh in range(4):
                for dd in range(14):
                    V.tensor_tensor(out=NAB[:, hh, dd, :], in0=NAB[:, hh, dd, :], in1=MSK[:, :], op=ALU.add)
            B()
            VT_e = VT[:, 0:256].rearrange("(c p) (h f) -> h p c f", p=128, f=64)
            VT_o = VT[64:64 + 17 * 128, 0:256].rearrange("(c p) (h f) -> h p c f", p=128, f=64)
            with MyFori(0, 4) as h:
                dq.start(QT[:], PT64[h]); dq.start(KT[:], PT64[4 + h])
                dq.start(Ve[:, :, 0:64], VT_e[h]); dq.start(Vo[:, :, 0:64], VT_o[h])
                V.memset(Ve[:, :, 64:65], 1.0)
                V.memset(Vo[:, :, 64:65], 1.0)
                V.tensor_copy(out=NABh[:], in_=NAB[:, h, :, :])
                LW()

                def row(qcols, r0tok, off, vsel, slot):
                    for j in range(4):
                        PE.matmul(PS[0][:, j * 64:(j + 1) * 64], r0tok(j), qcols, start=True, stop=True)
                    for j in range(2):
                        PE.matmul(PS[0][:, (4 + j) * 64:(5 + j) * 64], KT[:, j * 128:(j + 1) * 128], qcols, start=True, stop=True)
                    B()
                    b0 = 7 - off
                    A.activation(out=E[:, 0:4, :], in_=PS[0][:, 0:256].rearrange("p (j c) -> p j c", c=64), func=AF.Copy, scale=0.125)
                    A.activation(out=E[:, 4:6, :], in_=PS[0][:, 256:384].rearrange("p (j c) -> p j c", c=64), func=AF.Exp, scale=0.125)
                    B()
                    for j in range(4):
                        V.tensor_tensor(out=E[:, j, :], in0=E[:, j, :], in1=NABh[:, b0 + 2 * j, :], op=ALU.add)
                    B()
                    A.activation(out=E[:, 0:4, :], in_=E[:, 0:4, :], func=AF.Exp)
                    B()
                    for j in range(6):
                        lhs = vsel(j) if j < 4 else Ve[:, j - 4, :]
                        PE.matmul(PS[1][0:65, slot * 64:(slot + 1) * 64], lhs, E[:, j, :], start=(j == 0), stop=(j == 5))
                    B()

                def static_row(r):
                    r0 = min(max(r - 4, 0), 24)
                    off = r - r0
                    if r0 % 2 == 0:
                        vs = lambda j: Ve[:, 2 + r0 // 2 + j, :]
                    else:
                        vs = lambda j: Vo[:, (r0 + 3) // 2 + j, :]
                    row(QT[:, NCTX + r * 64:NCTX + (r + 1) * 64],
                        lambda j: KT[:, NCTX + (r0 + 2 * j) * 64:NCTX + (r0 + 2 * j) * 64 + 128], off, vs, r % 2)
                    A.activation(out=OAll[:, r * 64:(r + 1) * 64], in_=PS[1][0:65, (r % 2) * 64:(r % 2 + 1) * 64], func=AF.Copy)

                for r in range(32):
                    static_row(r)
                B()
                for qt in range(4):
                    PE.matmul(PS[2 + qt][0:64, 0:512], SELD[0:65, 0:64], OAll[0:65, qt * 512:(qt + 1) * 512], start=True, stop=True)
                B()
                for qt in range(4):
                    V.reciprocal(out=E4[0:64, qt, :], in_=PS[2 + qt][0:64, 0:512])
                B()
                for qt in range(4):
                    V.tensor_tensor(out=OAll[0:64, qt * 512:(qt + 1) * 512], in0=OAll[0:64, qt * 512:(qt + 1) * 512],
                                    in1=E4[0:64, qt, :], op=ALU.mult)
                B()
                dq.start(YT64[h][:, NCTX:T], OAll[0:64, :])
                LW()
                attn_core(QT[:, 0:NCTX], KT, 64, NCTX, [0, 1], lambda c: Ve[:, c, :], 0.125, 4, E4)
                attn_finish(4, NCTX, OAS, RZ, YO)
                dq.start(YT64[h][:, 0:NCTX], YO[:, 0:NCTX])
                LW()

    def stage_ret(l):
        with ExitStack() as s2:
            def t(name, shape):
                return s2.enter_context(SBT(name, list(shape)))
            QR = t("r_qr", [64, 4, T]); KR = t("r_kr", [64, 4, T])
            RP = t("r_rp", [128, 8]); LG = t("r_lg", [128, 8])
            INTRA = t("r_intra", [128, 8, 128]); QDEC = t("r_qdec", [64, 8, 128]); KDEC = t("r_kdec", [128, 8, 64])
            CDv = t("r_cd", [64, 8]); KD1 = t("r_kd1", [128, 8])
            ST = t("r_st", [64, 8, 64]); Qd = t("r_qd", [64, 8, 128]); Am = t("r_am", [128, 8, 128]); Kd = t("r_kdm", [128, 8, 64])
            s_tab = ExitStack()
            RCn = s_tab.enter_context(SBT("r_rcn", [128, 4, 128])); RPOS = s_tab.enter_context(SBT("r_rpos", [128, 4, 128]))
            dq.start(RP[:], retp_in[l])
            dq.start(RCn[:], retc_in); dq.start(RPOS[:], retpos_in)
            LW()
            A.activation(out=LG[:], in_=RP[:], func=AF.Exp)
            B()
            V.tensor_scalar(out=LG[:], in0=LG[:], scalar1=-1.0, scalar2=1.0, op0=ALU.mult, op1=ALU.add)
            B()
            A.activation(out=LG[:], in_=LG[:], func=AF.Ln)
            B()
            for hd in range(8):
                dr = hd // 4
                A.activation(out=INTRA[:, hd, :], in_=RCn[:, 2 * dr, :], func=AF.Exp, scale=LG[:, hd:hd + 1])
                A.activation(out=QDEC[:, hd, :], in_=RPOS[0:64, dr, :], func=AF.Exp, scale=LG[0:64, hd:hd + 1])
                A.activation(out=KD1[:, hd:hd + 1], in_=RPOS[:, 2 + dr, 0:1], func=AF.Exp, scale=LG[:, hd:hd + 1])
            A.activation(out=CDv[:], in_=LG[0:64, :], func=AF.Exp, scale=128.0)
            B()
            for hd in range(8):
                dr = hd // 4
                V.scalar_tensor_tensor(out=INTRA[:, hd, :], in0=INTRA[:, hd, :], scalar=0.125, in1=RCn[:, 2 * dr + 1, :], op0=ALU.mult, op1=ALU.mult)
                V.tensor_scalar(out=KDEC[:, hd, :], in0=ones[:, 0:64], scalar1=KD1[:, hd:hd + 1], scalar2=0.125,
                                op0=ALU.mult, op1=ALU.mult)
            V.memset(ST[:], 0.0)
            B()
            s_tab.close()
            with ExitStack() as sp:
                def tp(name, shape):
                    return sp.enter_context(SBT(name, list(shape)))
                RC = tp("r_rc", [64, T]); RS = tp("r_rs", [64, T])
                Ab = tp("r_a", [64, T]); Bb = tp("r_b", [64, T]); t1 = None; t2 = None
                dq.start(RC[:], rope64_in[0]); dq.start(RS[:], rope64_in[1])
                LW()
                bufs = (Ab, Bb, t1, t2)
                for hh in range(4):
                    prep(QR[:, hh, :], (768 + hh * 64) // 16, 64, (RC[:], RS[:]), bufs)
                    prep(KR[:, hh, :], (1024 + hh * 64) // 16, 64, (RC[:], RS[:]), bufs)

            Vs = t("r_vs", [128, 2, 256])
            OAcc = t("r_oacc", [64, 4, T])
            V.memset(OAcc[:], 0.0)
            B()

            def step(cf, cb):
                csl = [slice(cf * 128, (cf + 1) * 128), slice(cb * 128, (cb + 1) * 128)]
                cidx = [cf, cb]
                dq.start(Vs[:, 0, :], VTc[:, cf, 256:512]); dq.start(Vs[:, 1, :], VTc[:, cb, 256:512])
                LW()
                for hd in range(8):
                    dr, hh = hd // 4, hd % 4
                    PE.matmul(PS[dr][:, hh * 128:(hh + 1) * 128], KR[:, hh, csl[dr]], QR[:, hh, csl[dr]], start=True, stop=True)
                    PE.matmul(PS[2][:, hd * 64:(hd + 1) * 64], KR[:, hh, csl[dr]], ident[0:64, 0:64], start=True, stop=True)
                for dr in range(2):
                    V.tensor_tensor(out=Qd[:, dr * 4:(dr + 1) * 4, :], in0=QR[:, :, csl[dr]], in1=QDEC[:, dr * 4:(dr + 1) * 4, :], op=ALU.mult)
                B()
                for dr in range(2):
                    V.tensor_tensor(out=Am[:, dr * 4:(dr + 1) * 4, :], in0=PS[dr][:, :].rearrange("p (h c) -> p h c", c=128),
                                    in1=INTRA[:, dr * 4:(dr + 1) * 4, :], op=ALU.mult)
                V.tensor_tensor(out=Kd[:], in0=PS[2][:, :].rearrange("p (h c) -> p h c", c=64), in1=KDEC[:], op=ALU.mult)
                B()
                for hd in range(8):
                    dr, hh = hd // 4, hd % 4
                    vch = Vs[:, dr, hh * 64:(hh + 1) * 64]
                    ob = PS[3 + dr][0:64, hh * 128:(hh + 1) * 128]
                    PE.matmul(ob, vch, Am[:, hd, :], start=True, stop=True)
                    PE.matmul(PS[6 + dr][0:64, hh * 128:(hh + 1) * 128], ST[:, hd, :], Qd[:, hd, :], start=True, stop=True)
                    PE.matmul(PS[5][0:64, hd * 64:(hd + 1) * 64], Kd[:, hd, :], vch, start=True, stop=True)
                B()
                for hd in range(8):
                    V.scalar_tensor_tensor(out=ST[:, hd, :], in0=ST[:, hd, :], scalar=CDv[:, hd:hd + 1],
                                           in1=PS[5][0:64, hd * 64:(hd + 1) * 64], op0=ALU.mult, op1=ALU.add)
                for dr in range(2):
                    V.tensor_tensor(out=OAcc[:, :, csl[dr]], in0=OAcc[:, :, csl[dr]],
                                    in1=PS[3 + dr][0:64, :].rearrange("p (h c) -> p h c", c=128), op=ALU.add)
                B()
                for dr in range(2):
                    V.tensor_tensor(out=OAcc[:, :, csl[dr]], in0=OAcc[:, :, csl[dr]],
                                    in1=PS[6 + dr][0:64, :].rearrange("p (h c) -> p h c", c=128), op=ALU.add)
                B()

            if not os.environ.get("RET_SKIPSTEPS"):
                step(0, 1)
                step(1, 0)
                for j in range(16):
                    step(j + 2, 17 - j)
            with ExitStack() as s3:
                G = s3.enter_context(SBT("r_g", [64, 512]))
                SQ = s3.enter_context(SBT("r_sq", [64, 512]))
                CEN = s3.enter_context(SBT("r_cen", [64, 512]))
                MSQ = s3.enter_context(SBT("r_msq", [64, 512]))
                for hh in range(4):
                    for ti, (t0, n) in enumerate(TILES):
                        O = OAcc[:, hh, t0:t0 + n]
                        dq.start(G[:, 0:n], PT64[(1536 // 64) + hh][:, t0:t0 + n])
                        A.activation(out=SQ[:, 0:n], in_=O, func=AF.Square)
                        LW()
                        A.activation(out=G[:, 0:n], in_=G[:, 0:n], func=AF.Silu)
                        PE.matmul(PS[0][0:64, 0:n], ones[0:64, 0:64], O, start=True, stop=True)
                        PE.matmul(PS[1][0:64, 0:n], ones[0:64, 0:64], SQ[:, 0:n], start=True, stop=True)
                        B()
                        A.activation(out=CEN[:, 0:n], in_=PS[0][0:64, 0:n], func=AF.Copy, scale=1.0 / 64)
                        A.activation(out=SQ[:, 0:n], in_=PS[1][0:64, 0:n], func=AF.Copy, scale=1.0 / 64)
                        B()
                        A.activation(out=MSQ[:, 0:n], in_=CEN[:, 0:n], func=AF.Square)
                        B()
                        V.tensor_tensor(out=CEN[:, 0:n], in0=O, in1=CEN[:, 0:n], op=ALU.subtract)
                        V.tensor_tensor(out=MSQ[:, 0:n], in0=SQ[:, 0:n], in1=MSQ[:, 0:n], op=ALU.subtract)
                        B()
                        A.activation(out=MSQ[:, 0:n], in_=MSQ[:, 0:n], func=AF.Sqrt, bias=epsc[0:64, :], scale=1.0)
                        B()
                        V.reciprocal(out=MSQ[:, 0:n], in_=MSQ[:, 0:n])
                        B()
                        V.tensor_tensor(out=CEN[:, 0:n], in0=CEN[:, 0:n], in1=MSQ[:, 0:n], op=ALU.mult)
                        B()
                        V.tensor_tensor(out=G[:, 0:n], in0=CEN[:, 0:n], in1=G[:, 0:n], op=ALU.mult)
                        B()
                        dq.start(YT64[4 + hh][:, t0:t0 + n], G[:, 0:n])
                        LW()

    def stage_merge(l):
        wb = w_branch[l].rearrange("n (h p) (m j) -> p n h m j", p=64, j=128)
        GTv = PT[3072:7168, :].rearrange("(n m p) t -> m p n t", n=4, p=128)
        YTv = YT.rearrange("(a p) t -> p a t", p=64)
        ATv = AT.rearrange("(m p) t -> m p t", p=128)
        with ExitStack() as s2:
            def t(name, shape):
                return s2.enter_context(SBT(name, list(shape)))
            Y = t("m_y", [64, 16, 512]); W = t("m_w", [64, 4, 4, 128]); Gt = t("m_g", [128, 4, 512])
            Tm = t("m_t", [128, 4, 512])
            for ti, (t0, n) in enumerate(TILES):
                dq.start(Y[:, :, 0:n], YTv[:, :, t0:t0 + n])
                LW()
                with MyFori(0, 8) as m:
                    for nb in range(4):
                        dq.start(W[:, nb, :, :], wb[:, nb, :, m, :])
                    dq.start(Gt[:, :, 0:n], GTv[m][:, :, t0:t0 + n])
                    LW()
                    for nb in range(4):
                        for hh in range(4):
                            PE.matmul(PS[nb][:, 0:n], W[:, nb, hh, :], Y[:, nb * 4 + hh, 0:n], start=(hh == 0), stop=(hh == 3))
                    B()
                    for nb in range(4):
                        V.tensor_tensor(out=Tm[:, nb, 0:n], in0=PS[nb][:, 0:n], in1=Gt[:, nb, 0:n], op=ALU.mult)
                    B()
                    V.tensor_tensor(out=Tm[:, 0, 0:n], in0=Tm[:, 0, 0:n], in1=Tm[:, 1, 0:n], op=ALU.add)
                    V.tensor_tensor(out=Tm[:, 2, 0:n], in0=Tm[:, 2, 0:n], in1=Tm[:, 3, 0:n], op=ALU.add)
                    B()
                    V.tensor_tensor(out=Tm[:, 0, 0:n], in0=Tm[:, 0, 0:n], in1=Tm[:, 2, 0:n], op=ALU.add)
                    B()
                    dq.start(ATv[m][:, t0:t0 + n], Tm[:, 0, 0:n])
                    LW()

    def stage_moe(l):
        XSv = XS.rearrange("(k p) t -> p k t", p=128)
        with ExitStack() as s2:
            def t(name, shape):
                return s2.enter_context(SBT(name, list(shape)))
            WT = t("e_wt", [16, T])
            with ExitStack() as s3:
                BIG = s3.enter_context(SBT("e_big", [128, 8, T]))
                stage_norm(BIG, lambda v, k: GG[:, v, 1, l, k:k + 1], lambda v, k: MODS[:, v, l, 24 + k:25 + k])
                dq.start(MTD.rearrange("(k p) t -> p k t", p=128), BIG[:])
                LW()
                WR = s3.enter_context(SBT("e_wr", [128, 8, 16]))
                RB = s3.enter_context(SBT("e_rb", [128, 16]))
                S = s3.enter_context(SBT("e_s", [128, 18, 16]))
                SEL = s3.enter_context(SBT("e_sel", [128, 18, 16]))
                SEL2 = s3.enter_context(SBT("e_sel2", [128, 18, 16]))
                EM = s3.enter_context(SBT("e_em", [128, 18, 16]))
                M1 = s3.enter_context(SBT("e_m1", [128, 72]))
                M2 = s3.enter_context(SBT("e_m2", [128, 72]))
                GS = s3.enter_context(SBT("e_gs", [128, 72]))
                GM = s3.enter_context(SBT("e_gm", [128, 72]))
                GX = s3.enter_context(SBT("e_gx", [128, 18]))
                dq.start(WR[:], w_router.rearrange("(k p) e -> p k e", p=128))
                dq.start(RB[:], rb_in)
                LW()
                for tc in range(18):
                    for k in range(8):
                        PE.matmul(PS[0][:, tc * 16:(tc + 1) * 16], BIG[:, k, tc * 128:(tc + 1) * 128], WR[:, k, :], start=(k == 0), stop=(k == 7))
                B()
                A.activation(out=S[:].rearrange("p c e -> p (c e)"), in_=PS[0][:, 0:288], func=AF.Sigmoid)
                B()
                for c in range(18):
                    V.tensor_tensor(out=SEL[:, c, :], in0=S[:, c, :], in1=RB[:], op=ALU.add)
                B()
                sel4 = SEL[:].rearrange("p c (g i) -> p (c g) i", i=4)
                sel24 = SEL2[:].rearrange("p c (g i) -> p (c g) i", i=4)
                em4 = EM[:].rearrange("p c (g i) -> p (c g) i", i=4)
                gs3 = GS[:].rearrange("p (c g) -> p c g", g=4)
                gm3 = GM[:].rearrange("p (c g) -> p c g", g=4)
                V.reduce_max(out=M1[:], in_=sel4, axis=AX.X)
                B()
                for i in range(4):
                    V.tensor_tensor(out=em4[:, :, i], in0=sel4[:, :, i], in1=M1[:, :], op=ALU.is_equal)
                B()
                V.tensor_scalar(out=sel24, in0=em4, scalar1=-1.0e9, scalar2=None, op0=ALU.mult)
                B()
                V.tensor_tensor(out=sel24, in0=sel24, in1=sel4, op=ALU.add)
                B()
                V.reduce_max(out=M2[:], in_=sel24, axis=AX.X)
                B()
                V.tensor_tensor(out=GS[:], in0=M1[:], in1=M2[:], op=ALU.add)
                B()
                V.reduce_max(out=GX[:], in_=gs3, axis=AX.X)
                B()
                for g in range(4):
                    V.tensor_tensor(out=gm3[:, :, g], in0=gs3[:, :, g], in1=GX[:, :], op=ALU.is_equal)
                for i in range(4):
                    V.tensor_tensor(out=em4[:, :, i], in0=sel4[:, :, i], in1=M2[:, :], op=ALU.is_ge)
                B()
                for i in range(4):
                    V.tensor_tensor(out=em4[:, :, i], in0=em4[:, :, i], in1=GM[:, :], op=ALU.mult)
                B()
                V.tensor_tensor(out=EM[:], in0=EM[:], in1=S[:], op=ALU.mult)
                B()
                V.reduce_sum(out=GX[:], in_=EM[:], axis=AX.X)
                B()
                V.reciprocal(out=GX[:], in_=GX[:])
                B()
                for ee in range(16):
                    V.tensor_tensor(out=EM[:, :, ee], in0=EM[:, :, ee], in1=GX[:, :], op=ALU.mult)
                B()
                for c in range(18):
                    PE.matmul(PS[c // 4][0:16, (c % 4) * 128:(c % 4 + 1) * 128], EM[:, c, :], ident[:, :], start=True, stop=True)
                B()
                for c in range(18):
                    A.activation(out=WT[:, c * 128:(c + 1) * 128], in_=PS[c // 4][0:16, (c % 4) * 128:(c % 4 + 1) * 128], func=AF.Copy)
                B()
            with ExitStack() as s3:
                def t3(name, shape):
                    return s3.enter_context(SBT(name, list(shape)))
                WG = t3("e_wg", [128, 8, 512]); WU = t3("e_wu", [128, 8, 512]); WD = t3("e_wd", [128, 4, D])
                WGb = s3.enter_context(SBT("e_wgb", [128, 8, 512], BF16)); WUb = s3.enter_context(SBT("e_wub", [128, 8, 512], BF16))
                WDb = s3.enter_context(SBT("e_wdb", [128, 4, D], BF16))
                MTb = s3.enter_context(SBT("e_mtb", [128, 8, 512], BF16)); HIDb = s3.enter_context(SBT("e_hidb", [128, 4, 512], BF16))
                SELS = t3("e_sels", [16, 128]); WBC = t3("e_wbc", [128, T])
                MTt = t3("e_mt", [128, 8, 512]); SG = t3("e_sg", [128, 4, 512]); HID = SG; WBCt = t3("e_wbct", [128, 512]); ACCt = t3("e_acct", [128, 8, 512])
                ATk = MTD.rearrange("(k p) t -> p k t", p=128)
                ACCk = ACCD.rearrange("(k p) t -> p k t", p=128)
                V.memset(ACCt[:], 0.0)
                B()
                for ti, (t0, n) in enumerate(TILES):
                    dq.start(ACCk[:, :, t0:t0 + n], ACCt[:, :, 0:n])
                LW()
                wgv = w_gate[l].rearrange("e (k p) f -> e p k f", p=128)
                wuv = w_up[l].rearrange("e (k p) f -> e p k f", p=128)
                wdv = w_down[l].rearrange("e (c p) f -> e p c f", p=128)

                def tile_body(sl, n):
                    dq.start(MTt[:, :, 0:n], sl(ATk))
                    V.tensor_copy(out=WBCt[:, 0:n], in_=sl(WBC))
                    dq.start(ACCt[:, :, 0:n], sl(ACCk))
                    LW()
                    A.activation(out=MTb[:, 0:4, 0:n], in_=MTt[:, 0:4, 0:n], func=AF.Copy)
                    V.tensor_copy(out=MTb[:, 4:8, 0:n], in_=MTt[:, 4:8, 0:n])
                    B()
                    for hc in range(4):
                        for k in range(8):
                            PE.matmul(PS[hc][:, 0:n], WGb[:, k, hc * 128:(hc + 1) * 128], MTb[:, k, 0:n], start=(k == 0), stop=(k == 7))
                        for k in range(8):
                            PE.matmul(PS[4 + hc][:, 0:n], WUb[:, k, hc * 128:(hc + 1) * 128], MTb[:, k, 0:n], start=(k == 0), stop=(k == 7))
                    B()
                    for hc in range(4):
                        A.activation(out=SG[:, hc, 0:n], in_=PS[hc][:, 0:n], func=AF.Silu)
                    B()
                    for hc in range(4):
                        V.tensor_tensor(out=HID[:, hc, 0:n], in0=PS[4 + hc][:, 0:n], in1=SG[:, hc, 0:n], op=ALU.mult)
                    B()
                    for hc in range(4):
                        V.tensor_tensor(out=HIDb[:, hc, 0:n], in0=HID[:, hc, 0:n], in1=WBCt[:, 0:n], op=ALU.mult)
                    B()
                    for m in range(8):
                        for hc in range(4):
                            PE.matmul(PS[m][:, 0:n], WDb[:, hc, m * 128:(m + 1) * 128], HIDb[:, hc, 0:n], start=(hc == 0), stop=(hc == 3))
                    B()
                    for m in range(8):
                        V.tensor_tensor(out=ACCt[:, m, 0:n], in0=ACCt[:, m, 0:n], in1=PS[m][:, 0:n], op=ALU.add)
                    B()
                    dq.start(sl(ACCk), ACCt[:, :, 0:n])
                    LW()

                def sl_ctx(ap):
                    return ap[:, :, 0:NCTX] if len(ap.shape) == 3 else ap[:, 0:NCTX]

                with MyFori(0, 16) as e:
                    dq.start(WG[:], wgv[e]); dq.start(WU[:], wuv[e]); dq.start(WD[:], wdv[e])
                    dq.start(SELS[:], sel16_in[:, e, :])
                    LW()
                    for ti, (t0, n) in enumerate(TILES):
                        PE.matmul(PS[ti][:, 0:n], SELS[:, :], WT[:, t0:t0 + n], start=True, stop=True)
                    LW()
                    for ti, (t0, n) in enumerate(TILES):
                        A.activation(out=WBC[:, t0:t0 + n], in_=PS[ti][:, 0:n], func=AF.Copy)
                    V.tensor_copy(out=WGb[:], in_=WG[:])
                    nc.gpsimd.tensor_copy(out=WUb[:], in_=WU[:])
                    A.activation(out=WDb[:, 0:2, :], in_=WD[:, 0:2, :], func=AF.Copy)
                    V.tensor_copy(out=WDb[:, 2:4, :], in_=WD[:, 2:4, :])
                    B()
                    if l < DEPTH - 1:
                        tile_body(sl_ctx, NCTX)
                    with MyFori(0, 4) as qt:
                        def sl_lat(ap):
                            if len(ap.shape) == 3:
                                return ap[:, :, NCTX:T][:, :, bass.ts(qt, 512)]
                            return ap[:, NCTX:T][:, bass.ts(qt, 512)]
                        tile_body(sl_lat, 512)

            with SBT("e_xs", [128, 8, 512]) as xs, SBT("e_ac", [128, 8, 512]) as ac:
                ACCk = ACCD.rearrange("(k p) t -> p k t", p=128)
                for ti, (t0, n) in enumerate(TILES):
                    v = 0 if ti == 0 else 1
                    dq.start(xs[:, :, 0:n], XSv[:, :, t0:t0 + n])
                    dq.start(ac[:, :, 0:n], ACCk[:, :, t0:t0 + n])
                    LW()
                    for k in range(8):
                        V.scalar_tensor_tensor(out=xs[:, k, 0:n], in0=ac[:, k, 0:n], scalar=MODS[:, v, l, 40 + k:41 + k],
                                               in1=xs[:, k, 0:n], op0=ALU.mult, op1=ALU.add)
                    B()
                    dq.start(XSv[:, :, t0:t0 + n], xs[:, :, 0:n])
                    LW()


    STAGES = ["norm1", "inproj", "vtok", "diff", "gqa", "ret", "na", "merge", "outproj", "moe"]

    def enabled(name):
        return stop_after is None or STAGES.index(name) <= STAGES.index(stop_after)

    from contextlib import nullcontext as _nullctx

    class _Const:
        def __enter__(self):
            return 0

        def __exit__(self, *a):
            return False

    with (MyFori(0, nsamp) if nsamp > 1 else _Const()) as s:
        dq.start(out[s], OUTS)
        dq.start(XS, xin[s])
        V.tensor_copy(out=MODS[:, 1, :, :], in_=MOD[:, :, :, s])
        V.tensor_copy(out=MODS[:, 0, :, :], in_=MOD[:, :, :, 4])
        LW()
        for v in range(2):
            for nrm in range(2):
                V.scalar_tensor_tensor(out=GG[:, v, nrm, :, :], in0=MODS[:, v, :, 8 + 24 * nrm:16 + 24 * nrm], scalar=1.0,
                                       in1=GN[:, 2 * nrm:2 * nrm + 2, :], op0=ALU.add, op1=ALU.mult)
        B()
        for l in range(nlayers):
            with SBT("BIG", [128, 8, T]) as BIG:
                if enabled("norm1"):
                    stage_norm(BIG, lambda v, k: GG[:, v, 0, l, k:k + 1], lambda v, k: MODS[:, v, l, k:k + 1])
                    if dbg and "HT" in dbg:
                        dq.start(dbg_t["HT"].rearrange("(k p) t -> p k t", p=128), BIG[:])
                        LW()
                wv = w_in[l].rearrange("(k p) (m j) -> p k m j", p=128, j=128)
                if enabled("inproj"):
                    with SBT("BIGb", [128, 8, T], BF16) as BIGb:
                        cast_big(BIG, BIGb)
                        stage_gemm(BIGb, wv, 0, 24, 0, PT, ("copy",))
                        stage_gemm(BIGb, wv, 24, 56, 0, PT, ("sigmoid",))
                if enabled("vtok"):
                    stage_vtok(BIG, l)
            if enabled("diff"):
                stage_diff(l)
            if enabled("gqa"):
                stage_gqa(l)
            if enabled("ret"):
                stage_ret(l)
            if enabled("na"):
                stage_na(l)
            if enabled("merge"):
                stage_merge(l)
            if enabled("outproj"):
                with SBT("BIG2", [128, 8, T]) as BIG:
                    dq.start(BIG[:], AT.rearrange("(k p) t -> p k t", p=128))
                    LW()
                    wo = w_out[l].rearrange("(k p) (m j) -> p k m j", p=128, j=128)
                    with SBT("BIGb2", [128, 8, T], BF16) as BIGb:
                        cast_big(BIG, BIGb)
                        stage_gemm(BIGb, wo, 0, 8, 0, XS, ("resid", lambda v, m: MODS[:, v, l, 16:24][:, bass.ts(m, 1)]))
            if dbg and "X1" in dbg:
                dq.start(dbg_t["X1"], XS)
                LW()
            if enabled("moe"):
                stage_moe(l)
        with SBT("BIG3", [128, 8, T]) as BIG:
            stage_norm(BIG, lambda v, k: GN[:, 4, k:k + 1], lambda v, k: ZERO8[:, k:k + 1])
            dq.start(OUTS.rearrange("(k p) t -> p k t", p=128), BIG[:, :, NCTX:T])
            LW()
    dq.start(out[nsamp], OUTS)
    LW()
    es.close()
    if os.environ.get('SBUF_PEAK'):
        print('SBUF peaks', {k: v // 1024 for k, v in _peak.items()})
    return nc


def _consts():
    c = {}
    c["ident"] = np.eye(128, dtype=np.float32)
    seld = np.zeros((65, 64), np.float32); seld[64, :] = 1.0
    c["seld"] = seld
    sel16 = np.zeros((16, 16, 128), np.float32)
    for e in range(16):
        sel16[e, e, :] = 1.0
    c["sel16"] = sel16
    k = np.arange(128)[:, None].astype(np.float32); q = np.arange(128)[None, :].astype(np.float32)
    retc = np.zeros((128, 4, 128), np.float32)
    retc[:, 0, :] = np.maximum(q - k, 0); retc[:, 1, :] = (q >= k)
    retc[:, 2, :] = np.maximum(k - q, 0); retc[:, 3, :] = (k >= q)
    c["retc"] = retc
    rp = np.zeros((128, 4, 128), np.float32)
    t = np.arange(128, dtype=np.float32)
    rp[:, 0, :] = (t + 1.0)[None, :]; rp[:, 1, :] = (128.0 - t)[None, :]
    rp[:, 2, 0] = 127.0 - t; rp[:, 3, 0] = t
    c["retpos"] = rp

    def rope_tab(d):
        half = d // 2; nf = half // 2
        inv = (1.0 / (10000.0 ** (np.arange(nf, dtype=np.float32) / nf))).astype(np.float32)
        tt = np.arange(L, dtype=np.int32)
        rows = (tt // 64).astype(np.float32); cols = (tt % 64).astype(np.float32)
        C = np.ones((d, T), np.float32); S = np.zeros((d, T), np.float32)
        for blk, pos in ((0, rows), (1, cols)):
            ang = (pos[None, :] * inv[:, None]).astype(np.float32)
            cs, sn = np.cos(ang).astype(np.float32), np.sin(ang).astype(np.float32)
            b0 = blk * half
            C[b0:b0 + nf, NCTX:] = cs; C[b0 + nf:b0 + half, NCTX:] = cs
            S[b0:b0 + nf, NCTX:] = -sn; S[b0 + nf:b0 + half, NCTX:] = sn
        return np.stack([C, S])
    c["rope64"] = rope_tab(64)
    c["rope32"] = rope_tab(32)
    cc = np.arange(64)[:, None]; kc = np.arange(64)[None, :]
    ws = np.clip(cc - 8, 0, 48)
    ok = (kc >= ws) & (kc < ws + 16)
    idx = np.clip(kc - cc, -15, 15) + 15
    msk = np.where(ok, 0.0, NEGM).astype(np.float32)
    c["namask"] = np.ascontiguousarray(np.concatenate([msk.T, msk.T], axis=0))
    c["_naidx"] = idx
    lamc = np.zeros((128, 2, 2), np.float32)
    c["lamc"] = lamc
    return c


_CACHE = {}


def _prep_inputs(inp, nsamp_per_core, ncores):
    c = _consts()
    f = lambda a: np.ascontiguousarray(a, dtype=np.float32)
    shared = {}
    shared["w_mod"] = f(inp["w_mod"]); shared["w_in"] = f(inp["w_in"]); shared["w_branch"] = f(inp["w_branch"])
    shared["w_out"] = f(inp["w_out"]); shared["w_router"] = f(inp["w_router"])
    shared["w_gate"] = f(inp["w_gate_e"]); shared["w_up"] = f(inp["w_up_e"]); shared["w_down"] = f(inp["w_down_e"])
    shared["bmod"] = f(inp["b_mod"].reshape(2, 48, 128).transpose(2, 0, 1))
    gn = np.stack([inp["g_norm1"][0], inp["g_norm1"][1], inp["g_norm2"][0], inp["g_norm2"][1], inp["g_final"]])
    shared["gn"] = f(gn.reshape(5, 8, 128).transpose(2, 0, 1))
    idx = c["_naidx"]
    rpb = inp["na_rpb"]
    g = rpb[:, :, :, idx]
    nab = np.zeros((2, 128, 4, 14, 64), np.float32)
    for jj in range(2):
        nab[:, jj * 64:(jj + 1) * 64] = g[:, :, jj:jj + 14].transpose(0, 4, 1, 2, 3)
    shared["nab"] = f(nab)
    shared["namask"] = c["namask"]
    shared["retp"] = f(np.broadcast_to(inp["ret_log_decay"].reshape(2, 1, 8), (2, 128, 8)))
    perm = np.concatenate([np.arange(16, 32), np.arange(0, 16), np.arange(48, 64), np.arange(32, 48)])
    qg, kg = inp["gqa_q_gain"], inp["gqa_k_gain"]
    shared["gqag"] = f(np.stack([qg, qg[:, perm], kg, kg[:, perm]], axis=-1))
    shared["dlam"] = f(np.broadcast_to(inp["diff_lambda"].reshape(2, 1, 128), (2, 128, 128)))
    shared["dsub"] = f(inp["diff_subln"].T)
    shared["lamc"] = c["lamc"]
    shared["rb"] = f(np.broadcast_to(inp["router_bias"][None, :], (128, 16)))
    for k in ("rope64", "rope32", "ident", "seld", "sel16", "retc", "retpos"):
        shared[k] = c[k]
    maps = []
    for ci in range(ncores):
        b0 = ci * nsamp_per_core
        bs = slice(b0, b0 + nsamp_per_core)
        xin = np.concatenate([inp["ctx"][bs].transpose(0, 2, 1), inp["x"][bs].transpose(0, 2, 1)], axis=2)
        cc = np.concatenate([inp["c"][bs], np.zeros((4 - nsamp_per_core, D), np.float32), inp["c_ctx"][None, :]], axis=0)
        m = dict(shared)
        m["xin"] = f(xin)
        m["cT"] = f(cc.T.reshape(8, 128, 5).transpose(1, 0, 2))
        maps.append(m)
    return maps


def kernel(**inputs):
    inp = {k: np.asarray(v) for k, v in inputs.items()}
    ncores = 8
    nb = inp["x"].shape[0] // ncores
    if "nc" not in _CACHE:
        _CACHE["nc"] = build(nsamp=nb, nlayers=DEPTH)
    nc = _CACHE["nc"]
    maps = _prep_inputs(inp, nb, ncores)
    res = run_bass_kernel_spmd(nc, maps, core_ids=list(range(ncores)))
    outs = [r["out"][1:] for r in res.results]
    o = np.concatenate(outs, axis=0).transpose(0, 2, 1)
    return np.ascontiguousarray(o, dtype=np.float32)
```
